# Optimizing a Trainium2 kernel written in Bass

```python
import jax, jax.numpy as jnp
from jax import lax
import numpy as np

D_MODEL = 1024
BATCH = 16
SEQ = 2048
DEPTH = 2

N_MIXERS = 2
HEAD_SIZE = 64
N_HEADS = D_MODEL // HEAD_SIZE
DECAY_LORA = max(32, int(round(D_MODEL ** 0.5 * 1.8 / 32)) * 32)
AAA_LORA = max(32, int(round(D_MODEL ** 0.5 * 1.8 / 32)) * 32)
GATE_LORA = max(32, int(round(D_MODEL ** 0.8 * 0.6 / 32)) * 32)
D_FF = int(round(8 * D_MODEL / 3 / 256)) * 256
CONV_WIDTH = 31
N_RWKV = (DEPTH + 1) // 2
N_CONV = DEPTH // 2
RMS_EPS = 1e-6
LN_EPS = 1e-5
GN_EPS = 64e-5
L2_EPS = 1e-12

kernel_name = 'hybrid_rwkv7_conformer_macaron'


def rms_norm(x, gain):
    x32 = x.astype(jnp.float32)
    y = x32 * lax.rsqrt(jnp.mean(x32 * x32, axis=-1, keepdims=True) + RMS_EPS)
    return (y * gain.astype(jnp.float32)).astype(x.dtype)


def swiglu(x, w_gate, w_up, w_down):
    return (jax.nn.silu(x @ w_gate) * (x @ w_up)) @ w_down


def rwkv7_time_mix(x, mu, w_rkv, w0, w1, w2, a0, a1, a2, g1, g2, k_k, k_a, r_k,
                   ln_gain, ln_bias, w_out):
    B, T, D = x.shape
    H, N = N_HEADS, HEAD_SIZE
    x_prev = jnp.pad(x[:, :-1], ((0, 0), (1, 0), (0, 0)))
    xx = x_prev - x
    xr = x + xx * mu[0]
    xw = x + xx * mu[1]
    xk = x + xx * mu[2]
    xv = x + xx * mu[3]
    xa = x + xx * mu[4]
    xg = x + xx * mu[5]

    r = xr @ w_rkv[0]
    k = xk @ w_rkv[1]
    v = xv @ w_rkv[2]
    w = -jax.nn.softplus(-(w0 + jnp.tanh(xw @ w1) @ w2)) - 0.5
    a = jax.nn.sigmoid(a0 + (xa @ a1) @ a2)
    g = jax.nn.sigmoid(xg @ g1) @ g2

    kk = (k * k_k).reshape(B, T, H, N).astype(jnp.float32)
    kk = kk / jnp.maximum(jnp.linalg.norm(kk, axis=-1, keepdims=True), L2_EPS)
    k = k * (1.0 + (a - 1.0) * k_a)

    f32 = jnp.float32
    r_h = r.reshape(B, T, H, N).astype(f32)
    k_h = k.reshape(B, T, H, N).astype(f32)
    v_h = v.reshape(B, T, H, N).astype(f32)
    decay = jnp.exp(-jnp.exp(w.reshape(B, T, H, N).astype(f32)))
    a_vec = -kk
    b_vec = kk * a.reshape(B, T, H, N).astype(f32)

    def step(S, inp):
        r_t, w_t, k_t, v_t, a_t, b_t = inp
        sa = jnp.einsum('bhij,bhj->bhi', S, a_t)
        S = (S * w_t[:, :, None, :] + sa[..., None] * b_t[:, :, None, :]
             + v_t[..., None] * k_t[:, :, None, :])
        y_t = jnp.einsum('bhij,bhj->bhi', S, r_t)
        return S, y_t

    to_time = lambda z: jnp.moveaxis(z, 1, 0)
    S0 = jnp.zeros((B, H, N, N), f32)
    _, ys = lax.scan(step, S0, (to_time(r_h), to_time(decay), to_time(k_h),
                                to_time(v_h), to_time(a_vec), to_time(b_vec)))
    y = jnp.moveaxis(ys, 0, 1)

    mean = jnp.mean(y, axis=-1, keepdims=True)
    var = jnp.mean(jnp.square(y - mean), axis=-1, keepdims=True)
    y = ((y - mean) * lax.rsqrt(var + GN_EPS)).reshape(B, T, D)
    y = y * ln_gain.astype(f32) + ln_bias.astype(f32)
    bonus = jnp.sum(r_h * k_h * r_k.astype(f32), axis=-1, keepdims=True) * v_h
    y = y + bonus.reshape(B, T, D)
    return ((y * g.astype(f32)) @ w_out.astype(f32)).astype(x.dtype)


def conformer_conv(x, w_in, b_in, dw, dw_b, ln_gain, ln_bias, w_out, b_out):
    D = x.shape[-1]
    h = x @ w_in + b_in
    h = h[..., :D] * jax.nn.sigmoid(h[..., D:])
    h = lax.conv_general_dilated(
        h, dw[:, None, :].astype(h.dtype), window_strides=(1,),
        padding=[(CONV_WIDTH - 1, 0)],
        dimension_numbers=('NWC', 'WIO', 'NWC'),
        feature_group_count=D) + dw_b
    h32 = h.astype(jnp.float32)
    mean = jnp.mean(h32, axis=-1, keepdims=True)
    var = jnp.mean(jnp.square(h32 - mean), axis=-1, keepdims=True)
    h32 = (h32 - mean) * lax.rsqrt(var + LN_EPS) * ln_gain.astype(jnp.float32) + ln_bias.astype(jnp.float32)
    h = jax.nn.silu(h32).astype(x.dtype)
    return h @ w_out + b_out


def setup_inputs(seed: int = 0) -> dict:
    key = jax.random.key(seed)
    ks = iter(jax.random.split(key, 64))
    nrm = lambda shape, scale: scale * jax.random.normal(next(ks), shape, jnp.float32)
    D, F = D_MODEL, D_FF

    ratio = (jnp.arange(D, dtype=jnp.float32) / (D - 1)) ** 0.85
    w0 = (-7.0 + 5.0 * ratio + 0.5)[None, :] + nrm((N_RWKV, D), 0.1)

    return {
        'x': nrm((BATCH, SEQ, D), 1.0),
        'norm_gains': 1.0 + nrm((DEPTH, 3, D), 0.02),
        'ffn_w_gate': nrm((DEPTH, 2, D, F), D ** -0.5),
        'ffn_w_up': nrm((DEPTH, 2, D, F), D ** -0.5),
        'ffn_w_down': nrm((DEPTH, 2, F, D), F ** -0.5),
        'rwkv_mu': jax.random.uniform(next(ks), (N_RWKV, 6, D), jnp.float32),
        'rwkv_w_rkv': nrm((N_RWKV, 3, D, D), D ** -0.5),
        'rwkv_w0': w0,
        'rwkv_w1': nrm((N_RWKV, D, DECAY_LORA), 0.1 * D ** -0.5),
        'rwkv_w2': nrm((N_RWKV, DECAY_LORA, D), 0.1 * DECAY_LORA ** -0.5),
        'rwkv_a0': nrm((N_RWKV, D), 0.1),
        'rwkv_a1': nrm((N_RWKV, D, AAA_LORA), 0.1 * D ** -0.5),
        'rwkv_a2': nrm((N_RWKV, AAA_LORA, D), 0.1 * AAA_LORA ** -0.5),
        'rwkv_g1': nrm((N_RWKV, D, GATE_LORA), D ** -0.5),
        'rwkv_g2': nrm((N_RWKV, GATE_LORA, D), GATE_LORA ** -0.5),
        'rwkv_k_k': 0.85 + nrm((N_RWKV, D), 0.05),
        'rwkv_k_a': 1.0 + nrm((N_RWKV, D), 0.05),
        'rwkv_r_k': -0.04 + nrm((N_RWKV, N_HEADS, HEAD_SIZE), 0.05),
        'rwkv_ln_gain': 1.0 + nrm((N_RWKV, D), 0.02),
        'rwkv_ln_bias': nrm((N_RWKV, D), 0.02),
        'rwkv_w_out': nrm((N_RWKV, D, D), D ** -0.5),
        'conv_w_in': nrm((N_CONV, D, 2 * D), D ** -0.5),
        'conv_b_in': nrm((N_CONV, 2 * D), 0.02),
        'conv_dw': nrm((N_CONV, CONV_WIDTH, D), CONV_WIDTH ** -0.5),
        'conv_dw_b': nrm((N_CONV, D), 0.02),
        'conv_ln_gain': 1.0 + nrm((N_CONV, D), 0.02),
        'conv_ln_bias': nrm((N_CONV, D), 0.02),
        'conv_w_out': nrm((N_CONV, D, D), D ** -0.5),
        'conv_b_out': nrm((N_CONV, D), 0.02),
        'final_norm': 1.0 + nrm((D,), 0.02),
    }


def reference(x, norm_gains, ffn_w_gate, ffn_w_up, ffn_w_down,
              rwkv_mu, rwkv_w_rkv, rwkv_w0, rwkv_w1, rwkv_w2, rwkv_a0, rwkv_a1, rwkv_a2,
              rwkv_g1, rwkv_g2, rwkv_k_k, rwkv_k_a, rwkv_r_k, rwkv_ln_gain, rwkv_ln_bias,
              rwkv_w_out, conv_w_in, conv_b_in, conv_dw, conv_dw_b, conv_ln_gain,
              conv_ln_bias, conv_w_out, conv_b_out, final_norm):
    for i in range(DEPTH):
        x = x + 0.5 * swiglu(rms_norm(x, norm_gains[i, 0]),
                             ffn_w_gate[i, 0], ffn_w_up[i, 0], ffn_w_down[i, 0])
        h = rms_norm(x, norm_gains[i, 1])
        j = i // N_MIXERS
        if i % N_MIXERS == 0:
            x = x + rwkv7_time_mix(h, rwkv_mu[j], rwkv_w_rkv[j], rwkv_w0[j], rwkv_w1[j],
                                   rwkv_w2[j], rwkv_a0[j], rwkv_a1[j], rwkv_a2[j],
                                   rwkv_g1[j], rwkv_g2[j], rwkv_k_k[j], rwkv_k_a[j],
                                   rwkv_r_k[j], rwkv_ln_gain[j], rwkv_ln_bias[j],
                                   rwkv_w_out[j])
        else:
            x = x + conformer_conv(h, conv_w_in[j], conv_b_in[j], conv_dw[j], conv_dw_b[j],
                                   conv_ln_gain[j], conv_ln_bias[j], conv_w_out[j],
                                   conv_b_out[j])
        x = x + 0.5 * swiglu(rms_norm(x, norm_gains[i, 2]),
                             ffn_w_gate[i, 1], ffn_w_up[i, 1], ffn_w_down[i, 1])
    return rms_norm(x, final_norm)
```

```python
import bisect
import contextlib
import math

import numpy as np
import concourse.bass as bass
import concourse.mybir as mybir
from concourse.bass_utils import run_bass_kernel_spmd

F32 = mybir.dt.float32
BF16 = mybir.dt.bfloat16
AF = mybir.ActivationFunctionType
ALU = mybir.AluOpType
AX = mybir.AxisListType

D = 1024
DFF = 2816
NFC = DFF // 128
NKC = D // 128
H = 16
HS = 64
CONVW = 31
RMS_EPS = 1e-6
LN_EPS = 1e-5
GN_EPS = 64e-5
NCORES = 8

SAME_ENGINE_SYNC = True
_PH = [0]


def _pfx():
    _PH[0] += 1
    return f"p{_PH[0]}_"


class Trk:
    __slots__ = ("name", "writer", "readers")

    def __init__(self, name=""):
        self.name = name
        self.writer = None
        self.readers = {}


class Lane:
    def __init__(self, name, eng, sem, inc):
        self.name = name
        self.eng = eng
        self.sem = sem
        self.inc = inc
        self.n = 0
        self.count = 0
        self.sig_idx = []
        self.sig_val = []
        self.last = None
        self.last_sig = True
        self.seen = {}

    def ticket(self, idx):
        p = bisect.bisect_left(self.sig_idx, idx)
        if p < len(self.sig_idx):
            return self.sig_val[p]
        assert self.last is not None and not self.last_sig and self.n - 1 >= idx, (self.name, idx, self.n)
        self.last.then_inc(self.sem, self.inc)
        self.count += self.inc
        self.sig_idx.append(self.n - 1)
        self.sig_val.append(self.count)
        self.last_sig = True
        return self.count


class Sched:
    def __init__(self, nc):
        self.nc = nc

    def _wait(self, issuer, lane, idx):
        if lane is issuer and (not SAME_ENGINE_SYNC or lane.name == "pe"):
            return
        t = lane.ticket(idx)
        if issuer.seen.get(lane, 0) >= t:
            return
        issuer.seen[lane] = t
        issuer.eng.wait_ge(lane.sem, t)

    def _deps(self, issuer, reads, writes):
        for b in reads:
            if b.writer is not None:
                self._wait(issuer, *b.writer)
        for b in writes:
            relax = False
            if b.writer is not None and not (relax and b.writer[0] is issuer):
                self._wait(issuer, *b.writer)
            for ln, idx in b.readers.items():
                if not (relax and ln is issuer):
                    self._wait(issuer, ln, idx)

    def emit(self, lane, fn, reads=(), writes=()):
        self._deps(lane, reads, writes)
        ins = fn()
        idx = lane.n
        lane.n += 1
        lane.last = ins
        lane.last_sig = False
        for b in reads:
            b.readers[lane] = idx
        for b in writes:
            b.writer = (lane, idx)
            b.readers = {}
        return ins

    def dma(self, issuer, slot, fn, reads=(), writes=()):
        if slot.n > 0:
            t = slot.sig_val[-1]
            if issuer.seen.get(slot, 0) < t:
                issuer.seen[slot] = t
                issuer.eng.wait_ge(slot.sem, t)
        self._deps(issuer, reads, writes)
        if issuer.last is not None and not issuer.last_sig:
            issuer.ticket(issuer.n - 1)
        ins = fn()
        ins.then_inc(slot.sem, 16)
        idx = slot.n
        slot.n += 1
        slot.count += 16
        slot.sig_idx.append(idx)
        slot.sig_val.append(slot.count)
        issuer.n += 1
        issuer.last = None
        issuer.last_sig = True
        for b in reads:
            b.readers[slot] = idx
        for b in writes:
            b.writer = (slot, idx)
            b.readers = {}
        return ins

    def drain(self, issuer, slots):
        for slot in slots:
            if slot.n > 0:
                t = slot.sig_val[-1]
                if issuer.seen.get(slot, 0) < t:
                    issuer.seen[slot] = t
                    issuer.eng.wait_ge(slot.sem, t)


class Ctx:
    def __init__(self, nc, es):
        self.nc = nc
        self.S = Sched(nc)
        sem = lambda n: es.enter_context(nc.semaphore(n))
        self.PE = Lane("pe", nc.tensor, sem("s_pe"), 1)
        self.ACT = Lane("act", nc.scalar, sem("s_act"), 1)
        self.DVE = Lane("dve", nc.vector, sem("s_dve"), 1)
        self.POOL = Lane("pool", nc.gpsimd, sem("s_pool"), 1)
        self.SP = Lane("sp", nc.sync, sem("s_sp"), 1)
        self.slots_sp = [Lane(f"qs{i}", None, sem(f"s_qs{i}"), 16) for i in range(8)]
        self.slots_pl = [Lane(f"qp{i}", None, sem(f"s_qp{i}"), 16) for i in range(8)]
        self.rr_sp = 0
        self.rr_pl = 0

    def dma_sp(self, out, in_, reads=(), writes=()):
        slot = self.slots_sp[self.rr_sp % len(self.slots_sp)]
        self.rr_sp += 1
        nc = self.nc
        return self.S.dma(self.SP, slot, lambda: nc.sync.dma_start(out=out, in_=in_), reads, writes)

    def dma_pl(self, out, in_, reads=(), writes=()):
        slot = self.slots_pl[self.rr_pl % len(self.slots_pl)]
        self.rr_pl += 1
        nc = self.nc
        return self.S.dma(self.POOL, slot, lambda: nc.gpsimd.dma_start(out=out, in_=in_), reads, writes)

    def pe(self, fn, reads=(), writes=()):
        return self.S.emit(self.PE, fn, reads, writes)

    def act(self, fn, reads=(), writes=()):
        return self.S.emit(self.ACT, fn, reads, writes)

    def dve(self, fn, reads=(), writes=()):
        return self.S.emit(self.DVE, fn, reads, writes)

    def pool(self, fn, reads=(), writes=()):
        return self.S.emit(self.POOL, fn, reads, writes)

    def phase_end(self):
        self.S.drain(self.SP, self.slots_sp + self.slots_pl)
        self.nc.all_engine_barrier()


def tok_view(ap, r0, nrows):
    return ap[r0:r0 + nrows, :].rearrange("(s p) d -> p s d", p=128)


def ffn_phase(cx, src, dst, ntok, wg, wu, wd, gcol_ap, final_gain=None):
    nc = cx.nc
    TT = 256
    NS = TT // 128
    ntiles = ntok // TT
    with contextlib.ExitStack() as es:
        pf = _pfx()
        sb = lambda n, s, d: es.enter_context(nc.sbuf_tensor(pf + n, s, d))
        ps = lambda n, s, d: es.enter_context(nc.psum_tensor(pf + n, s, d))
        Wg = sb("Wg", [128, NFC, NKC, 128], BF16)
        Wu = sb("Wu", [128, NFC, NKC, 128], BF16)
        Wd = sb("Wd", [128, NFC, D], BF16)
        t_wg = [Trk() for _ in range(NFC)]
        t_wu = [Trk() for _ in range(NFC)]
        t_wd = [Trk() for _ in range(NFC)]
        xb = [sb(f"xb{i}", [128, NS, D], F32) for i in range(2)]
        t_xb = [Trk(), Trk()]
        xn = sb("xn", [128, NS, D], BF16)
        t_xn = Trk()
        junk = sb("junk", [128, D], BF16)
        t_junk = Trk()
        xnT = [sb(f"xnT{i}", [128, NKC, TT], BF16) for i in range(2)]
        t_xnT = [Trk(), Trk()]
        hT = [sb(f"hT{i}", [128, NFC, TT], BF16) for i in range(2)]
        t_hT = [Trk(), Trk()]
        sg = [sb(f"sg{i}", [128, TT], F32) for i in range(2)]
        t_sg = [Trk(), Trk()]
        ss = sb("ss", [128, 4], F32)
        t_ss = Trk()
        rstd = sb("rstd", [128, 4], F32)
        t_rstd = Trk()
        gcol = sb("gcol", [128, NKC], F32)
        t_gcol = Trk()
        ident = sb("ident", [128, 128], BF16)
        t_ident = Trk()
        epsb = sb("epsb", [128, 1], F32)
        t_eps = Trk()
        if final_gain is not None:
            fg = sb("fg", [128, D], F32)
            t_fg = Trk()
        pT = ps("pT", [128, NKC, TT], BF16)
        t_pT = Trk()
        pgu = [ps(f"pgu{i}", [128, 2, TT], F32) for i in range(2)]
        t_pgu = [Trk(), Trk()]
        py = [ps(f"py{i}", [128, 512], F32) for i in range(2)]
        t_py = [Trk(), Trk()]

        cx.pool(lambda: nc.gpsimd.memset(ident[:], 0.0), writes=[t_ident])
        cx.pool(lambda: nc.gpsimd.affine_select(
            out=ident[:], in_=ident[:], pattern=[[-1, 128]], compare_op=ALU.not_equal,
            fill=1.0, base=0, channel_multiplier=1), reads=[t_ident], writes=[t_ident])
        cx.pool(lambda: nc.gpsimd.memset(epsb[:], RMS_EPS), writes=[t_eps])
        cx.dma_sp(gcol[:], gcol_ap, writes=[t_gcol])
        if final_gain is not None:
            cx.dma_sp(fg[:], final_gain.partition_broadcast(128), writes=[t_fg])

        def load_x(i):
            cx.dma_sp(xb[i % 2][:], tok_view(src, i * TT, TT), writes=[t_xb[i % 2]])

        def load_w():
            for fc in range(NFC):
                cx.dma_pl(Wg[:, fc], wg[:, fc], writes=[t_wg[fc]])
                cx.dma_pl(Wu[:, fc], wu[:, fc], writes=[t_wu[fc]])
            for fc in range(NFC):
                cx.dma_pl(Wd[:, fc], wd[:, fc], writes=[t_wd[fc]])

        def norm(i):
            xt = xb[i % 2]
            tx = t_xb[i % 2]
            for s in range(NS):
                cx.act(lambda s=s: nc.scalar.activation(out=junk[:], in_=xt[:, s, :], func=AF.Square,
                                                        accum_out=ss[:, s:s + 1]),
                       reads=[tx], writes=[t_junk, t_ss])
            cx.act(lambda: nc.scalar.activation(out=rstd[:, 0:NS], in_=ss[:, 0:NS], func=AF.Sqrt,
                                                bias=epsb[:, 0:1], scale=1.0 / D),
                   reads=[t_ss, t_eps], writes=[t_rstd])
            cx.dve(lambda: nc.vector.reciprocal(out=rstd[:, 0:NS], in_=rstd[:, 0:NS]),
                   reads=[t_rstd], writes=[t_rstd])
            for s in range(NS):
                cx.dve(lambda s=s: nc.vector.tensor_scalar(out=xn[:, s, :], in0=xt[:, s, :],
                                                           scalar1=rstd[:, s:s + 1], scalar2=None,
                                                           op0=ALU.mult),
                       reads=[tx, t_rstd], writes=[t_xn])

        def transp(i):
            for s in range(NS):
                for kc in range(NKC):
                    cx.pe(lambda s=s, kc=kc: nc.tensor.transpose(
                        out=pT[:, kc, s * 128:(s + 1) * 128], in_=xn[:, s, kc * 128:(kc + 1) * 128],
                        identity=ident[:]), reads=[t_xn, t_ident], writes=[t_pT])
            cx.dve(lambda: nc.vector.tensor_tensor(
                out=xnT[i % 2][:], in0=pT[:], in1=gcol[:, :].unsqueeze(2).to_broadcast([128, NKC, TT]),
                op=ALU.mult), reads=[t_pT, t_gcol], writes=[t_xnT[i % 2]])

        def gate_up(i, fc):
            b = fc % 2
            xT = xnT[i % 2]
            for which, (W, tw) in enumerate(((Wg, t_wg), (Wu, t_wu))):
                for kc in range(NKC):
                    cx.pe(lambda which=which, W=W, kc=kc: nc.tensor.matmul(
                        pgu[b][:, which, :], lhsT=W[:, fc, kc, :], rhs=xT[:, kc, :],
                        start=(kc == 0), stop=(kc == NKC - 1)),
                        reads=[tw[fc], t_xnT[i % 2]], writes=[t_pgu[b]])
            cx.act(lambda: nc.scalar.activation(out=sg[b][:], in_=pgu[b][:, 0, :], func=AF.Silu),
                   reads=[t_pgu[b]], writes=[t_sg[b]])
            cx.dve(lambda: nc.vector.tensor_tensor(out=hT[i % 2][:, fc, :], in0=sg[b][:], in1=pgu[b][:, 1, :],
                                                   op=ALU.mult),
                   reads=[t_sg[b], t_pgu[b]], writes=[t_hT[i % 2]])

        def down(i):
            xt = xb[i % 2]
            tx = t_xb[i % 2]
            g = 0
            for s in range(NS):
                for half in range(2):
                    b = g % 2
                    g += 1
                    for fc in range(NFC):
                        cx.pe(lambda fc=fc: nc.tensor.matmul(
                            py[b][:], lhsT=hT[i % 2][:, fc, s * 128:(s + 1) * 128],
                            rhs=Wd[:, fc, half * 512:(half + 1) * 512],
                            start=(fc == 0), stop=(fc == NFC - 1)),
                            reads=[t_hT[i % 2], t_wd[fc]], writes=[t_py[b]])
                    cx.dve(lambda: nc.vector.scalar_tensor_tensor(
                        out=xt[:, s, half * 512:(half + 1) * 512], in0=py[b][:], scalar=0.5,
                        in1=xt[:, s, half * 512:(half + 1) * 512], op0=ALU.mult, op1=ALU.add),
                        reads=[t_py[b], tx], writes=[tx])

        def finish(i):
            xt = xb[i % 2]
            tx = t_xb[i % 2]
            if final_gain is not None:
                for s in range(NS):
                    cx.act(lambda s=s: nc.scalar.activation(out=junk[:], in_=xt[:, s, :], func=AF.Square,
                                                            accum_out=ss[:, 2 + s:3 + s]),
                           reads=[tx], writes=[t_junk, t_ss])
                cx.act(lambda: nc.scalar.activation(out=rstd[:, 2:2 + NS], in_=ss[:, 2:2 + NS], func=AF.Sqrt,
                                                    bias=epsb[:, 0:1], scale=1.0 / D),
                       reads=[t_ss, t_eps], writes=[t_rstd])
                cx.dve(lambda: nc.vector.reciprocal(out=rstd[:, 2:2 + NS], in_=rstd[:, 2:2 + NS]),
                       reads=[t_rstd], writes=[t_rstd])
                for s in range(NS):
                    cx.dve(lambda s=s: nc.vector.scalar_tensor_tensor(
                        out=xt[:, s, :], in0=xt[:, s, :], scalar=rstd[:, 2 + s:3 + s], in1=fg[:],
                        op0=ALU.mult, op1=ALU.mult), reads=[tx, t_rstd, t_fg], writes=[tx])
            cx.dma_sp(tok_view(dst, i * TT, TT), xt[:], reads=[tx])

        load_x(0)
        load_w()
        norm(0)
        transp(0)
        for i in range(ntiles):
            if i + 1 < ntiles:
                load_x(i + 1)
            for fc in range(NFC):
                gate_up(i, fc)
                if fc == 12 and i + 1 < ntiles:
                    norm(i + 1)
            if i + 1 < ntiles:
                transp(i + 1)
            down(i)
            finish(i)
        cx.phase_end()


class NormT:
    def __init__(self, cx, es, TT, nbuf_xnT=2):
        nc = cx.nc
        self.cx = cx
        self.TT = TT
        self.NS = TT // 128
        pf = _pfx()
        sb = lambda n, s, d: es.enter_context(nc.sbuf_tensor(pf + n, s, d))
        ps = lambda n, s, d: es.enter_context(nc.psum_tensor(pf + n, s, d))
        self.xn = sb("xn", [128, self.NS, D], BF16)
        self.t_xn = Trk()
        self.junk = sb("junk", [128, D], BF16)
        self.t_junk = Trk()
        self.xnT = [sb(f"xnT{i}", [128, NKC, TT], BF16) for i in range(nbuf_xnT)]
        self.t_xnT = [Trk() for _ in range(nbuf_xnT)]
        self.ss = sb("ss", [128, 4], F32)
        self.t_ss = Trk()
        self.rstd = sb("rstd", [128, 4], F32)
        self.t_rstd = Trk()
        self.gcol = sb("gcol", [128, NKC], F32)
        self.t_gcol = Trk()
        self.ident = sb("ident", [128, 128], BF16)
        self.t_ident = Trk()
        self.epsb = sb("epsb", [128, 1], F32)
        self.t_eps = Trk()
        self.pT = ps("pT", [128, NKC, TT], BF16)
        self.t_pT = Trk()

    def init(self, gcol_ap):
        cx, nc = self.cx, self.cx.nc
        ident, epsb = self.ident, self.epsb
        cx.pool(lambda: nc.gpsimd.memset(ident[:], 0.0), writes=[self.t_ident])
        cx.pool(lambda: nc.gpsimd.affine_select(
            out=ident[:], in_=ident[:], pattern=[[-1, 128]], compare_op=ALU.not_equal,
            fill=1.0, base=0, channel_multiplier=1), reads=[self.t_ident], writes=[self.t_ident])
        cx.pool(lambda: nc.gpsimd.memset(epsb[:], RMS_EPS), writes=[self.t_eps])
        cx.dma_sp(self.gcol[:], gcol_ap, writes=[self.t_gcol])

    def norm(self, xt, tx):
        cx, nc = self.cx, self.cx.nc
        NS = self.NS
        ss, rstd, junk, xn, epsb = self.ss, self.rstd, self.junk, self.xn, self.epsb
        for s in range(NS):
            cx.act(lambda s=s: nc.scalar.activation(out=junk[:], in_=xt[:, s, :], func=AF.Square,
                                                    accum_out=ss[:, s:s + 1]),
                   reads=[tx], writes=[self.t_junk, self.t_ss])
        cx.act(lambda: nc.scalar.activation(out=rstd[:, 0:NS], in_=ss[:, 0:NS], func=AF.Sqrt,
                                            bias=epsb[:, 0:1], scale=1.0 / D),
               reads=[self.t_ss, self.t_eps], writes=[self.t_rstd])
        cx.dve(lambda: nc.vector.reciprocal(out=rstd[:, 0:NS], in_=rstd[:, 0:NS]),
               reads=[self.t_rstd], writes=[self.t_rstd])
        for s in range(NS):
            cx.dve(lambda s=s: nc.vector.tensor_scalar(out=xn[:, s, :], in0=xt[:, s, :],
                                                       scalar1=rstd[:, s:s + 1], scalar2=None,
                                                       op0=ALU.mult),
                   reads=[tx, self.t_rstd], writes=[self.t_xn])

    def transp(self, j):
        cx, nc = self.cx, self.cx.nc
        pT, xn, ident, gcol, TT = self.pT, self.xn, self.ident, self.gcol, self.TT
        for s in range(self.NS):
            for kc in range(NKC):
                cx.pe(lambda s=s, kc=kc: nc.tensor.transpose(
                    out=pT[:, kc, s * 128:(s + 1) * 128], in_=xn[:, s, kc * 128:(kc + 1) * 128],
                    identity=ident[:]), reads=[self.t_xn, self.t_ident], writes=[self.t_pT])
        cx.dve(lambda: nc.vector.tensor_tensor(
            out=self.xnT[j][:], in0=pT[:], in1=gcol[:, :].unsqueeze(2).to_broadcast([128, NKC, TT]),
            op=ALU.mult), reads=[self.t_pT, self.t_gcol], writes=[self.t_xnT[j]])


def conv_phase(cx, src, dst, ntok, T, w_in, w_out, small_ap, b_out_ap, gcol_ap):
    nc = cx.nc
    TT = 256
    NS = TT // 128
    ntiles = ntok // TT
    tps = T // TT
    HALO = CONVW - 1
    with contextlib.ExitStack() as es:
        pf = _pfx()
        sb = lambda n, s, d: es.enter_context(nc.sbuf_tensor(pf + n, s, d))
        ps = lambda n, s, d: es.enter_context(nc.psum_tensor(pf + n, s, d))
        nt = NormT(cx, es, TT)
        Win = sb("Win", [128, 16, NKC, 128], BF16)
        t_win = [Trk() for _ in range(16)]
        Wout = sb("Wout", [128, NKC, D], BF16)
        t_wout = Trk()
        Dg = sb("Dg", [128, NKC, CONVW, 128], BF16)
        t_dg = Trk()
        small = sb("small", [128, 288], F32)
        t_small = Trk()
        bout = sb("bout", [128, D], F32)
        t_bout = Trk()
        identf = sb("identf", [128, 128], F32)
        t_identf = Trk()
        ones = sb("ones", [128, 128], F32)
        t_ones = Trk()
        lneps = sb("lneps", [128, 1], F32)
        t_lneps = Trk()
        xb = [sb(f"xb{i}", [128, NS, D], F32) for i in range(2)]
        t_xb = [Trk(), Trk()]
        gT = [sb(f"gT{i}", [128, NKC, HALO + TT], BF16) for i in range(2)]
        t_gT = [Trk(), Trk()]
        sg = [sb(f"sg{i}", [128, TT], F32) for i in range(2)]
        t_sg = [Trk(), Trk()]
        cT = sb("cT", [128, NKC, TT], F32)
        t_cT = Trk()
        sq = sb("sq", [128, NKC, TT], F32)
        t_sq = Trk()
        mean = sb("mean", [128, TT], F32)
        t_mean = Trk()
        var = sb("var", [128, TT], F32)
        t_var = Trk()
        aT = sb("aT", [128, NKC, TT], BF16)
        t_aT = Trk()
        pab = [ps(f"pab{i}", [128, 2, TT], F32) for i in range(2)]
        t_pab = [Trk(), Trk()]
        pc = ps("pc", [128, 2, 512], F32)
        t_pc = [Trk(), Trk()]
        pst = ps("pst", [128, 2, TT], F32)
        t_pst = Trk()
        py = [ps("py0", [128, 512], F32)] * 2
        t_py = [Trk()] * 2

        nt.init(gcol_ap)
        cx.dma_sp(small[:], small_ap, writes=[t_small])
        cx.dma_sp(bout[:], b_out_ap.partition_broadcast(128), writes=[t_bout])
        for oc in range(16):
            cx.dma_pl(Win[:, oc], w_in[:, oc], writes=[t_win[oc]])
        for kc in range(0, NKC, 2):
            cx.dma_pl(Wout[:, kc:kc + 2], w_out[:, kc:kc + 2], writes=[t_wout])
        cx.pool(lambda: nc.gpsimd.memset(identf[:], 0.0), writes=[t_identf])
        cx.pool(lambda: nc.gpsimd.affine_select(
            out=identf[:], in_=identf[:], pattern=[[-1, 128]], compare_op=ALU.not_equal,
            fill=1.0, base=0, channel_multiplier=1), reads=[t_identf], writes=[t_identf])
        cx.pool(lambda: nc.gpsimd.memset(ones[:], 1.0), writes=[t_ones])
        cx.pool(lambda: nc.gpsimd.memset(lneps[:], LN_EPS), writes=[t_lneps])
        for cc in range(NKC):
            cx.dve(lambda cc=cc: nc.vector.tensor_tensor(
                out=Dg[:, cc], in0=identf[:, :].unsqueeze(1).to_broadcast([128, CONVW, 128]),
                in1=small[:, 40 + cc * CONVW:40 + (cc + 1) * CONVW].unsqueeze(2).to_broadcast([128, CONVW, 128]),
                op=ALU.mult), reads=[t_identf, t_small], writes=[t_dg])

        def load_x(i):
            cx.dma_sp(xb[i % 2][:], tok_view(src, i * TT, TT), writes=[t_xb[i % 2]])

        def glu(i):
            j = i % 2
            xT = nt.xnT[j]
            g = gT[j]
            if i % tps == 0:
                cx.pool(lambda: nc.gpsimd.memset(g[:, :, 0:HALO], 0.0), writes=[t_gT[j]])
            else:
                cx.pool(lambda: nc.gpsimd.tensor_copy(out=g[:, :, 0:HALO], in_=gT[1 - j][:, :, TT:TT + HALO]),
                        reads=[t_gT[1 - j]], writes=[t_gT[j]])
            for cc in range(NKC):
                b = cc % 2
                for which in range(2):
                    oc = which * 8 + cc
                    for kc in range(NKC):
                        cx.pe(lambda which=which, oc=oc, kc=kc: nc.tensor.matmul(
                            pab[b][:, which, :], lhsT=Win[:, oc, kc, :], rhs=xT[:, kc, :],
                            start=(kc == 0), stop=(kc == NKC - 1)),
                            reads=[t_win[oc], nt.t_xnT[j]], writes=[t_pab[b]])
                cx.act(lambda cc=cc: nc.scalar.activation(out=sg[b][:], in_=pab[b][:, 1, :], func=AF.Sigmoid,
                                                          bias=small[:, 8 + cc:9 + cc]),
                       reads=[t_pab[b], t_small], writes=[t_sg[b]])
                cx.dve(lambda cc=cc: nc.vector.scalar_tensor_tensor(
                    out=g[:, cc, HALO:HALO + TT], in0=pab[b][:, 0, :], scalar=small[:, cc:cc + 1],
                    in1=sg[b][:], op0=ALU.add, op1=ALU.mult),
                    reads=[t_pab[b], t_sg[b], t_small], writes=[t_gT[j]])

        def dwconv(i):
            j = i % 2
            g = gT[j]
            for cc in range(NKC):
                b = cc % 2
                for k in range(CONVW):
                    cx.pe(lambda cc=cc, k=k: nc.tensor.matmul(
                        pc[:, b, 0:TT], lhsT=Dg[:, cc, k, :], rhs=g[:, cc, k:k + TT],
                        start=(k == 0), stop=(k == CONVW - 1)),
                        reads=[t_dg, t_gT[j]], writes=[t_pc[b]])
                cx.act(lambda cc=cc: nc.scalar.activation(out=cT[:, cc, :], in_=pc[:, b, 0:TT], func=AF.Identity,
                                                          bias=small[:, 16 + cc:17 + cc]),
                       reads=[t_pc[b], t_small], writes=[t_cT])
                cx.pool(lambda cc=cc: nc.gpsimd.tensor_tensor(out=sq[:, cc, :], in0=cT[:, cc, :], in1=cT[:, cc, :],
                                                              op=ALU.mult),
                        reads=[t_cT], writes=[t_sq])

        def lnorm_stats(i):
            for which, (buf, tb) in enumerate(((cT, t_cT), (sq, t_sq))):
                for cc in range(NKC):
                    cx.pe(lambda which=which, buf=buf, cc=cc: nc.tensor.matmul(
                        pst[:, which, :], lhsT=ones[:], rhs=buf[:, cc, :],
                        start=(cc == 0), stop=(cc == NKC - 1)),
                        reads=[t_ones, tb], writes=[t_pst])

        def lnorm_chain(i):
            cx.dve(lambda: nc.vector.tensor_scalar(out=mean[:], in0=pst[:, 0, :], scalar1=1.0 / D, scalar2=None,
                                                   op0=ALU.mult), reads=[t_pst], writes=[t_mean])
            cx.dve(lambda: nc.vector.tensor_tensor(out=var[:], in0=mean[:], in1=mean[:], op=ALU.mult),
                   reads=[t_mean], writes=[t_var])
            cx.dve(lambda: nc.vector.scalar_tensor_tensor(out=var[:], in0=pst[:, 1, :], scalar=1.0 / D, in1=var[:],
                                                          op0=ALU.mult, op1=ALU.subtract),
                   reads=[t_pst, t_var], writes=[t_var])
            cx.act(lambda: nc.scalar.activation(out=var[:], in_=var[:], func=AF.Sqrt, bias=lneps[:, 0:1]),
                   reads=[t_var, t_lneps], writes=[t_var])
            cx.dve(lambda: nc.vector.reciprocal(out=var[:], in_=var[:]), reads=[t_var], writes=[t_var])
            cx.dve(lambda: nc.vector.tensor_tensor(
                out=cT[:], in0=cT[:], in1=mean[:, :].unsqueeze(1).to_broadcast([128, NKC, TT]), op=ALU.subtract),
                reads=[t_cT, t_mean], writes=[t_cT])
            cx.dve(lambda: nc.vector.tensor_tensor(
                out=cT[:], in0=cT[:], in1=var[:, :].unsqueeze(1).to_broadcast([128, NKC, TT]), op=ALU.mult),
                reads=[t_cT, t_var], writes=[t_cT])
            for cc in range(NKC):
                cx.act(lambda cc=cc: nc.scalar.activation(out=aT[:, cc, :], in_=cT[:, cc, :], func=AF.Silu,
                                                          scale=small[:, 24 + cc:25 + cc],
                                                          bias=small[:, 32 + cc:33 + cc]),
                       reads=[t_cT, t_small], writes=[t_aT])

        def outproj(i):
            xt = xb[i % 2]
            tx = t_xb[i % 2]
            for s in range(NS):
                cx.pool(lambda s=s: nc.gpsimd.tensor_tensor(out=xt[:, s, :], in0=xt[:, s, :], in1=bout[:], op=ALU.add),
                        reads=[tx, t_bout], writes=[tx])
            g = 0
            for s in range(NS):
                for half in range(2):
                    b = g % 2
                    g += 1
                    for kc in range(NKC):
                        cx.pe(lambda kc=kc: nc.tensor.matmul(
                            py[b][:], lhsT=aT[:, kc, s * 128:(s + 1) * 128],
                            rhs=Wout[:, kc, half * 512:(half + 1) * 512],
                            start=(kc == 0), stop=(kc == NKC - 1)),
                            reads=[t_aT, t_wout], writes=[t_py[b]])
                    cx.dve(lambda: nc.vector.tensor_tensor(
                        out=xt[:, s, half * 512:(half + 1) * 512], in0=py[b][:],
                        in1=xt[:, s, half * 512:(half + 1) * 512], op=ALU.add),
                        reads=[t_py[b], tx], writes=[tx])
            cx.dma_sp(tok_view(dst, i * TT, TT), xt[:], reads=[tx])

        load_x(0)
        nt.norm(xb[0], t_xb[0])
        nt.transp(0)
        glu(0)
        for i in range(ntiles):
            nxt = i + 1 < ntiles
            if nxt:
                load_x(i + 1)
            dwconv(i)
            if nxt:
                nt.norm(xb[(i + 1) % 2], t_xb[(i + 1) % 2])
                nt.transp((i + 1) % 2)
            lnorm_stats(i)
            if nxt:
                glu(i + 1)
            lnorm_chain(i)
            outproj(i)
        cx.phase_end()


C0 = math.exp(-0.5)
RDT = BF16


RW_STOP = [None]


def rwkv_phase(cx, src, dst, ntok, T, w_rkvo, w1, a1, g1, w2, a2, g2, vecs, mu_ap, gcol_ap, dbg=None):
    nc = cx.nc
    L = 64
    nchunks = T // L
    assert ntok == 2 * T
    with contextlib.ExitStack() as es:
        pf = _pfx()
        sb = lambda n, s, d: es.enter_context(nc.sbuf_tensor(pf + n, s, d))
        ps = lambda n, s, d: es.enter_context(nc.psum_tensor(pf + n, s, d))

        Wrkvo = sb("Wrkvo", [128, 4, NKC, D], BF16)
        t_W = [Trk() for _ in range(4)]
        W1 = sb("W1", [128, NKC, 64], BF16)
        A1 = sb("A1", [128, NKC, 64], BF16)
        G1 = sb("G1", [128, NKC, 160], BF16)
        W2 = sb("W2", [64, D], BF16)
        A2 = sb("A2", [64, D], BF16)
        G2a = sb("G2a", [128, D], BF16)
        G2b = sb("G2b", [32, D], BF16)
        t_lw = Trk()
        VB = sb("VB", [128, 7, D], F32)
        t_vb = Trk()
        mu = sb("mu", [128, 6, NKC], F32)
        gcol = sb("gcol", [128, NKC], F32)
        t_small = Trk()
        identf = sb("identf", [128, 128], F32)
        identb = sb("identb", [128, 128], BF16)
        triI = sb("triI", [128, 128], F32)
        triLt = sb("triLt", [128, 128], F32)
        triGt = sb("triGt", [128, 128], F32)
        bsel = sb("bsel", [128, 2], F32)
        ones1 = sb("ones1", [128, 1], F32)
        mST = sb("mST", [128, 64], F32)
        mIT = sb("mIT", [128, 64], F32)
        mS = sb("mS", [128, 64], F32)
        epsr = sb("epsr", [128, 1], F32)
        epsg = sb("epsg", [128, 1], F32)
        t_c = Trk()

        XT = sb("XT", [128, D], F32); t_XT = Trk()
        Rt = sb("Rt", [128, D], F32); t_R = Trk()
        Kt = sb("Kt", [128, D], F32); t_K = Trk()
        Vt = sb("Vt", [128, D], F32); t_V = Trk()
        SW = sb("SW", [128, D], F32); t_SW = Trk()
        AS = sb("AS", [128, D], F32); t_AS = Trk()
        KK = sb("KK", [128, D], F32); t_KK = Trk()
        X1 = sb("X1", [128, D], F32); t_X1 = Trk()
        X2 = sb("X2", [128, D], F32); t_X2 = Trk()
        RB = sb("RB", [128, D], RDT); t_RB = Trk()
        AB = sb("AB", [128, D], RDT); t_AB = Trk()
        KB = sb("KB", [128, D], RDT); t_KB = Trk()
        BB = sb("BB", [128, D], RDT); t_BB = Trk()
        KH = sb("KH", [128, D], RDT); t_KH = Trk()
        BH = sb("BH", [128, D], RDT); t_BH = Trk()
        Vb = sb("Vb", [128, D], RDT); t_Vb = Trk()
        MB = sb("MB", [128, H, HS], RDT); t_MB = Trk()
        xnT = sb("xnT", [128, NKC, 2, L + 1], F32); t_xnT = Trk()
        mix = [sb(f"mix{i}", [128, NKC, 128], BF16) for i in range(2)]
        t_mix = [[Trk() for _ in range(NKC)] for _ in range(2)]
        thw = sb("thw", [64, 128], BF16); t_thw = Trk()
        haT = sb("haT", [64, 128], BF16); t_haT = Trk()
        sgT_ = [sb(f"sgT{i}", [128, 128], BF16) for i in range(2)]
        sgT2_ = [sb(f"sgT2{i}", [32, 128], BF16) for i in range(2)]
        t_sgT_ = [Trk(), Trk()]
        FT = sb("FT", [128, H, 4, L], RDT); t_FT = Trk()
        AabT = sb("AabT", [128, H, L], RDT); t_AabT = [Trk(), Trk()]
        ArbT = sb("ArbT", [128, H, L], RDT); t_ArbT = [Trk(), Trk()]
        AakT = sb("AakT", [128, H, L], RDT); t_AakT = [Trk(), Trk()]
        ArkT = sb("ArkT", [128, H, L], RDT); t_ArkT = [Trk(), Trk()]
        GX = sb("GX", [128, H, 2 * L], RDT); t_GX = [Trk(), Trk()]
        M = sb("M", [128, H, HS], F32); t_M = Trk()
        WL = sb("WL", [128, H], F32); t_WL = Trk()
        st = sb("st", [128, 8, H], F32); t_st = Trk()
        sc = sb("sc", [128, 8], F32); t_sc = Trk()
        ygT = sb("ygT", [128, NKC, 128], BF16); t_ygT = Trk()
        YG = sb("YG", [128, D], BF16); t_YG = Trk()

        PS = ps("PS", [128, 8, 512], F32)
        t_bank = [Trk() for _ in range(8)]

        def bank(i):
            return PS[:, i, :]

        for q in range(4):
            for kc in range(0, NKC, 2):
                cx.dma_pl(Wrkvo[:, q, kc:kc + 2], w_rkvo[q, :, kc:kc + 2], writes=[t_W[q]])
        cx.dma_pl(W1[:], w1, writes=[t_lw])
        cx.dma_pl(A1[:], a1, writes=[t_lw])
        cx.dma_pl(G1[:], g1, writes=[t_lw])
        cx.dma_pl(W2[:], w2, writes=[t_lw])
        cx.dma_pl(A2[:], a2, writes=[t_lw])
        cx.dma_pl(G2a[:], g2[0:128, :], writes=[t_lw])
        cx.dma_pl(G2b[:], g2[128:160, :], writes=[t_lw])
        for i in range(7):
            cx.dma_sp(VB[:, i, :], vecs[i].partition_broadcast(128), writes=[t_vb])
        cx.dma_sp(mu[:], mu_ap, writes=[t_small])
        cx.dma_sp(gcol[:], gcol_ap, writes=[t_small])

        P = cx.pool
        g = nc.gpsimd

        def ident_like(t):
            P(lambda: g.memset(t[:], 0.0), writes=[t_c])
            P(lambda: g.affine_select(out=t[:], in_=t[:], pattern=[[-1, 128]], compare_op=ALU.not_equal,
                                      fill=1.0, base=0, channel_multiplier=1), reads=[t_c], writes=[t_c])
        ident_like(identf)
        ident_like(identb)
        for t in (triI, triLt, triGt):
            P(lambda t=t: g.memset(t[:], 1.0), writes=[t_c])
        P(lambda: g.affine_select(out=triI[:], in_=triI[:], pattern=[[1, 128]], compare_op=ALU.is_ge,
                                  fill=0.0, base=0, channel_multiplier=-1), reads=[t_c], writes=[t_c])
        P(lambda: g.affine_select(out=triLt[:], in_=triLt[:], pattern=[[1, 128]], compare_op=ALU.is_gt,
                                  fill=0.0, base=0, channel_multiplier=-1), reads=[t_c], writes=[t_c])
        P(lambda: g.affine_select(out=triGt[:], in_=triGt[:], pattern=[[-1, 128]], compare_op=ALU.is_gt,
                                  fill=0.0, base=0, channel_multiplier=1), reads=[t_c], writes=[t_c])
        P(lambda: g.memset(triI[0:64, 64:128], 0.0), writes=[t_c])
        P(lambda: g.memset(triLt[0:64, 64:128], 0.0), writes=[t_c])
        P(lambda: g.memset(triGt[64:128, 0:64], 0.0), writes=[t_c])
        P(lambda: g.memset(ones1[:], 1.0), writes=[t_c])
        P(lambda: g.memset(bsel[:], 0.0), writes=[t_c])
        P(lambda: g.memset(bsel[0:64, 0:1], 1.0), writes=[t_c])
        P(lambda: g.memset(bsel[64:128, 1:2], 1.0), writes=[t_c])
        for t in (mST, mIT, mS):
            P(lambda t=t: g.memset(t[:], 1.0), writes=[t_c])
        for hb in range(2):
            sl = slice(64 * hb, 64 * hb + 64)
            P(lambda sl=sl: g.affine_select(out=mST[sl, :], in_=mST[sl, :], pattern=[[1, 64]], compare_op=ALU.is_gt,
                                            fill=0.0, base=0, channel_multiplier=-1), reads=[t_c], writes=[t_c])
            P(lambda sl=sl: g.affine_select(out=mIT[sl, :], in_=mIT[sl, :], pattern=[[1, 64]], compare_op=ALU.is_ge,
                                            fill=0.0, base=0, channel_multiplier=-1), reads=[t_c], writes=[t_c])
            P(lambda sl=sl: g.affine_select(out=mS[sl, :], in_=mS[sl, :], pattern=[[-1, 64]], compare_op=ALU.is_gt,
                                            fill=0.0, base=0, channel_multiplier=1), reads=[t_c], writes=[t_c])
        P(lambda: g.memset(epsr[:], RMS_EPS), writes=[t_c])
        P(lambda: g.memset(epsg[:], GN_EPS), writes=[t_c])
        P(lambda: g.memset(M[:], 0.0), writes=[t_M])
        P(lambda: g.memset(MB[:], 0.0), writes=[t_MB])

        V3 = lambda t: t[:, :].rearrange("p (h i) -> p h i", i=HS)
        bc_h = lambda col: col.unsqueeze(2).to_broadcast([128, H, HS])
        evac_flip = [0]

        def evac_copy(out, in_, reads, writes):
            evac_flip[0] ^= 1
            if evac_flip[0]:
                cx.act(lambda: nc.scalar.copy(out=out, in_=in_), reads=reads, writes=writes)
            else:
                cx.dve(lambda: nc.vector.tensor_copy(out=out, in_=in_), reads=reads, writes=writes)

        def proj512(lhs_fn, rhs_fn, nk, bk, reads):
            for k in range(nk):
                cx.pe(lambda k=k: nc.tensor.matmul(bank(bk), lhsT=lhs_fn(k), rhs=rhs_fn(k),
                                                   start=(k == 0), stop=(k == nk - 1)),
                      reads=(reads(k) if callable(reads) else reads), writes=[t_bank[bk]])

        def make_mix(m, j):
            XX = XT[:, :].rearrange("p (k b s) -> p k b s", k=NKC, b=2)
            if m % 2 == 1:
                T4 = X2[:, :].rearrange("p (k b s) -> p k b s", k=NKC, b=2)
                P(lambda: g.tensor_tensor(out=T4, in0=XX,
                                          in1=mu[:, m, :].unsqueeze(2).unsqueeze(3).to_broadcast([128, NKC, 2, L]),
                                          op=ALU.mult), reads=[t_XT, t_small], writes=[t_X2])
                P(lambda: g.tensor_tensor(out=mix[j][:, :, :].rearrange("p k (b s) -> p k b s", b=2), in0=T4,
                                          in1=xnT[:, :, :, 1:L + 1], op=ALU.add),
                  reads=[t_X2, t_xnT], writes=t_mix[j])
                return
            for kc in range(NKC):
                cx.dve(lambda kc=kc: nc.vector.scalar_tensor_tensor(
                    out=mix[j][:, kc, :].rearrange("p (b s) -> p b s", b=2), in0=XX[:, kc], scalar=mu[:, m, kc:kc + 1],
                    in1=xnT[:, kc, :, 1:L + 1], op0=ALU.mult, op1=ALU.add),
                    reads=[t_XT, t_xnT, t_small], writes=[t_mix[j][kc]])

        _order = ['A', 'B', 'C', 'D', 'E1', 'E2', 'E3', 'E4', 'E5', 'F']
        en = lambda s: RW_STOP[0] is None or _order.index(s) <= _order.index(RW_STOP[0])

        def stage_AB(c):
            for b in range(2):
                cx.dma_sp(XT[64 * b:64 * b + 64, :], src[b * T + c * L:b * T + (c + 1) * L, :], writes=[t_XT])
            cx.act(lambda: nc.scalar.activation(out=YG[:], in_=XT[:], func=AF.Square, accum_out=sc[:, 0:1]),
                   reads=[t_XT], writes=[t_YG, t_sc])
            cx.act(lambda: nc.scalar.activation(out=sc[:, 1:2], in_=sc[:, 0:1], func=AF.Sqrt, bias=epsr[:, 0:1],
                                                scale=1.0 / D), reads=[t_sc, t_c], writes=[t_sc])
            cx.dve(lambda: nc.vector.reciprocal(out=sc[:, 1:2], in_=sc[:, 1:2]), reads=[t_sc], writes=[t_sc])
            cx.dve(lambda: nc.vector.tensor_scalar(out=XT[:], in0=XT[:], scalar1=sc[:, 1:2], scalar2=None,
                                                   op0=ALU.mult), reads=[t_XT, t_sc], writes=[t_XT])
            if c == 0:
                P(lambda: g.memset(xnT[:, :, :, 0:1], 0.0), writes=[t_xnT])
            else:
                P(lambda: g.tensor_copy(out=xnT[:, :, :, 0:1], in_=xnT[:, :, :, L:L + 1]), reads=[t_xnT], writes=[t_xnT])
            PTv = PS[:, 0:2, :].rearrange("p a (k t) -> p (a k) t", t=128)
            for kc in range(NKC):
                cx.pe(lambda kc=kc: nc.tensor.transpose(out=PTv[:, kc, :], in_=XT[:, kc * 128:(kc + 1) * 128],
                                                        identity=identf[:]),
                      reads=[t_XT, t_c], writes=[t_bank[kc // 4]])
            cx.dve(lambda: nc.vector.tensor_tensor(
                out=xnT[:, :, :, 1:L + 1], in0=PTv.rearrange("p k (b s) -> p k b s", b=2),
                in1=gcol[:, :].unsqueeze(2).unsqueeze(3).to_broadcast([128, NKC, 2, L]), op=ALU.mult),
                reads=[t_bank[0], t_bank[1], t_small], writes=[t_xnT])
            cx.dve(lambda: nc.vector.tensor_tensor(
                out=XT[:, :].rearrange("p (k b s) -> p k b s", k=NKC, b=2), in0=xnT[:, :, :, 0:L],
                in1=xnT[:, :, :, 1:L + 1], op=ALU.subtract), reads=[t_xnT], writes=[t_XT])

            if en('B'):
                pass
                def rkv(m, q, dstt, tdst, j):
                    make_mix(m, j)
                    for half in range(2):
                        bk = 2 + half
                        proj512(lambda k: mix[j][:, k, :], lambda k: Wrkvo[:, q, k, half * 512:(half + 1) * 512], NKC, bk,
                                lambda k: [t_mix[j][k], t_W[q]])
                        evac_copy(dstt[:, half * 512:(half + 1) * 512], bank(bk), [t_bank[bk]], [tdst])
                rkv(0, 0, Rt, t_R, 0)
                rkv(2, 1, Kt, t_K, 1)
                rkv(3, 2, Vt, t_V, 0)
                P(lambda: g.tensor_copy(out=Vb[:], in_=Vt[:]), reads=[t_V], writes=[t_Vb])
                make_mix(1, 1)
                for k in range(NKC):
                    cx.pe(lambda k=k: nc.tensor.matmul(PS[0:64, 4, 0:128], lhsT=W1[:, k, :], rhs=mix[1][:, k, :],
                                                       start=(k == 0), stop=(k == NKC - 1)),
                          reads=[t_mix[1][k], t_lw], writes=[t_bank[4]])
                cx.act(lambda: nc.scalar.activation(out=thw[:], in_=PS[0:64, 4, 0:128], func=AF.Tanh),
                       reads=[t_bank[4]], writes=[t_thw])
                for half in range(2):
                    bk = 2 + half
                    cx.pe(lambda: nc.tensor.matmul(bank(bk), lhsT=thw[:], rhs=W2[:, half * 512:(half + 1) * 512],
                                                   start=True, stop=True), reads=[t_thw, t_lw], writes=[t_bank[bk]])
                    cx.dve(lambda: nc.vector.tensor_tensor(out=SW[:, half * 512:(half + 1) * 512], in0=bank(bk),
                                                           in1=VB[:, 0, half * 512:(half + 1) * 512], op=ALU.add),
                           reads=[t_bank[bk], t_vb], writes=[t_SW])
                cx.act(lambda: nc.scalar.activation(out=SW[:], in_=SW[:], func=AF.Sigmoid), reads=[t_SW], writes=[t_SW])
                make_mix(4, 0)
                for k in range(NKC):
                    cx.pe(lambda k=k: nc.tensor.matmul(PS[0:64, 5, 0:128], lhsT=A1[:, k, :], rhs=mix[0][:, k, :],
                                                       start=(k == 0), stop=(k == NKC - 1)),
                          reads=[t_mix[0][k], t_lw], writes=[t_bank[5]])
                cx.act(lambda: nc.scalar.copy(out=haT[:], in_=PS[0:64, 5, 0:128]), reads=[t_bank[5]], writes=[t_haT])
                for half in range(2):
                    bk = 2 + half
                    cx.pe(lambda: nc.tensor.matmul(bank(bk), lhsT=haT[:], rhs=A2[:, half * 512:(half + 1) * 512],
                                                   start=True, stop=True), reads=[t_haT, t_lw], writes=[t_bank[bk]])
                    cx.dve(lambda: nc.vector.tensor_tensor(out=AS[:, half * 512:(half + 1) * 512], in0=bank(bk),
                                                           in1=VB[:, 1, half * 512:(half + 1) * 512], op=ALU.add),
                           reads=[t_bank[bk], t_vb], writes=[t_AS])
                cx.act(lambda: nc.scalar.activation(out=AS[:], in_=AS[:], func=AF.Sigmoid), reads=[t_AS], writes=[t_AS])
                make_mix(5, 1)
                for k in range(NKC):
                    cx.pe(lambda k=k: nc.tensor.matmul(PS[:, 4, 0:128], lhsT=G1[:, k, 0:128], rhs=mix[1][:, k, :],
                                                       start=(k == 0), stop=(k == NKC - 1)),
                          reads=[t_mix[1][k], t_lw], writes=[t_bank[4]])
                for k in range(NKC):
                    cx.pe(lambda k=k: nc.tensor.matmul(PS[0:32, 5, 0:128], lhsT=G1[:, k, 128:160], rhs=mix[1][:, k, :],
                                                       start=(k == 0), stop=(k == NKC - 1)),
                          reads=[t_mix[1][k], t_lw], writes=[t_bank[5]])
                sgT, sgT2, t_sgT = sgT_[c % 2], sgT2_[c % 2], t_sgT_[c % 2]
                cx.act(lambda: nc.scalar.activation(out=sgT[:], in_=PS[:, 4, 0:128], func=AF.Sigmoid),
                       reads=[t_bank[4]], writes=[t_sgT])
                cx.act(lambda: nc.scalar.activation(out=sgT2[:], in_=PS[0:32, 5, 0:128], func=AF.Sigmoid),
                       reads=[t_bank[5]], writes=[t_sgT])

        def stage_CDE(c):
            if en('C'):
                pass
                P(lambda: g.tensor_tensor(out=KK[:], in0=Kt[:], in1=VB[:, 2, :], op=ALU.mult),
                  reads=[t_K, t_vb], writes=[t_KK])
                cx.dve(lambda: nc.vector.tensor_tensor(out=XT[:], in0=KK[:], in1=KK[:], op=ALU.mult),
                       reads=[t_KK], writes=[t_XT])
                cx.dve(lambda: nc.vector.tensor_reduce(out=st[:, 0, :], in_=V3(XT), axis=AX.X, op=ALU.add),
                       reads=[t_XT], writes=[t_st])
                cx.act(lambda: nc.scalar.activation(out=st[:, 0, :], in_=st[:, 0, :], func=AF.Sqrt),
                       reads=[t_st], writes=[t_st])
                cx.dve(lambda: nc.vector.tensor_scalar_max(out=st[:, 0, :], in0=st[:, 0, :], scalar1=1e-12),
                       reads=[t_st], writes=[t_st])
                cx.dve(lambda: nc.vector.reciprocal(out=st[:, 0, :], in_=st[:, 0, :]), reads=[t_st], writes=[t_st])
                cx.dve(lambda: nc.vector.tensor_tensor(out=V3(KK), in0=V3(KK), in1=bc_h(st[:, 0, :]), op=ALU.mult),
                       reads=[t_KK, t_st], writes=[t_KK])
                cx.dve(lambda: nc.vector.scalar_tensor_tensor(out=XT[:], in0=AS[:], scalar=-1.0, in1=VB[:, 3, :],
                                                              op0=ALU.add, op1=ALU.mult),
                       reads=[t_AS, t_vb], writes=[t_XT])
                cx.dve(lambda: nc.vector.scalar_tensor_tensor(out=Kt[:], in0=XT[:], scalar=1.0, in1=Kt[:],
                                                              op0=ALU.add, op1=ALU.mult),
                       reads=[t_XT, t_K], writes=[t_K])
                P(lambda: g.tensor_tensor(out=AS[:], in0=AS[:], in1=KK[:], op=ALU.mult), reads=[t_AS, t_KK], writes=[t_AS])
                P(lambda: g.tensor_tensor(out=XT[:], in0=Rt[:], in1=Kt[:], op=ALU.mult), reads=[t_R, t_K], writes=[t_XT])
                P(lambda: g.tensor_tensor(out=XT[:], in0=XT[:], in1=VB[:, 4, :], op=ALU.mult), reads=[t_XT, t_vb], writes=[t_XT])
                cx.dve(lambda: nc.vector.tensor_reduce(out=st[:, 1, :], in_=V3(XT), axis=AX.X, op=ALU.add),
                       reads=[t_XT], writes=[t_st])
                WLp = PS[:, 6, 0:H]
                for h in range(H):
                    for b in range(2):
                        cs = slice(64 * b, 64 * b + 64)
                        cx.pe(lambda: nc.tensor.matmul(WLp[cs, h:h + 1], lhsT=SW[cs, h * HS:(h + 1) * HS], rhs=ones1[cs, 0:1],
                                                       start=True, stop=True), reads=[t_SW, t_c], writes=[t_bank[6]])
                cx.act(lambda: nc.scalar.activation(out=WL[:], in_=WLp, func=AF.Exp, scale=-C0),
                       reads=[t_bank[6]], writes=[t_WL])
                for half in range(2):
                    hs = slice(half * 512, (half + 1) * 512)
                    bkI, bkE, bkT = (2, 3, 4) if half == 0 else (5, 6, 2)
                    cx.pe(lambda: nc.tensor.matmul(bank(bkI), lhsT=triI[:], rhs=SW[:, hs], start=True, stop=True),
                          reads=[t_SW, t_c], writes=[t_bank[bkI]])
                    cx.pe(lambda: nc.tensor.matmul(bank(bkE), lhsT=triLt[:], rhs=SW[:, hs], start=True, stop=True),
                          reads=[t_SW, t_c], writes=[t_bank[bkE]])
                    cx.pe(lambda: nc.tensor.matmul(bank(bkT), lhsT=triGt[:], rhs=SW[:, hs], start=True, stop=True),
                          reads=[t_SW, t_c], writes=[t_bank[bkT]])
                    tmps = ((XT, t_XT), (X1, t_X1), (X2, t_X2))
                    E, tE = tmps[(4 * half + 0) % 3]
                    cx.act(lambda: nc.scalar.activation(out=E[:, hs], in_=bank(bkI), func=AF.Exp, scale=-C0),
                           reads=[t_bank[bkI]], writes=[tE])
                    cx.dve(lambda: nc.vector.tensor_tensor(out=RB[:, hs], in0=Rt[:, hs], in1=E[:, hs], op=ALU.mult),
                           reads=[t_R, tE], writes=[t_RB])
                    E, tE = tmps[(4 * half + 1) % 3]
                    cx.act(lambda: nc.scalar.activation(out=E[:, hs], in_=bank(bkI), func=AF.Exp, scale=C0),
                           reads=[t_bank[bkI]], writes=[tE])
                    cx.dve(lambda: nc.vector.tensor_tensor(out=KB[:, hs], in0=Kt[:, hs], in1=E[:, hs], op=ALU.mult),
                           reads=[t_K, tE], writes=[t_KB])
                    P(lambda: g.tensor_tensor(out=BB[:, hs], in0=AS[:, hs], in1=E[:, hs], op=ALU.mult),
                      reads=[t_AS, tE], writes=[t_BB])
                    E, tE = tmps[(4 * half + 2) % 3]
                    cx.act(lambda: nc.scalar.activation(out=E[:, hs], in_=bank(bkE), func=AF.Exp, scale=-C0),
                           reads=[t_bank[bkE]], writes=[tE])
                    cx.dve(lambda: nc.vector.scalar_tensor_tensor(out=AB[:, hs], in0=KK[:, hs], scalar=-1.0, in1=E[:, hs],
                                                                  op0=ALU.mult, op1=ALU.mult),
                           reads=[t_KK, tE], writes=[t_AB])
                    E, tE = tmps[(4 * half + 3) % 3]
                    cx.act(lambda: nc.scalar.activation(out=E[:, hs], in_=bank(bkT), func=AF.Exp, scale=-C0),
                           reads=[t_bank[bkT]], writes=[tE])
                    cx.dve(lambda: nc.vector.tensor_tensor(out=KH[:, hs], in0=Kt[:, hs], in1=E[:, hs], op=ALU.mult),
                           reads=[t_K, tE], writes=[t_KH])
                    P(lambda: g.tensor_tensor(out=BH[:, hs], in0=AS[:, hs], in1=E[:, hs], op=ALU.mult),
                      reads=[t_AS, tE], writes=[t_BH])

            if en('D'):
                pass
                srcs = ((AB, t_AB), (RB, t_RB), (BB, t_BB), (KB, t_KB))
                identq = identb if RDT == BF16 else identf
                for hp in range(NKC):
                    bk = hp % 2
                    PTq = bank(bk).rearrange("p (e q t) -> p e q t", e=2, q=4)
                    for hh in range(2):
                        h = 2 * hp + hh
                        for q, (tl, tt) in enumerate(srcs):
                            for b in range(2):
                                cs = slice(64 * b, 64 * b + 64)
                                cx.pe(lambda q=q, tl=tl: nc.tensor.matmul(
                                    PTq[cs, hh, q, :], lhsT=tl[cs, h * HS:(h + 1) * HS], rhs=identq[cs, cs],
                                    start=True, stop=True), reads=[tt, t_c], writes=[t_bank[bk]])
                    evac_copy(FT[:, 2 * hp:2 * hp + 2], PTq, [t_bank[bk]], [t_FT])

            if en('E1'):
                pass
                for grp in range(4):
                    b1, b2, b3 = (2, 3, 4) if grp % 2 == 0 else (5, 6, 7)
                    P1 = bank(b1).rearrange("p (h q t) -> p h q t", h=4, q=2)
                    P2 = bank(b2).rearrange("p (h q t) -> p h q t", h=4, q=2)
                    P3 = bank(b3)[:, 0:256].rearrange("p (h t) -> p h t", h=4)
                    for hl in range(4):
                        h = grp * 4 + hl
                        for b in range(2):
                            cs = slice(64 * b, 64 * b + 64)
                            cx.pe(lambda: nc.tensor.matmul(P1[cs, hl], lhsT=FT[cs, h, 2, :], rhs=FT[cs, h, 0:2, :],
                                                           start=True, stop=True), reads=[t_FT], writes=[t_bank[b1]])
                            cx.pe(lambda: nc.tensor.matmul(P2[cs, hl], lhsT=FT[cs, h, 3, :], rhs=FT[cs, h, 0:2, :],
                                                           start=True, stop=True), reads=[t_FT], writes=[t_bank[b2]])
                            cx.pe(lambda: nc.tensor.matmul(P3[cs, hl], lhsT=FT[cs, h, 0, :], rhs=FT[cs, h, 2, :],
                                                           start=True, stop=True), reads=[t_FT], writes=[t_bank[b3]])
                    hsl = slice(grp * 4, grp * 4 + 4)
                    mb = lambda m: m[:, :].unsqueeze(1).to_broadcast([128, 4, L])
                    cx.dve(lambda: nc.vector.tensor_tensor(out=AabT[:, hsl, :], in0=P1[:, :, 0, :], in1=mb(mST), op=ALU.mult),
                           reads=[t_bank[b1], t_c], writes=[t_AabT[grp // 2]])
                    cx.dve(lambda: nc.vector.tensor_tensor(out=ArbT[:, hsl, :], in0=P1[:, :, 1, :], in1=mb(mIT), op=ALU.mult),
                           reads=[t_bank[b1], t_c], writes=[t_ArbT[grp // 2]])
                    cx.dve(lambda: nc.vector.tensor_tensor(out=AakT[:, hsl, :], in0=P2[:, :, 0, :], in1=mb(mST), op=ALU.mult),
                           reads=[t_bank[b2], t_c], writes=[t_AakT[grp // 2]])
                    cx.dve(lambda: nc.vector.tensor_tensor(out=ArkT[:, hsl, :], in0=P2[:, :, 1, :], in1=mb(mIT), op=ALU.mult),
                           reads=[t_bank[b2], t_c], writes=[t_ArkT[grp // 2]])
                    cx.dve(lambda: nc.vector.tensor_tensor(out=GX[:, hsl, 0:L], in0=P3, in1=mb(mS), op=ALU.mult),
                           reads=[t_bank[b3], t_c], writes=[t_GX[grp // 2]])

            if en('E2'):
                pass
                PX = PS[:, 0:2, :].rearrange("p a (h i) -> p (a h) i", i=HS)
                for h in range(H):
                    for b in range(2):
                        cs = slice(64 * b, 64 * b + 64)
                        cx.pe(lambda: nc.tensor.matmul(PX[cs, h, :], lhsT=FT[cs, h, 0, :], rhs=MB[cs, h, :],
                                                       start=True, stop=False), reads=[t_FT, t_MB], writes=[t_bank[h // 8]])
                        cx.pe(lambda: nc.tensor.matmul(PX[cs, h, :], lhsT=AakT[cs, h, :], rhs=Vb[cs, h * HS:(h + 1) * HS],
                                                       start=False, stop=True), reads=[t_AakT[h // 8], t_Vb], writes=[t_bank[h // 8]])
                for hf in range(2):
                    hsl = slice(8 * hf, 8 * hf + 8)
                    cx.act(lambda: nc.scalar.copy(out=GX[:, hsl, L:2 * L], in_=PX[:, hsl, :]), reads=[t_bank[hf]], writes=[t_GX[hf]])

            if en('E3'):
                pass
                PA = PS[:, 2:6, :].rearrange("p a (h t) -> p (a h) t", t=2 * L)
                PB = PS[:, 6:8, :].rearrange("p a (h t) -> p (a h) t", t=L)
                NLEV = 6
                for lev in range(NLEV):
                    last = (lev == NLEV - 1)
                    for hf in range(2):
                        hsl = slice(8 * hf, 8 * hf + 8)
                        for h in range(8 * hf, 8 * hf + 8):
                            for b in range(2):
                                cs = slice(64 * b, 64 * b + 64)
                                if not last:
                                    cx.pe(lambda: nc.tensor.matmul(PA[cs, h, :], lhsT=AabT[cs, h, :], rhs=GX[cs, h, :],
                                                                   start=True, stop=True),
                                          reads=[t_AabT[hf], t_GX[hf]], writes=[t_bank[2 + h // 4]])
                                    cx.pe(lambda: nc.tensor.matmul(PB[cs, h, :], lhsT=GX[cs, h, 0:L], rhs=AabT[cs, h, :],
                                                                   start=True, stop=True),
                                          reads=[t_AabT[hf], t_GX[hf]], writes=[t_bank[6 + hf]])
                                else:
                                    cx.pe(lambda: nc.tensor.matmul(PA[cs, h, L:2 * L], lhsT=AabT[cs, h, :], rhs=GX[cs, h, L:2 * L],
                                                                   start=True, stop=True),
                                          reads=[t_AabT[hf], t_GX[hf]], writes=[t_bank[2 + h // 4]])
                        pa_t = [t_bank[2 + 2 * hf], t_bank[3 + 2 * hf]]
                        cx.dve(lambda: nc.vector.tensor_tensor(out=GX[:, hsl, L:2 * L], in0=PA[:, hsl, L:2 * L],
                                                               in1=GX[:, hsl, L:2 * L], op=ALU.add),
                               reads=pa_t + [t_GX[hf]], writes=[t_GX[hf]])
                        if not last:
                            cx.act(lambda: nc.scalar.copy(out=GX[:, hsl, 0:L], in_=PA[:, hsl, 0:L]), reads=pa_t, writes=[t_GX[hf]])
                            cx.act(lambda: nc.scalar.copy(out=AabT[:, hsl, :], in_=PB[:, hsl, :]), reads=[t_bank[6 + hf]],
                                   writes=[t_AabT[hf]])

            if en('E4'):
                pass
                PY = PX
                for h in range(H):
                    for b in range(2):
                        cs = slice(64 * b, 64 * b + 64)
                        cx.pe(lambda: nc.tensor.matmul(PY[cs, h, :], lhsT=FT[cs, h, 1, :], rhs=MB[cs, h, :],
                                                       start=True, stop=False), reads=[t_FT, t_MB], writes=[t_bank[h // 8]])
                        cx.pe(lambda: nc.tensor.matmul(PY[cs, h, :], lhsT=ArbT[cs, h, :], rhs=GX[cs, h, L:2 * L],
                                                       start=False, stop=False), reads=[t_ArbT[h // 8], t_GX[h // 8]], writes=[t_bank[h // 8]])
                        cx.pe(lambda: nc.tensor.matmul(PY[cs, h, :], lhsT=ArkT[cs, h, :], rhs=Vb[cs, h * HS:(h + 1) * HS],
                                                       start=False, stop=True), reads=[t_ArkT[h // 8], t_Vb], writes=[t_bank[h // 8]])
                cx.act(lambda: nc.scalar.copy(out=V3(X1), in_=PY), reads=[t_bank[0], t_bank[1]], writes=[t_X1])

            if en('E5'):
                pass
                PM = PS[:, 2:4, :].rearrange("p a (h i) -> p (a h) i", i=HS)
                for h in range(H):
                    for b in range(2):
                        cs = slice(64 * b, 64 * b + 64)
                        cx.pe(lambda: nc.tensor.matmul(PM[cs, h, :], lhsT=BH[cs, h * HS:(h + 1) * HS], rhs=GX[cs, h, L:2 * L],
                                                       start=True, stop=False), reads=[t_BH, t_GX[h // 8]], writes=[t_bank[2 + h // 8]])
                        cx.pe(lambda: nc.tensor.matmul(PM[cs, h, :], lhsT=KH[cs, h * HS:(h + 1) * HS], rhs=Vb[cs, h * HS:(h + 1) * HS],
                                                       start=False, stop=True), reads=[t_KH, t_Vb], writes=[t_bank[2 + h // 8]])
                cx.dve(lambda: nc.vector.tensor_tensor(out=M[:], in0=M[:], in1=WL[:, :].unsqueeze(2).to_broadcast([128, H, HS]),
                                                       op=ALU.mult), reads=[t_M, t_WL], writes=[t_M])
                cx.dve(lambda: nc.vector.tensor_tensor(out=M[:], in0=M[:], in1=PM, op=ALU.add),
                       reads=[t_M, t_bank[2], t_bank[3]], writes=[t_M])
                cx.act(lambda: nc.scalar.copy(out=MB[:], in_=M[:]), reads=[t_M], writes=[t_MB])

            if en('F'):
                P(lambda: g.tensor_tensor(out=V3(KK), in0=V3(Vt), in1=bc_h(st[:, 1, :]), op=ALU.mult),
                  reads=[t_V, t_st, t_KK], writes=[t_KK])

        def stage_F(c):
            if en('F'):
                pass
                Y = X1
                cx.dve(lambda: nc.vector.tensor_reduce(out=st[:, 2, :], in_=V3(Y), axis=AX.X, op=ALU.add),
                       reads=[t_X1], writes=[t_st])
                cx.dve(lambda: nc.vector.scalar_tensor_tensor(out=V3(Y), in0=bc_h(st[:, 2, :]), scalar=-1.0 / HS, in1=V3(Y),
                                                              op0=ALU.mult, op1=ALU.add), reads=[t_X1, t_st], writes=[t_X1])
                P(lambda: g.tensor_tensor(out=X2[:], in0=Y[:], in1=Y[:], op=ALU.mult), reads=[t_X1], writes=[t_X2])
                cx.dve(lambda: nc.vector.tensor_reduce(out=st[:, 3, :], in_=V3(X2), axis=AX.X, op=ALU.add),
                       reads=[t_X2], writes=[t_st])
                cx.act(lambda: nc.scalar.activation(out=st[:, 3, :], in_=st[:, 3, :], func=AF.Sqrt, bias=epsg[:, 0:1],
                                                    scale=1.0 / HS), reads=[t_st, t_c], writes=[t_st])
                cx.dve(lambda: nc.vector.reciprocal(out=st[:, 3, :], in_=st[:, 3, :]), reads=[t_st], writes=[t_st])
                cx.dve(lambda: nc.vector.tensor_tensor(out=V3(Y), in0=V3(Y), in1=bc_h(st[:, 3, :]), op=ALU.mult),
                       reads=[t_X1, t_st], writes=[t_X1])
                P(lambda: g.tensor_tensor(out=Y[:], in0=Y[:], in1=VB[:, 5, :], op=ALU.mult), reads=[t_X1, t_vb], writes=[t_X1])
                P(lambda: g.tensor_tensor(out=Y[:], in0=Y[:], in1=VB[:, 6, :], op=ALU.add), reads=[t_X1, t_vb], writes=[t_X1])
                cx.dve(lambda: nc.vector.tensor_tensor(out=Y[:], in0=Y[:], in1=KK[:], op=ALU.add),
                       reads=[t_X1, t_KK], writes=[t_X1])
                sgT, sgT2, t_sgT = sgT_[c % 2], sgT2_[c % 2], t_sgT_[c % 2]
                for half in range(2):
                    bk = half
                    hs = slice(half * 512, (half + 1) * 512)
                    cx.pe(lambda: nc.tensor.matmul(bank(bk), lhsT=sgT[:], rhs=G2a[:, hs], start=True, stop=False),
                          reads=[t_sgT, t_lw], writes=[t_bank[bk]])
                    cx.pe(lambda: nc.tensor.matmul(bank(bk), lhsT=sgT2[:], rhs=G2b[:, hs], start=False, stop=True),
                          reads=[t_sgT, t_lw], writes=[t_bank[bk]])
                    cx.dve(lambda: nc.vector.tensor_tensor(out=YG[:, hs], in0=Y[:, hs], in1=bank(bk), op=ALU.mult),
                           reads=[t_X1, t_bank[bk]], writes=[t_YG])
                PTb = PS[:, 7, :].bitcast(BF16)[:, 0:NKC * 128].rearrange("p (k t) -> p k t", t=128)
                for kc in range(NKC):
                    cx.pe(lambda kc=kc: nc.tensor.transpose(out=PTb[:, kc, :], in_=YG[:, kc * 128:(kc + 1) * 128],
                                                            identity=identb[:]), reads=[t_YG, t_c], writes=[t_bank[7]])
                cx.act(lambda: nc.scalar.copy(out=ygT[:], in_=PTb), reads=[t_bank[7]], writes=[t_ygT])
            for b in range(2):
                cx.dma_sp(XT[64 * b:64 * b + 64, :], src[b * T + c * L:b * T + (c + 1) * L, :], writes=[t_XT])
            for half in range(2 if en('F') else 0):
                bk = half
                hs = slice(half * 512, (half + 1) * 512)
                proj512(lambda k: ygT[:, k, :], lambda k: Wrkvo[:, 3, k, hs], NKC, bk, [t_ygT, t_W[3]])
                cx.dve(lambda: nc.vector.tensor_tensor(out=XT[:, hs], in0=XT[:, hs], in1=bank(bk), op=ALU.add),
                       reads=[t_XT, t_bank[bk]], writes=[t_XT])
            for b in range(2):
                cx.dma_sp(dst[b * T + c * L:b * T + (c + 1) * L, :], XT[64 * b:64 * b + 64, :], reads=[t_XT])

        stage_AB(0)
        for c in range(nchunks):
            stage_CDE(c)
            if c + 1 < nchunks:
                stage_AB(c + 1)
            stage_F(c)
        cx.phase_end()


def lay_gu(w):
    return np.ascontiguousarray(w.reshape(NKC, 128, NFC, 128).transpose(1, 2, 0, 3))


def lay_rows(w):
    r = w.shape[0] // 128
    return np.ascontiguousarray(w.reshape(r, 128, w.shape[1]).transpose(1, 0, 2))


def lay_col(v):
    return np.ascontiguousarray(v.reshape(-1, 128).T)


def build_program(ntok, phases, T=2048):
    nc = bass.Bass("TRN2", target_bir_lowering=False)
    dram_in = lambda n, s: nc.dram_tensor(n, s, F32, kind="ExternalInput").ap()
    x = dram_in("x", [ntok, D])
    out = nc.dram_tensor("out", [ntok, D], F32, kind="ExternalOutput").ap()
    scr = nc.dram_tensor("scr", [ntok, D], F32, kind="Internal").ap()
    wg = dram_in("ffn_wg", [4, 128, NFC, NKC, 128])
    wu = dram_in("ffn_wu", [4, 128, NFC, NKC, 128])
    wd = dram_in("ffn_wd", [4, 128, NFC, D])
    gcols = dram_in("gcols", [6, 128, NKC])
    fnorm = dram_in("final_norm", [D])
    cw_in = dram_in("conv_win", [128, 16, NKC, 128])
    cw_out = dram_in("conv_wout", [128, NKC, D])
    c_small = dram_in("conv_small", [128, 288])
    c_bout = dram_in("conv_bout", [D])
    rw_rkvo = dram_in("rw_rkvo", [4, 128, NKC, D])
    rw_w1 = dram_in("rw_w1", [128, NKC, 64])
    rw_a1 = dram_in("rw_a1", [128, NKC, 64])
    rw_g1 = dram_in("rw_g1", [128, NKC, 160])
    rw_w2 = dram_in("rw_w2", [64, D])
    rw_a2 = dram_in("rw_a2", [64, D])
    rw_g2 = dram_in("rw_g2", [160, D])
    rw_vecs = dram_in("rw_vecs", [7, D])
    rw_mu = dram_in("rw_mu", [128, 6, NKC])
    with contextlib.ExitStack() as es:
        cx = Ctx(nc, es)
        n = len(phases)
        for pi, ph in enumerate(phases):
            src = x if pi == 0 else scr
            dst = out if pi == n - 1 else scr
            if ph.startswith("ffn"):
                k = int(ph[3])
                layer, which = divmod(k, 2)
                gidx = layer * 3 + (0 if which == 0 else 2)
                ffn_phase(cx, src, dst, ntok, wg[k], wu[k], wd[k], gcols[gidx],
                          final_gain=(fnorm if ph.endswith("f") else None))
            elif ph == "rwkv":
                rwkv_phase(cx, src, dst, ntok, T, rw_rkvo, rw_w1, rw_a1, rw_g1, rw_w2, rw_a2, rw_g2, rw_vecs, rw_mu,
                           gcols[1])
            elif ph == "conv":
                conv_phase(cx, src, dst, ntok, T, cw_in, cw_out, c_small, c_bout, gcols[4])
            else:
                raise ValueError(ph)
    return nc


def prep_weights(inp):
    w = {}
    g = np.asarray(inp["ffn_w_gate"], np.float32)
    u = np.asarray(inp["ffn_w_up"], np.float32)
    dn = np.asarray(inp["ffn_w_down"], np.float32)
    w["ffn_wg"] = np.stack([lay_gu(g[l, j]) for l in range(2) for j in range(2)])
    w["ffn_wu"] = np.stack([lay_gu(u[l, j]) for l in range(2) for j in range(2)])
    w["ffn_wd"] = np.stack([lay_rows(dn[l, j]) for l in range(2) for j in range(2)])
    ng = np.asarray(inp["norm_gains"], np.float32)
    w["gcols"] = np.stack([lay_col(ng[l, j]) for l in range(2) for j in range(3)])
    w["final_norm"] = np.ascontiguousarray(np.asarray(inp["final_norm"], np.float32))
    cwi = np.asarray(inp["conv_w_in"], np.float32)[0]
    w["conv_win"] = np.ascontiguousarray(cwi.reshape(NKC, 128, 16, 128).transpose(1, 2, 0, 3))
    w["conv_wout"] = lay_rows(np.asarray(inp["conv_w_out"], np.float32)[0])
    dw = np.asarray(inp["conv_dw"], np.float32)[0]
    dwcol = dw.T.reshape(NKC, 128, CONVW).transpose(1, 0, 2).reshape(128, NKC * CONVW)
    w["conv_small"] = np.ascontiguousarray(np.concatenate([
        lay_col(np.asarray(inp["conv_b_in"], np.float32)[0]),
        lay_col(np.asarray(inp["conv_dw_b"], np.float32)[0]),
        lay_col(np.asarray(inp["conv_ln_gain"], np.float32)[0]),
        lay_col(np.asarray(inp["conv_ln_bias"], np.float32)[0]),
        dwcol], axis=1))
    w["conv_bout"] = np.ascontiguousarray(np.asarray(inp["conv_b_out"], np.float32)[0])
    f = lambda k: np.asarray(inp[k], np.float32)[0]
    rkv = f("rwkv_w_rkv")
    w["rw_rkvo"] = np.stack([lay_rows(rkv[0]), lay_rows(rkv[1]), lay_rows(rkv[2]), lay_rows(f("rwkv_w_out"))])
    w["rw_w1"] = lay_rows(f("rwkv_w1"))
    w["rw_a1"] = lay_rows(f("rwkv_a1"))
    w["rw_g1"] = lay_rows(f("rwkv_g1"))
    w["rw_w2"] = np.ascontiguousarray(f("rwkv_w2"))
    w["rw_a2"] = np.ascontiguousarray(f("rwkv_a2"))
    w["rw_g2"] = np.ascontiguousarray(f("rwkv_g2"))
    w["rw_vecs"] = np.ascontiguousarray(np.stack([f("rwkv_w0"), f("rwkv_a0"), f("rwkv_k_k"), f("rwkv_k_a"),
                                                  f("rwkv_r_k").reshape(D), f("rwkv_ln_gain"), f("rwkv_ln_bias")]))
    muv = f("rwkv_mu")
    w["rw_mu"] = np.ascontiguousarray(muv.reshape(6, NKC, 128).transpose(2, 0, 1))
    return w


ALL_PHASES = ["ffn0", "rwkv", "ffn1", "ffn2", "conv", "ffn3f"]


def kernel(**inputs):
    x = np.asarray(inputs["x"], np.float32)
    B, T, _ = x.shape
    nb = B // NCORES
    ntok = nb * T
    w = prep_weights(inputs)
    nc = build_program(ntok, ALL_PHASES, T=T)
    in_maps = []
    for c in range(NCORES):
        m = dict(w)
        m["x"] = np.ascontiguousarray(x[c * nb:(c + 1) * nb].reshape(ntok, D))
        in_maps.append(m)
    res = run_bass_kernel_spmd(nc, in_maps, core_ids=list(range(NCORES)))
    outs = [np.asarray(r["out"]).reshape(nb, T, D) for r in res.results]
    return np.concatenate(outs, axis=0).astype(np.float32)
```

```python
import bisect
import contextlib
import math

import numpy as np
import concourse.bass as bass
import concourse.mybir as mybir
from concourse.bass_utils import run_bass_kernel_spmd

F32 = mybir.dt.float32
BF16 = mybir.dt.bfloat16
AF = mybir.ActivationFunctionType
ALU = mybir.AluOpType
AX = mybir.AxisListType

D = 1024
DFF = 2816
NFC = DFF // 128
NKC = D // 128
H = 16
HS = 64
CONVW = 31
RMS_EPS = 1e-6
LN_EPS = 1e-5
GN_EPS = 64e-5
NCORES = 8

SAME_ENGINE_SYNC = True
_PH = [0]


def _pfx():
    _PH[0] += 1
    return f"p{_PH[0]}_"


class Trk:
    __slots__ = ("name", "writer", "readers")

    def __init__(self, name=""):
        self.name = name
        self.writer = None
        self.readers = {}


class Lane:
    def __init__(self, name, eng, sem, inc):
        self.name = name
        self.eng = eng
        self.sem = sem
        self.inc = inc
        self.n = 0
        self.count = 0
        self.sig_idx = []
        self.sig_val = []
        self.last = None
        self.last_sig = True
        self.seen = {}

    def ticket(self, idx):
        p = bisect.bisect_left(self.sig_idx, idx)
        if p < len(self.sig_idx):
            return self.sig_val[p]
        assert self.last is not None and not self.last_sig and self.n - 1 >= idx, (self.name, idx, self.n)
        self.last.then_inc(self.sem, self.inc)
        self.count += self.inc
        self.sig_idx.append(self.n - 1)
        self.sig_val.append(self.count)
        self.last_sig = True
        return self.count


class Sched:
    def __init__(self, nc):
        self.nc = nc

    def _wait(self, issuer, lane, idx):
        if lane is issuer and (not SAME_ENGINE_SYNC or lane.name == "pe"):
            return
        t = lane.ticket(idx)
        if issuer.seen.get(lane, 0) >= t:
            return
        issuer.seen[lane] = t
        issuer.eng.wait_ge(lane.sem, t)

    def _deps(self, issuer, reads, writes):
        for b in reads:
            if b.writer is not None:
                self._wait(issuer, *b.writer)
        for b in writes:
            relax = False
            if b.writer is not None and not (relax and b.writer[0] is issuer):
                self._wait(issuer, *b.writer)
            for ln, idx in b.readers.items():
                if not (relax and ln is issuer):
                    self._wait(issuer, ln, idx)

    def emit(self, lane, fn, reads=(), writes=()):
        self._deps(lane, reads, writes)
        ins = fn()
        idx = lane.n
        lane.n += 1
        lane.last = ins
        lane.last_sig = False
        for b in reads:
            b.readers[lane] = idx
        for b in writes:
            b.writer = (lane, idx)
            b.readers = {}
        return ins

    def dma(self, issuer, slot, fn, reads=(), writes=()):
        if slot.n > 0:
            t = slot.sig_val[-1]
            if issuer.seen.get(slot, 0) < t:
                issuer.seen[slot] = t
                issuer.eng.wait_ge(slot.sem, t)
        self._deps(issuer, reads, writes)
        if issuer.last is not None and not issuer.last_sig:
            issuer.ticket(issuer.n - 1)
        ins = fn()
        ins.then_inc(slot.sem, 16)
        idx = slot.n
        slot.n += 1
        slot.count += 16
        slot.sig_idx.append(idx)
        slot.sig_val.append(slot.count)
        issuer.n += 1
        issuer.last = None
        issuer.last_sig = True
        for b in reads:
            b.readers[slot] = idx
        for b in writes:
            b.writer = (slot, idx)
            b.readers = {}
        return ins

    def drain(self, issuer, slots):
        for slot in slots:
            if slot.n > 0:
                t = slot.sig_val[-1]
                if issuer.seen.get(slot, 0) < t:
                    issuer.seen[slot] = t
                    issuer.eng.wait_ge(slot.sem, t)


class Ctx:
    def __init__(self, nc, es):
        self.nc = nc
        self.S = Sched(nc)
        sem = lambda n: es.enter_context(nc.semaphore(n))
        self.PE = Lane("pe", nc.tensor, sem("s_pe"), 1)
        self.ACT = Lane("act", nc.scalar, sem("s_act"), 1)
        self.DVE = Lane("dve", nc.vector, sem("s_dve"), 1)
        self.POOL = Lane("pool", nc.gpsimd, sem("s_pool"), 1)
        self.SP = Lane("sp", nc.sync, sem("s_sp"), 1)
        self.slots_sp = [Lane(f"qs{i}", None, sem(f"s_qs{i}"), 16) for i in range(8)]
        self.slots_pl = [Lane(f"qp{i}", None, sem(f"s_qp{i}"), 16) for i in range(8)]
        self.rr_sp = 0
        self.rr_pl = 0

    def dma_sp(self, out, in_, reads=(), writes=()):
        slot = self.slots_sp[self.rr_sp % len(self.slots_sp)]
        self.rr_sp += 1
        nc = self.nc
        return self.S.dma(self.SP, slot, lambda: nc.sync.dma_start(out=out, in_=in_), reads, writes)

    def dma_pl(self, out, in_, reads=(), writes=()):
        slot = self.slots_pl[self.rr_pl % len(self.slots_pl)]
        self.rr_pl += 1
        nc = self.nc
        return self.S.dma(self.POOL, slot, lambda: nc.gpsimd.dma_start(out=out, in_=in_), reads, writes)

    def pe(self, fn, reads=(), writes=()):
        return self.S.emit(self.PE, fn, reads, writes)

    def act(self, fn, reads=(), writes=()):
        return self.S.emit(self.ACT, fn, reads, writes)

    def dve(self, fn, reads=(), writes=()):
        return self.S.emit(self.DVE, fn, reads, writes)

    def pool(self, fn, reads=(), writes=()):
        return self.S.emit(self.POOL, fn, reads, writes)

    def phase_end(self):
        self.S.drain(self.SP, self.slots_sp + self.slots_pl)
        self.nc.all_engine_barrier()


def tok_view(ap, r0, nrows):
    return ap[r0:r0 + nrows, :].rearrange("(s p) d -> p s d", p=128)


def ffn_phase(cx, src, dst, ntok, wg, wu, wd, gcol_ap, final_gain=None):
    nc = cx.nc
    TT = 256
    NS = TT // 128
    ntiles = ntok // TT
    with contextlib.ExitStack() as es:
        pf = _pfx()
        sb = lambda n, s, d: es.enter_context(nc.sbuf_tensor(pf + n, s, d))
        ps = lambda n, s, d: es.enter_context(nc.psum_tensor(pf + n, s, d))
        Wg = sb("Wg", [128, NFC, NKC, 128], BF16)
        Wu = sb("Wu", [128, NFC, NKC, 128], BF16)
        Wd = sb("Wd", [128, NFC, D], BF16)
        t_wg = [Trk() for _ in range(NFC)]
        t_wu = [Trk() for _ in range(NFC)]
        t_wd = [Trk() for _ in range(NFC)]
        xb = [sb(f"xb{i}", [128, NS, D], F32) for i in range(2)]
        t_xb = [Trk(), Trk()]
        xn = sb("xn", [128, NS, D], BF16)
        t_xn = Trk()
        junk = sb("junk", [128, D], BF16)
        t_junk = Trk()
        xnT = [sb(f"xnT{i}", [128, NKC, TT], BF16) for i in range(2)]
        t_xnT = [Trk(), Trk()]
        hT = [sb(f"hT{i}", [128, NFC, TT], BF16) for i in range(2)]
        t_hT = [Trk(), Trk()]
        sg = [sb(f"sg{i}", [128, TT], F32) for i in range(2)]
        t_sg = [Trk(), Trk()]
        ss = sb("ss", [128, 4], F32)
        t_ss = Trk()
        rstd = sb("rstd", [128, 4], F32)
        t_rstd = Trk()
        gcol = sb("gcol", [128, NKC], F32)
        t_gcol = Trk()
        ident = sb("ident", [128, 128], BF16)
        t_ident = Trk()
        epsb = sb("epsb", [128, 1], F32)
        t_eps = Trk()
        if final_gain is not None:
            fg = sb("fg", [128, D], F32)
            t_fg = Trk()
        pT = ps("pT", [128, NKC, TT], BF16)
        t_pT = Trk()
        pgu = [ps(f"pgu{i}", [128, 2, TT], F32) for i in range(2)]
        t_pgu = [Trk(), Trk()]
        py = [ps(f"py{i}", [128, 512], F32) for i in range(2)]
        t_py = [Trk(), Trk()]

        cx.pool(lambda: nc.gpsimd.memset(ident[:], 0.0), writes=[t_ident])
        cx.pool(lambda: nc.gpsimd.affine_select(
            out=ident[:], in_=ident[:], pattern=[[-1, 128]], compare_op=ALU.not_equal,
            fill=1.0, base=0, channel_multiplier=1), reads=[t_ident], writes=[t_ident])
        cx.pool(lambda: nc.gpsimd.memset(epsb[:], RMS_EPS), writes=[t_eps])
        cx.dma_sp(gcol[:], gcol_ap, writes=[t_gcol])
        if final_gain is not None:
            cx.dma_sp(fg[:], final_gain.partition_broadcast(128), writes=[t_fg])

        def load_x(i):
            cx.dma_sp(xb[i % 2][:], tok_view(src, i * TT, TT), writes=[t_xb[i % 2]])

        def load_w():
            for fc in range(NFC):
                cx.dma_pl(Wg[:, fc], wg[:, fc], writes=[t_wg[fc]])
                cx.dma_pl(Wu[:, fc], wu[:, fc], writes=[t_wu[fc]])
            for fc in range(NFC):
                cx.dma_pl(Wd[:, fc], wd[:, fc], writes=[t_wd[fc]])

        def norm(i):
            xt = xb[i % 2]
            tx = t_xb[i % 2]
            for s in range(NS):
                cx.act(lambda s=s: nc.scalar.activation(out=junk[:], in_=xt[:, s, :], func=AF.Square,
                                                        accum_out=ss[:, s:s + 1]),
                       reads=[tx], writes=[t_junk, t_ss])
            cx.act(lambda: nc.scalar.activation(out=rstd[:, 0:NS], in_=ss[:, 0:NS], func=AF.Sqrt,
                                                bias=epsb[:, 0:1], scale=1.0 / D),
                   reads=[t_ss, t_eps], writes=[t_rstd])
            cx.dve(lambda: nc.vector.reciprocal(out=rstd[:, 0:NS], in_=rstd[:, 0:NS]),
                   reads=[t_rstd], writes=[t_rstd])
            for s in range(NS):
                cx.dve(lambda s=s: nc.vector.tensor_scalar(out=xn[:, s, :], in0=xt[:, s, :],
                                                           scalar1=rstd[:, s:s + 1], scalar2=None,
                                                           op0=ALU.mult),
                       reads=[tx, t_rstd], writes=[t_xn])

        def transp(i):
            for s in range(NS):
                for kc in range(NKC):
                    cx.pe(lambda s=s, kc=kc: nc.tensor.transpose(
                        out=pT[:, kc, s * 128:(s + 1) * 128], in_=xn[:, s, kc * 128:(kc + 1) * 128],
                        identity=ident[:]), reads=[t_xn, t_ident], writes=[t_pT])
            cx.dve(lambda: nc.vector.tensor_tensor(
                out=xnT[i % 2][:], in0=pT[:], in1=gcol[:, :].unsqueeze(2).to_broadcast([128, NKC, TT]),
                op=ALU.mult), reads=[t_pT, t_gcol], writes=[t_xnT[i % 2]])

        def gate_up(i, fc):
            b = fc % 2
            xT = xnT[i % 2]
            for which, (W, tw) in enumerate(((Wg, t_wg), (Wu, t_wu))):
                for kc in range(NKC):
                    cx.pe(lambda which=which, W=W, kc=kc: nc.tensor.matmul(
                        pgu[b][:, which, :], lhsT=W[:, fc, kc, :], rhs=xT[:, kc, :],
                        start=(kc == 0), stop=(kc == NKC - 1)),
                        reads=[tw[fc], t_xnT[i % 2]], writes=[t_pgu[b]])
            cx.act(lambda: nc.scalar.activation(out=sg[b][:], in_=pgu[b][:, 0, :], func=AF.Silu),
                   reads=[t_pgu[b]], writes=[t_sg[b]])
            cx.dve(lambda: nc.vector.tensor_tensor(out=hT[i % 2][:, fc, :], in0=sg[b][:], in1=pgu[b][:, 1, :],
                                                   op=ALU.mult),
                   reads=[t_sg[b], t_pgu[b]], writes=[t_hT[i % 2]])

        def down(i):
            xt = xb[i % 2]
            tx = t_xb[i % 2]
            g = 0
            for s in range(NS):
                for half in range(2):
                    b = g % 2
                    g += 1
                    for fc in range(NFC):
                        cx.pe(lambda fc=fc: nc.tensor.matmul(
                            py[b][:], lhsT=hT[i % 2][:, fc, s * 128:(s + 1) * 128],
                            rhs=Wd[:, fc, half * 512:(half + 1) * 512],
                            start=(fc == 0), stop=(fc == NFC - 1)),
                            reads=[t_hT[i % 2], t_wd[fc]], writes=[t_py[b]])
                    cx.dve(lambda: nc.vector.scalar_tensor_tensor(
                        out=xt[:, s, half * 512:(half + 1) * 512], in0=py[b][:], scalar=0.5,
                        in1=xt[:, s, half * 512:(half + 1) * 512], op0=ALU.mult, op1=ALU.add),
                        reads=[t_py[b], tx], writes=[tx])

        def finish(i):
            xt = xb[i % 2]
            tx = t_xb[i % 2]
            if final_gain is not None:
                for s in range(NS):
                    cx.act(lambda s=s: nc.scalar.activation(out=junk[:], in_=xt[:, s, :], func=AF.Square,
                                                            accum_out=ss[:, 2 + s:3 + s]),
                           reads=[tx], writes=[t_junk, t_ss])
                cx.act(lambda: nc.scalar.activation(out=rstd[:, 2:2 + NS], in_=ss[:, 2:2 + NS], func=AF.Sqrt,
                                                    bias=epsb[:, 0:1], scale=1.0 / D),
                       reads=[t_ss, t_eps], writes=[t_rstd])
                cx.dve(lambda: nc.vector.reciprocal(out=rstd[:, 2:2 + NS], in_=rstd[:, 2:2 + NS]),
                       reads=[t_rstd], writes=[t_rstd])
                for s in range(NS):
                    cx.dve(lambda s=s: nc.vector.scalar_tensor_tensor(
                        out=xt[:, s, :], in0=xt[:, s, :], scalar=rstd[:, 2 + s:3 + s], in1=fg[:],
                        op0=ALU.mult, op1=ALU.mult), reads=[tx, t_rstd, t_fg], writes=[tx])
            cx.dma_sp(tok_view(dst, i * TT, TT), xt[:], reads=[tx])

        load_x(0)
        load_w()
        norm(0)
        transp(0)
        for i in range(ntiles):
            if i + 1 < ntiles:
                load_x(i + 1)
            for fc in range(NFC):
                gate_up(i, fc)
                if fc == 12 and i + 1 < ntiles:
                    norm(i + 1)
            if i + 1 < ntiles:
                transp(i + 1)
            down(i)
            finish(i)
        cx.phase_end()


class NormT:
    def __init__(self, cx, es, TT, nbuf_xnT=2):
        nc = cx.nc
        self.cx = cx
        self.TT = TT
        self.NS = TT // 128
        pf = _pfx()
        sb = lambda n, s, d: es.enter_context(nc.sbuf_tensor(pf + n, s, d))
        ps = lambda n, s, d: es.enter_context(nc.psum_tensor(pf + n, s, d))
        self.xn = sb("xn", [128, self.NS, D], BF16)
        self.t_xn = Trk()
        self.junk = sb("junk", [128, D], BF16)
        self.t_junk = Trk()
        self.xnT = [sb(f"xnT{i}", [128, NKC, TT], BF16) for i in range(nbuf_xnT)]
        self.t_xnT = [Trk() for _ in range(nbuf_xnT)]
        self.ss = sb("ss", [128, 4], F32)
        self.t_ss = Trk()
        self.rstd = sb("rstd", [128, 4], F32)
        self.t_rstd = Trk()
        self.gcol = sb("gcol", [128, NKC], F32)
        self.t_gcol = Trk()
        self.ident = sb("ident", [128, 128], BF16)
        self.t_ident = Trk()
        self.epsb = sb("epsb", [128, 1], F32)
        self.t_eps = Trk()
        self.pT = ps("pT", [128, NKC, TT], BF16)
        self.t_pT = Trk()

    def init(self, gcol_ap):
        cx, nc = self.cx, self.cx.nc
        ident, epsb = self.ident, self.epsb
        cx.pool(lambda: nc.gpsimd.memset(ident[:], 0.0), writes=[self.t_ident])
        cx.pool(lambda: nc.gpsimd.affine_select(
            out=ident[:], in_=ident[:], pattern=[[-1, 128]], compare_op=ALU.not_equal,
            fill=1.0, base=0, channel_multiplier=1), reads=[self.t_ident], writes=[self.t_ident])
        cx.pool(lambda: nc.gpsimd.memset(epsb[:], RMS_EPS), writes=[self.t_eps])
        cx.dma_sp(self.gcol[:], gcol_ap, writes=[self.t_gcol])

    def norm(self, xt, tx):
        cx, nc = self.cx, self.cx.nc
        NS = self.NS
        ss, rstd, junk, xn, epsb = self.ss, self.rstd, self.junk, self.xn, self.epsb
        for s in range(NS):
            cx.act(lambda s=s: nc.scalar.activation(out=junk[:], in_=xt[:, s, :], func=AF.Square,
                                                    accum_out=ss[:, s:s + 1]),
                   reads=[tx], writes=[self.t_junk, self.t_ss])
        cx.act(lambda: nc.scalar.activation(out=rstd[:, 0:NS], in_=ss[:, 0:NS], func=AF.Sqrt,
                                            bias=epsb[:, 0:1], scale=1.0 / D),
               reads=[self.t_ss, self.t_eps], writes=[self.t_rstd])
        cx.dve(lambda: nc.vector.reciprocal(out=rstd[:, 0:NS], in_=rstd[:, 0:NS]),
               reads=[self.t_rstd], writes=[self.t_rstd])
        for s in range(NS):
            cx.dve(lambda s=s: nc.vector.tensor_scalar(out=xn[:, s, :], in0=xt[:, s, :],
                                                       scalar1=rstd[:, s:s + 1], scalar2=None,
                                                       op0=ALU.mult),
                   reads=[tx, self.t_rstd], writes=[self.t_xn])

    def transp(self, j):
        cx, nc = self.cx, self.cx.nc
        pT, xn, ident, gcol, TT = self.pT, self.xn, self.ident, self.gcol, self.TT
        for s in range(self.NS):
            for kc in range(NKC):
                cx.pe(lambda s=s, kc=kc: nc.tensor.transpose(
                    out=pT[:, kc, s * 128:(s + 1) * 128], in_=xn[:, s, kc * 128:(kc + 1) * 128],
                    identity=ident[:]), reads=[self.t_xn, self.t_ident], writes=[self.t_pT])
        cx.dve(lambda: nc.vector.tensor_tensor(
            out=self.xnT[j][:], in0=pT[:], in1=gcol[:, :].unsqueeze(2).to_broadcast([128, NKC, TT]),
            op=ALU.mult), reads=[self.t_pT, self.t_gcol], writes=[self.t_xnT[j]])


def conv_phase(cx, src, dst, ntok, T, w_in, w_out, small_ap, b_out_ap, gcol_ap):
    nc = cx.nc
    TT = 256
    NS = TT // 128
    ntiles = ntok // TT
    tps = T // TT
    HALO = CONVW - 1
    with contextlib.ExitStack() as es:
        pf = _pfx()
        sb = lambda n, s, d: es.enter_context(nc.sbuf_tensor(pf + n, s, d))
        ps = lambda n, s, d: es.enter_context(nc.psum_tensor(pf + n, s, d))
        nt = NormT(cx, es, TT)
        Win = sb("Win", [128, 16, NKC, 128], BF16)
        t_win = [Trk() for _ in range(16)]
        Wout = sb("Wout", [128, NKC, D], BF16)
        t_wout = Trk()
        Dg = sb("Dg", [128, NKC, CONVW, 128], BF16)
        t_dg = Trk()
        small = sb("small", [128, 288], F32)
        t_small = Trk()
        bout = sb("bout", [128, D], F32)
        t_bout = Trk()
        identf = sb("identf", [128, 128], F32)
        t_identf = Trk()
        ones = sb("ones", [128, 128], F32)
        t_ones = Trk()
        lneps = sb("lneps", [128, 1], F32)
        t_lneps = Trk()
        xb = [sb(f"xb{i}", [128, NS, D], F32) for i in range(2)]
        t_xb = [Trk(), Trk()]
        gT = [sb(f"gT{i}", [128, NKC, HALO + TT], BF16) for i in range(2)]
        t_gT = [Trk(), Trk()]
        sg = [sb(f"sg{i}", [128, TT], F32) for i in range(2)]
        t_sg = [Trk(), Trk()]
        cT = sb("cT", [128, NKC, TT], F32)
        t_cT = Trk()
        sq = sb("sq", [128, NKC, TT], F32)
        t_sq = Trk()
        mean = sb("mean", [128, TT], F32)
        t_mean = Trk()
        var = sb("var", [128, TT], F32)
        t_var = Trk()
        aT = sb("aT", [128, NKC, TT], BF16)
        t_aT = Trk()
        pab = [ps(f"pab{i}", [128, 2, TT], F32) for i in range(2)]
        t_pab = [Trk(), Trk()]
        pc = ps("pc", [128, 2, 512], F32)
        t_pc = [Trk(), Trk()]
        pst = ps("pst", [128, 2, TT], F32)
        t_pst = Trk()
        py = [ps("py0", [128, 512], F32)] * 2
        t_py = [Trk()] * 2

        nt.init(gcol_ap)
        cx.dma_sp(small[:], small_ap, writes=[t_small])
        cx.dma_sp(bout[:], b_out_ap.partition_broadcast(128), writes=[t_bout])
        for oc in range(16):
            cx.dma_pl(Win[:, oc], w_in[:, oc], writes=[t_win[oc]])
        for kc in range(0, NKC, 2):
            cx.dma_pl(Wout[:, kc:kc + 2], w_out[:, kc:kc + 2], writes=[t_wout])
        cx.pool(lambda: nc.gpsimd.memset(identf[:], 0.0), writes=[t_identf])
        cx.pool(lambda: nc.gpsimd.affine_select(
            out=identf[:], in_=identf[:], pattern=[[-1, 128]], compare_op=ALU.not_equal,
            fill=1.0, base=0, channel_multiplier=1), reads=[t_identf], writes=[t_identf])
        cx.pool(lambda: nc.gpsimd.memset(ones[:], 1.0), writes=[t_ones])
        cx.pool(lambda: nc.gpsimd.memset(lneps[:], LN_EPS), writes=[t_lneps])
        for cc in range(NKC):
            cx.dve(lambda cc=cc: nc.vector.tensor_tensor(
                out=Dg[:, cc], in0=identf[:, :].unsqueeze(1).to_broadcast([128, CONVW, 128]),
                in1=small[:, 40 + cc * CONVW:40 + (cc + 1) * CONVW].unsqueeze(2).to_broadcast([128, CONVW, 128]),
                op=ALU.mult), reads=[t_identf, t_small], writes=[t_dg])

        def load_x(i):
            cx.dma_sp(xb[i % 2][:], tok_view(src, i * TT, TT), writes=[t_xb[i % 2]])

        def glu(i):
            j = i % 2
            xT = nt.xnT[j]
            g = gT[j]
            if i % tps == 0:
                cx.pool(lambda: nc.gpsimd.memset(g[:, :, 0:HALO], 0.0), writes=[t_gT[j]])
            else:
                cx.pool(lambda: nc.gpsimd.tensor_copy(out=g[:, :, 0:HALO], in_=gT[1 - j][:, :, TT:TT + HALO]),
                        reads=[t_gT[1 - j]], writes=[t_gT[j]])
            for cc in range(NKC):
                b = cc % 2
                for which in range(2):
                    oc = which * 8 + cc
                    for kc in range(NKC):
                        cx.pe(lambda which=which, oc=oc, kc=kc: nc.tensor.matmul(
                            pab[b][:, which, :], lhsT=Win[:, oc, kc, :], rhs=xT[:, kc, :],
                            start=(kc == 0), stop=(kc == NKC - 1)),
                            reads=[t_win[oc], nt.t_xnT[j]], writes=[t_pab[b]])
                cx.act(lambda cc=cc: nc.scalar.activation(out=sg[b][:], in_=pab[b][:, 1, :], func=AF.Sigmoid,
                                                          bias=small[:, 8 + cc:9 + cc]),
                       reads=[t_pab[b], t_small], writes=[t_sg[b]])
                cx.dve(lambda cc=cc: nc.vector.scalar_tensor_tensor(
                    out=g[:, cc, HALO:HALO + TT], in0=pab[b][:, 0, :], scalar=small[:, cc:cc + 1],
                    in1=sg[b][:], op0=ALU.add, op1=ALU.mult),
                    reads=[t_pab[b], t_sg[b], t_small], writes=[t_gT[j]])

        def dwconv(i):
            j = i % 2
            g = gT[j]
            for cc in range(NKC):
                b = cc % 2
                for k in range(CONVW):
                    cx.pe(lambda cc=cc, k=k: nc.tensor.matmul(
                        pc[:, b, 0:TT], lhsT=Dg[:, cc, k, :], rhs=g[:, cc, k:k + TT],
                        start=(k == 0), stop=(k == CONVW - 1)),
                        reads=[t_dg, t_gT[j]], writes=[t_pc[b]])
                cx.act(lambda cc=cc: nc.scalar.activation(out=cT[:, cc, :], in_=pc[:, b, 0:TT], func=AF.Identity,
                                                          bias=small[:, 16 + cc:17 + cc]),
                       reads=[t_pc[b], t_small], writes=[t_cT])
                cx.pool(lambda cc=cc: nc.gpsimd.tensor_tensor(out=sq[:, cc, :], in0=cT[:, cc, :], in1=cT[:, cc, :],
                                                              op=ALU.mult),
                        reads=[t_cT], writes=[t_sq])

        def lnorm_stats(i):
            for which, (buf, tb) in enumerate(((cT, t_cT), (sq, t_sq))):
                for cc in range(NKC):
                    cx.pe(lambda which=which, buf=buf, cc=cc: nc.tensor.matmul(
                        pst[:, which, :], lhsT=ones[:], rhs=buf[:, cc, :],
                        start=(cc == 0), stop=(cc == NKC - 1)),
                        reads=[t_ones, tb], writes=[t_pst])

        def lnorm_chain(i):
            cx.dve(lambda: nc.vector.tensor_scalar(out=mean[:], in0=pst[:, 0, :], scalar1=1.0 / D, scalar2=None,
                                                   op0=ALU.mult), reads=[t_pst], writes=[t_mean])
            cx.dve(lambda: nc.vector.tensor_tensor(out=var[:], in0=mean[:], in1=mean[:], op=ALU.mult),
                   reads=[t_mean], writes=[t_var])
            cx.dve(lambda: nc.vector.scalar_tensor_tensor(out=var[:], in0=pst[:, 1, :], scalar=1.0 / D, in1=var[:],
                                                          op0=ALU.mult, op1=ALU.subtract),
                   reads=[t_pst, t_var], writes=[t_var])
            cx.act(lambda: nc.scalar.activation(out=var[:], in_=var[:], func=AF.Sqrt, bias=lneps[:, 0:1]),
                   reads=[t_var, t_lneps], writes=[t_var])
            cx.dve(lambda: nc.vector.reciprocal(out=var[:], in_=var[:]), reads=[t_var], writes=[t_var])
            cx.dve(lambda: nc.vector.tensor_tensor(
                out=cT[:], in0=cT[:], in1=mean[:, :].unsqueeze(1).to_broadcast([128, NKC, TT]), op=ALU.subtract),
                reads=[t_cT, t_mean], writes=[t_cT])
            cx.dve(lambda: nc.vector.tensor_tensor(
                out=cT[:], in0=cT[:], in1=var[:, :].unsqueeze(1).to_broadcast([128, NKC, TT]), op=ALU.mult),
                reads=[t_cT, t_var], writes=[t_cT])
            for cc in range(NKC):
                cx.act(lambda cc=cc: nc.scalar.activation(out=aT[:, cc, :], in_=cT[:, cc, :], func=AF.Silu,
                                                          scale=small[:, 24 + cc:25 + cc],
                                                          bias=small[:, 32 + cc:33 + cc]),
                       reads=[t_cT, t_small], writes=[t_aT])

        def outproj(i):
            xt = xb[i % 2]
            tx = t_xb[i % 2]
            for s in range(NS):
                cx.pool(lambda s=s: nc.gpsimd.tensor_tensor(out=xt[:, s, :], in0=xt[:, s, :], in1=bout[:], op=ALU.add),
                        reads=[tx, t_bout], writes=[tx])
            g = 0
            for s in range(NS):
                for half in range(2):
                    b = g % 2
                    g += 1
                    for kc in range(NKC):
                        cx.pe(lambda kc=kc: nc.tensor.matmul(
                            py[b][:], lhsT=aT[:, kc, s * 128:(s + 1) * 128],
                            rhs=Wout[:, kc, half * 512:(half + 1) * 512],
                            start=(kc == 0), stop=(kc == NKC - 1)),
                            reads=[t_aT, t_wout], writes=[t_py[b]])
                    cx.dve(lambda: nc.vector.tensor_tensor(
                        out=xt[:, s, half * 512:(half + 1) * 512], in0=py[b][:],
                        in1=xt[:, s, half * 512:(half + 1) * 512], op=ALU.add),
                        reads=[t_py[b], tx], writes=[tx])
            cx.dma_sp(tok_view(dst, i * TT, TT), xt[:], reads=[tx])

        load_x(0)
        nt.norm(xb[0], t_xb[0])
        nt.transp(0)
        glu(0)
        for i in range(ntiles):
            nxt = i + 1 < ntiles
            if nxt:
                load_x(i + 1)
            dwconv(i)
            if nxt:
                nt.norm(xb[(i + 1) % 2], t_xb[(i + 1) % 2])
                nt.transp((i + 1) % 2)
            lnorm_stats(i)
            if nxt:
                glu(i + 1)
            lnorm_chain(i)
            outproj(i)
        cx.phase_end()


C0 = math.exp(-0.5)
RDT = BF16


RW_STOP = [None]


def rwkv_phase(cx, src, dst, ntok, T, w_rkvo, w1, a1, g1, w2, a2, g2, vecs, mu_ap, gcol_ap, dbg=None):
    nc = cx.nc
    L = 64
    nchunks = T // L
    assert ntok == 2 * T
    with contextlib.ExitStack() as es:
        pf = _pfx()
        sb = lambda n, s, d: es.enter_context(nc.sbuf_tensor(pf + n, s, d))
        ps = lambda n, s, d: es.enter_context(nc.psum_tensor(pf + n, s, d))

        Wrkvo = sb("Wrkvo", [128, 4, NKC, D], BF16)
        t_W = [Trk() for _ in range(4)]
        W1 = sb("W1", [128, NKC, 64], BF16)
        A1 = sb("A1", [128, NKC, 64], BF16)
        G1 = sb("G1", [128, NKC, 160], BF16)
        W2 = sb("W2", [64, D], BF16)
        A2 = sb("A2", [64, D], BF16)
        G2a = sb("G2a", [128, D], BF16)
        G2b = sb("G2b", [32, D], BF16)
        t_lw = Trk()
        VB = sb("VB", [128, 7, D], F32)
        t_vb = Trk()
        mu = sb("mu", [128, 6, NKC], F32)
        gcol = sb("gcol", [128, NKC], F32)
        t_small = Trk()
        identf = sb("identf", [128, 128], F32)
        identb = sb("identb", [128, 128], BF16)
        triI = sb("triI", [128, 128], F32)
        triLt = sb("triLt", [128, 128], F32)
        triGt = sb("triGt", [128, 128], F32)
        bsel = sb("bsel", [128, 2], F32)
        ones1 = sb("ones1", [128, 1], F32)
        mST = sb("mST", [128, 64], F32)
        mIT = sb("mIT", [128, 64], F32)
        mS = sb("mS", [128, 64], F32)
        epsr = sb("epsr", [128, 1], F32)
        epsg = sb("epsg", [128, 1], F32)
        t_c = Trk()

        XT = sb("XT", [128, D], F32); t_XT = Trk()
        Rt = sb("Rt", [128, D], F32); t_R = Trk()
        Kt = sb("Kt", [128, D], F32); t_K = Trk()
        Vt = sb("Vt", [128, D], F32); t_V = Trk()
        SW = sb("SW", [128, D], F32); t_SW = Trk()
        AS = sb("AS", [128, D], F32); t_AS = Trk()
        KK = sb("KK", [128, D], F32); t_KK = Trk()
        X1 = sb("X1", [128, D], F32); t_X1 = Trk()
        X2 = sb("X2", [128, D], F32); t_X2 = Trk()
        RB = sb("RB", [128, D], RDT); t_RB = Trk()
        AB = sb("AB", [128, D], RDT); t_AB = Trk()
        KB = sb("KB", [128, D], RDT); t_KB = Trk()
        BB = sb("BB", [128, D], RDT); t_BB = Trk()
        KH = sb("KH", [128, D], RDT); t_KH = Trk()
        BH = sb("BH", [128, D], RDT); t_BH = Trk()
        Vb = sb("Vb", [128, D], RDT); t_Vb = Trk()
        MB = sb("MB", [128, H, HS], RDT); t_MB = Trk()
        xnT = sb("xnT", [128, NKC, 2, L + 1], F32); t_xnT = Trk()
        mix = [sb(f"mix{i}", [128, NKC, 128], BF16) for i in range(2)]
        t_mix = [[Trk() for _ in range(NKC)] for _ in range(2)]
        thw = sb("thw", [64, 128], BF16); t_thw = Trk()
        haT = sb("haT", [64, 128], BF16); t_haT = Trk()
        sgT_ = [sb(f"sgT{i}", [128, 128], BF16) for i in range(2)]
        sgT2_ = [sb(f"sgT2{i}", [32, 128], BF16) for i in range(2)]
        t_sgT_ = [Trk(), Trk()]
        FT = sb("FT", [128, H, 4, L], RDT); t_FT = Trk()
        AabT = sb("AabT", [128, H, L], RDT); t_AabT = [Trk(), Trk()]
        ArbT = sb("ArbT", [128, H, L], RDT); t_ArbT = [Trk(), Trk()]
        AakT = sb("AakT", [128, H, L], RDT); t_AakT = [Trk(), Trk()]
        ArkT = sb("ArkT", [128, H, L], RDT); t_ArkT = [Trk(), Trk()]
        GX = sb("GX", [128, H, 2 * L], RDT); t_GX = [Trk(), Trk()]
        M = sb("M", [128, H, HS], F32); t_M = Trk()
        WL = sb("WL", [128, H], F32); t_WL = Trk()
        st = sb("st", [128, 8, H], F32); t_st = Trk()
        sc = sb("sc", [128, 8], F32); t_sc = Trk()
        ygT = sb("ygT", [128, NKC, 128], BF16); t_ygT = Trk()
        YG = sb("YG", [128, D], BF16); t_YG = Trk()

        PS = ps("PS", [128, 8, 512], F32)
        t_bank = [Trk() for _ in range(8)]

        def bank(i):
            return PS[:, i, :]

        for q in range(4):
            for kc in range(0, NKC, 2):
                cx.dma_pl(Wrkvo[:, q, kc:kc + 2], w_rkvo[q, :, kc:kc + 2], writes=[t_W[q]])
        cx.dma_pl(W1[:], w1, writes=[t_lw])
        cx.dma_pl(A1[:], a1, writes=[t_lw])
        cx.dma_pl(G1[:], g1, writes=[t_lw])
        cx.dma_pl(W2[:], w2, writes=[t_lw])
        cx.dma_pl(A2[:], a2, writes=[t_lw])
        cx.dma_pl(G2a[:], g2[0:128, :], writes=[t_lw])
        cx.dma_pl(G2b[:], g2[128:160, :], writes=[t_lw])
        for i in range(7):
            cx.dma_sp(VB[:, i, :], vecs[i].partition_broadcast(128), writes=[t_vb])
        cx.dma_sp(mu[:], mu_ap, writes=[t_small])
        cx.dma_sp(gcol[:], gcol_ap, writes=[t_small])

        P = cx.pool
        g = nc.gpsimd

        def ident_like(t):
            P(lambda: g.memset(t[:], 0.0), writes=[t_c])
            P(lambda: g.affine_select(out=t[:], in_=t[:], pattern=[[-1, 128]], compare_op=ALU.not_equal,
                                      fill=1.0, base=0, channel_multiplier=1), reads=[t_c], writes=[t_c])
        ident_like(identf)
        ident_like(identb)
        for t in (triI, triLt, triGt):
            P(lambda t=t: g.memset(t[:], 1.0), writes=[t_c])
        P(lambda: g.affine_select(out=triI[:], in_=triI[:], pattern=[[1, 128]], compare_op=ALU.is_ge,
                                  fill=0.0, base=0, channel_multiplier=-1), reads=[t_c], writes=[t_c])
        P(lambda: g.affine_select(out=triLt[:], in_=triLt[:], pattern=[[1, 128]], compare_op=ALU.is_gt,
                                  fill=0.0, base=0, channel_multiplier=-1), reads=[t_c], writes=[t_c])
        P(lambda: g.affine_select(out=triGt[:], in_=triGt[:], pattern=[[-1, 128]], compare_op=ALU.is_gt,
                                  fill=0.0, base=0, channel_multiplier=1), reads=[t_c], writes=[t_c])
        P(lambda: g.memset(triI[0:64, 64:128], 0.0), writes=[t_c])
        P(lambda: g.memset(triLt[0:64, 64:128], 0.0), writes=[t_c])
        P(lambda: g.memset(triGt[64:128, 0:64], 0.0), writes=[t_c])
        P(lambda: g.memset(ones1[:], 1.0), writes=[t_c])
        P(lambda: g.memset(bsel[:], 0.0), writes=[t_c])
        P(lambda: g.memset(bsel[0:64, 0:1], 1.0), writes=[t_c])
        P(lambda: g.memset(bsel[64:128, 1:2], 1.0), writes=[t_c])
        for t in (mST, mIT, mS):
            P(lambda t=t: g.memset(t[:], 1.0), writes=[t_c])
        for hb in range(2):
            sl = slice(64 * hb, 64 * hb + 64)
            P(lambda sl=sl: g.affine_select(out=mST[sl, :], in_=mST[sl, :], pattern=[[1, 64]], compare_op=ALU.is_gt,
                                            fill=0.0, base=0, channel_multiplier=-1), reads=[t_c], writes=[t_c])
            P(lambda sl=sl: g.affine_select(out=mIT[sl, :], in_=mIT[sl, :], pattern=[[1, 64]], compare_op=ALU.is_ge,
                                            fill=0.0, base=0, channel_multiplier=-1), reads=[t_c], writes=[t_c])
            P(lambda sl=sl: g.affine_select(out=mS[sl, :], in_=mS[sl, :], pattern=[[-1, 64]], compare_op=ALU.is_gt,
                                            fill=0.0, base=0, channel_multiplier=1), reads=[t_c], writes=[t_c])
        P(lambda: g.memset(epsr[:], RMS_EPS), writes=[t_c])
        P(lambda: g.memset(epsg[:], GN_EPS), writes=[t_c])
        P(lambda: g.memset(M[:], 0.0), writes=[t_M])
        P(lambda: g.memset(MB[:], 0.0), writes=[t_MB])

        V3 = lambda t: t[:, :].rearrange("p (h i) -> p h i", i=HS)
        bc_h = lambda col: col.unsqueeze(2).to_broadcast([128, H, HS])
        evac_flip = [0]

        def evac_copy(out, in_, reads, writes):
            evac_flip[0] ^= 1
            if evac_flip[0]:
                cx.act(lambda: nc.scalar.copy(out=out, in_=in_), reads=reads, writes=writes)
            else:
                cx.dve(lambda: nc.vector.tensor_copy(out=out, in_=in_), reads=reads, writes=writes)

        def proj512(lhs_fn, rhs_fn, nk, bk, reads):
            for k in range(nk):
                cx.pe(lambda k=k: nc.tensor.matmul(bank(bk), lhsT=lhs_fn(k), rhs=rhs_fn(k),
                                                   start=(k == 0), stop=(k == nk - 1)),
                      reads=(reads(k) if callable(reads) else reads), writes=[t_bank[bk]])

        def make_mix(m, j):
            XX = XT[:, :].rearrange("p (k b s) -> p k b s", k=NKC, b=2)
            if False:
                T4 = X2[:, :].rearrange("p (k b s) -> p k b s", k=NKC, b=2)
                P(lambda: g.tensor_tensor(out=T4, in0=XX,
                                          in1=mu[:, m, :].unsqueeze(2).unsqueeze(3).to_broadcast([128, NKC, 2, L]),
                                          op=ALU.mult), reads=[t_XT, t_small], writes=[t_X2])
                P(lambda: g.tensor_tensor(out=mix[j][:, :, :].rearrange("p k (b s) -> p k b s", b=2), in0=T4,
                                          in1=xnT[:, :, :, 1:L + 1], op=ALU.add),
                  reads=[t_X2, t_xnT], writes=t_mix[j])
                return
            for kc in range(NKC):
                cx.dve(lambda kc=kc: nc.vector.scalar_tensor_tensor(
                    out=mix[j][:, kc, :].rearrange("p (b s) -> p b s", b=2), in0=XX[:, kc], scalar=mu[:, m, kc:kc + 1],
                    in1=xnT[:, kc, :, 1:L + 1], op0=ALU.mult, op1=ALU.add),
                    reads=[t_XT, t_xnT, t_small], writes=[t_mix[j][kc]])

        _order = ['A', 'B', 'C', 'D', 'E1', 'E2', 'E3', 'E4', 'E5', 'F']
        en = lambda s: RW_STOP[0] is None or _order.index(s) <= _order.index(RW_STOP[0])

        def stage_AB(c):
            for b in range(2):
                cx.dma_sp(XT[64 * b:64 * b + 64, :], src[b * T + c * L:b * T + (c + 1) * L, :], writes=[t_XT])
            cx.act(lambda: nc.scalar.activation(out=YG[:], in_=XT[:], func=AF.Square, accum_out=sc[:, 0:1]),
                   reads=[t_XT], writes=[t_YG, t_sc])
            cx.act(lambda: nc.scalar.activation(out=sc[:, 1:2], in_=sc[:, 0:1], func=AF.Sqrt, bias=epsr[:, 0:1],
                                                scale=1.0 / D), reads=[t_sc, t_c], writes=[t_sc])
            cx.dve(lambda: nc.vector.reciprocal(out=sc[:, 1:2], in_=sc[:, 1:2]), reads=[t_sc], writes=[t_sc])
            cx.dve(lambda: nc.vector.tensor_scalar(out=XT[:], in0=XT[:], scalar1=sc[:, 1:2], scalar2=None,
                                                   op0=ALU.mult), reads=[t_XT, t_sc], writes=[t_XT])
            if c == 0:
                P(lambda: g.memset(xnT[:, :, :, 0:1], 0.0), writes=[t_xnT])
            else:
                P(lambda: g.tensor_copy(out=xnT[:, :, :, 0:1], in_=xnT[:, :, :, L:L + 1]), reads=[t_xnT], writes=[t_xnT])
            PTv = PS[:, 0:2, :].rearrange("p a (k t) -> p (a k) t", t=128)
            for kc in range(NKC):
                cx.pe(lambda kc=kc: nc.tensor.transpose(out=PTv[:, kc, :], in_=XT[:, kc * 128:(kc + 1) * 128],
                                                        identity=identf[:]),
                      reads=[t_XT, t_c], writes=[t_bank[kc // 4]])
            cx.dve(lambda: nc.vector.tensor_tensor(
                out=xnT[:, :, :, 1:L + 1], in0=PTv.rearrange("p k (b s) -> p k b s", b=2),
                in1=gcol[:, :].unsqueeze(2).unsqueeze(3).to_broadcast([128, NKC, 2, L]), op=ALU.mult),
                reads=[t_bank[0], t_bank[1], t_small], writes=[t_xnT])
            cx.dve(lambda: nc.vector.tensor_tensor(
                out=XT[:, :].rearrange("p (k b s) -> p k b s", k=NKC, b=2), in0=xnT[:, :, :, 0:L],
                in1=xnT[:, :, :, 1:L + 1], op=ALU.subtract), reads=[t_xnT], writes=[t_XT])

            if en('B'):
                pass
                def rkv(m, q, dstt, tdst, j):
                    make_mix(m, j)
                    for half in range(2):
                        bk = 2 + half
                        proj512(lambda k: mix[j][:, k, :], lambda k: Wrkvo[:, q, k, half * 512:(half + 1) * 512], NKC, bk,
                                lambda k: [t_mix[j][k], t_W[q]])
                        evac_copy(dstt[:, half * 512:(half + 1) * 512], bank(bk), [t_bank[bk]], [tdst])
                rkv(0, 0, Rt, t_R, 0)
                rkv(2, 1, Kt, t_K, 1)
                rkv(3, 2, Vt, t_V, 0)
                P(lambda: g.tensor_copy(out=Vb[:], in_=Vt[:]), reads=[t_V], writes=[t_Vb])
                make_mix(1, 1)
                for k in range(NKC):
                    cx.pe(lambda k=k: nc.tensor.matmul(PS[0:64, 4, 0:128], lhsT=W1[:, k, :], rhs=mix[1][:, k, :],
                                                       start=(k == 0), stop=(k == NKC - 1)),
                          reads=[t_mix[1][k], t_lw], writes=[t_bank[4]])
                cx.act(lambda: nc.scalar.activation(out=thw[:], in_=PS[0:64, 4, 0:128], func=AF.Tanh),
                       reads=[t_bank[4]], writes=[t_thw])
                for half in range(2):
                    bk = 2 + half
                    cx.pe(lambda: nc.tensor.matmul(bank(bk), lhsT=thw[:], rhs=W2[:, half * 512:(half + 1) * 512],
                                                   start=True, stop=True), reads=[t_thw, t_lw], writes=[t_bank[bk]])
                    cx.dve(lambda: nc.vector.tensor_tensor(out=SW[:, half * 512:(half + 1) * 512], in0=bank(bk),
                                                           in1=VB[:, 0, half * 512:(half + 1) * 512], op=ALU.add),
                           reads=[t_bank[bk], t_vb], writes=[t_SW])
                cx.act(lambda: nc.scalar.activation(out=SW[:], in_=SW[:], func=AF.Sigmoid), reads=[t_SW], writes=[t_SW])
                make_mix(4, 0)
                for k in range(NKC):
                    cx.pe(lambda k=k: nc.tensor.matmul(PS[0:64, 5, 0:128], lhsT=A1[:, k, :], rhs=mix[0][:, k, :],
                                                       start=(k == 0), stop=(k == NKC - 1)),
                          reads=[t_mix[0][k], t_lw], writes=[t_bank[5]])
                cx.act(lambda: nc.scalar.copy(out=haT[:], in_=PS[0:64, 5, 0:128]), reads=[t_bank[5]], writes=[t_haT])
                for half in range(2):
                    bk = 2 + half
                    cx.pe(lambda: nc.tensor.matmul(bank(bk), lhsT=haT[:], rhs=A2[:, half * 512:(half + 1) * 512],
                                                   start=True, stop=True), reads=[t_haT, t_lw], writes=[t_bank[bk]])
                    cx.dve(lambda: nc.vector.tensor_tensor(out=AS[:, half * 512:(half + 1) * 512], in0=bank(bk),
                                                           in1=VB[:, 1, half * 512:(half + 1) * 512], op=ALU.add),
                           reads=[t_bank[bk], t_vb], writes=[t_AS])
                cx.act(lambda: nc.scalar.activation(out=AS[:], in_=AS[:], func=AF.Sigmoid), reads=[t_AS], writes=[t_AS])
                make_mix(5, 1)
                for k in range(NKC):
                    cx.pe(lambda k=k: nc.tensor.matmul(PS[:, 4, 0:128], lhsT=G1[:, k, 0:128], rhs=mix[1][:, k, :],
                                                       start=(k == 0), stop=(k == NKC - 1)),
                          reads=[t_mix[1][k], t_lw], writes=[t_bank[4]])
                for k in range(NKC):
                    cx.pe(lambda k=k: nc.tensor.matmul(PS[0:32, 5, 0:128], lhsT=G1[:, k, 128:160], rhs=mix[1][:, k, :],
                                                       start=(k == 0), stop=(k == NKC - 1)),
                          reads=[t_mix[1][k], t_lw], writes=[t_bank[5]])
                sgT, sgT2, t_sgT = sgT_[c % 2], sgT2_[c % 2], t_sgT_[c % 2]
                cx.act(lambda: nc.scalar.activation(out=sgT[:], in_=PS[:, 4, 0:128], func=AF.Sigmoid),
                       reads=[t_bank[4]], writes=[t_sgT])
                cx.act(lambda: nc.scalar.activation(out=sgT2[:], in_=PS[0:32, 5, 0:128], func=AF.Sigmoid),
                       reads=[t_bank[5]], writes=[t_sgT])

        def stage_CDE(c):
            if en('C'):
                pass
                P(lambda: g.tensor_tensor(out=KK[:], in0=Kt[:], in1=VB[:, 2, :], op=ALU.mult),
                  reads=[t_K, t_vb], writes=[t_KK])
                cx.dve(lambda: nc.vector.tensor_tensor(out=XT[:], in0=KK[:], in1=KK[:], op=ALU.mult),
                       reads=[t_KK], writes=[t_XT])
                cx.dve(lambda: nc.vector.tensor_reduce(out=st[:, 0, :], in_=V3(XT), axis=AX.X, op=ALU.add),
                       reads=[t_XT], writes=[t_st])
                cx.act(lambda: nc.scalar.activation(out=st[:, 0, :], in_=st[:, 0, :], func=AF.Sqrt),
                       reads=[t_st], writes=[t_st])
                cx.dve(lambda: nc.vector.tensor_scalar_max(out=st[:, 0, :], in0=st[:, 0, :], scalar1=1e-12),
                       reads=[t_st], writes=[t_st])
                cx.dve(lambda: nc.vector.reciprocal(out=st[:, 0, :], in_=st[:, 0, :]), reads=[t_st], writes=[t_st])
                cx.dve(lambda: nc.vector.tensor_tensor(out=V3(KK), in0=V3(KK), in1=bc_h(st[:, 0, :]), op=ALU.mult),
                       reads=[t_KK, t_st], writes=[t_KK])
                cx.dve(lambda: nc.vector.scalar_tensor_tensor(out=XT[:], in0=AS[:], scalar=-1.0, in1=VB[:, 3, :],
                                                              op0=ALU.add, op1=ALU.mult),
                       reads=[t_AS, t_vb], writes=[t_XT])
                cx.dve(lambda: nc.vector.scalar_tensor_tensor(out=Kt[:], in0=XT[:], scalar=1.0, in1=Kt[:],
                                                              op0=ALU.add, op1=ALU.mult),
                       reads=[t_XT, t_K], writes=[t_K])
                P(lambda: g.tensor_tensor(out=AS[:], in0=AS[:], in1=KK[:], op=ALU.mult), reads=[t_AS, t_KK], writes=[t_AS])
                P(lambda: g.tensor_tensor(out=XT[:], in0=Rt[:], in1=Kt[:], op=ALU.mult), reads=[t_R, t_K], writes=[t_XT])
                P(lambda: g.tensor_tensor(out=XT[:], in0=XT[:], in1=VB[:, 4, :], op=ALU.mult), reads=[t_XT, t_vb], writes=[t_XT])
                cx.dve(lambda: nc.vector.tensor_reduce(out=st[:, 1, :], in_=V3(XT), axis=AX.X, op=ALU.add),
                       reads=[t_XT], writes=[t_st])
                WLp = PS[:, 6, 0:H]
                for h in range(H):
                    for b in range(2):
                        cs = slice(64 * b, 64 * b + 64)
                        cx.pe(lambda: nc.tensor.matmul(WLp[cs, h:h + 1], lhsT=SW[cs, h * HS:(h + 1) * HS], rhs=ones1[cs, 0:1],
                                                       start=True, stop=True), reads=[t_SW, t_c], writes=[t_bank[6]])
                cx.act(lambda: nc.scalar.activation(out=WL[:], in_=WLp, func=AF.Exp, scale=-C0),
                       reads=[t_bank[6]], writes=[t_WL])
                for half in range(2):
                    hs = slice(half * 512, (half + 1) * 512)
                    bkI, bkE, bkT = (2, 3, 4) if half == 0 else (5, 6, 2)
                    cx.pe(lambda: nc.tensor.matmul(bank(bkI), lhsT=triI[:], rhs=SW[:, hs], start=True, stop=True),
                          reads=[t_SW, t_c], writes=[t_bank[bkI]])
                    cx.pe(lambda: nc.tensor.matmul(bank(bkE), lhsT=triLt[:], rhs=SW[:, hs], start=True, stop=True),
                          reads=[t_SW, t_c], writes=[t_bank[bkE]])
                    cx.pe(lambda: nc.tensor.matmul(bank(bkT), lhsT=triGt[:], rhs=SW[:, hs], start=True, stop=True),
                          reads=[t_SW, t_c], writes=[t_bank[bkT]])
                    tmps = ((XT, t_XT), (X1, t_X1), (X2, t_X2))
                    E, tE = tmps[(4 * half + 0) % 3]
                    cx.act(lambda: nc.scalar.activation(out=E[:, hs], in_=bank(bkI), func=AF.Exp, scale=-C0),
                           reads=[t_bank[bkI]], writes=[tE])
                    cx.dve(lambda: nc.vector.tensor_tensor(out=RB[:, hs], in0=Rt[:, hs], in1=E[:, hs], op=ALU.mult),
                           reads=[t_R, tE], writes=[t_RB])
                    E, tE = tmps[(4 * half + 1) % 3]
                    cx.act(lambda: nc.scalar.activation(out=E[:, hs], in_=bank(bkI), func=AF.Exp, scale=C0),
                           reads=[t_bank[bkI]], writes=[tE])
                    cx.dve(lambda: nc.vector.tensor_tensor(out=KB[:, hs], in0=Kt[:, hs], in1=E[:, hs], op=ALU.mult),
                           reads=[t_K, tE], writes=[t_KB])
                    P(lambda: g.tensor_tensor(out=BB[:, hs], in0=AS[:, hs], in1=E[:, hs], op=ALU.mult),
                      reads=[t_AS, tE], writes=[t_BB])
                    E, tE = tmps[(4 * half + 2) % 3]
                    cx.act(lambda: nc.scalar.activation(out=E[:, hs], in_=bank(bkE), func=AF.Exp, scale=-C0),
                           reads=[t_bank[bkE]], writes=[tE])
                    cx.dve(lambda: nc.vector.scalar_tensor_tensor(out=AB[:, hs], in0=KK[:, hs], scalar=-1.0, in1=E[:, hs],
                                                                  op0=ALU.mult, op1=ALU.mult),
                           reads=[t_KK, tE], writes=[t_AB])
                    E, tE = tmps[(4 * half + 3) % 3]
                    cx.act(lambda: nc.scalar.activation(out=E[:, hs], in_=bank(bkT), func=AF.Exp, scale=-C0),
                           reads=[t_bank[bkT]], writes=[tE])
                    cx.dve(lambda: nc.vector.tensor_tensor(out=KH[:, hs], in0=Kt[:, hs], in1=E[:, hs], op=ALU.mult),
                           reads=[t_K, tE], writes=[t_KH])
                    P(lambda: g.tensor_tensor(out=BH[:, hs], in0=AS[:, hs], in1=E[:, hs], op=ALU.mult),
                      reads=[t_AS, tE], writes=[t_BH])

            if en('D'):
                pass
                srcs = ((AB, t_AB), (RB, t_RB), (BB, t_BB), (KB, t_KB))
                identq = identb if RDT == BF16 else identf
                for hp in range(NKC):
                    bk = hp % 2
                    PTq = bank(bk).rearrange("p (e q t) -> p e q t", e=2, q=4)
                    for hh in range(2):
                        h = 2 * hp + hh
                        for q, (tl, tt) in enumerate(srcs):
                            for b in range(2):
                                cs = slice(64 * b, 64 * b + 64)
                                cx.pe(lambda q=q, tl=tl: nc.tensor.matmul(
                                    PTq[cs, hh, q, :], lhsT=tl[cs, h * HS:(h + 1) * HS], rhs=identq[cs, cs],
                                    start=True, stop=True), reads=[tt, t_c], writes=[t_bank[bk]])
                    evac_copy(FT[:, 2 * hp:2 * hp + 2], PTq, [t_bank[bk]], [t_FT])

            if en('E1'):
                pass
                for grp in range(4):
                    b1, b2, b3 = (2, 3, 4) if grp % 2 == 0 else (5, 6, 7)
                    P1 = bank(b1).rearrange("p (h q t) -> p h q t", h=4, q=2)
                    P2 = bank(b2).rearrange("p (h q t) -> p h q t", h=4, q=2)
                    P3 = bank(b3)[:, 0:256].rearrange("p (h t) -> p h t", h=4)
                    for hl in range(4):
                        h = grp * 4 + hl
                        for b in range(2):
                            cs = slice(64 * b, 64 * b + 64)
                            cx.pe(lambda: nc.tensor.matmul(P1[cs, hl], lhsT=FT[cs, h, 2, :], rhs=FT[cs, h, 0:2, :],
                                                           start=True, stop=True), reads=[t_FT], writes=[t_bank[b1]])
                            cx.pe(lambda: nc.tensor.matmul(P2[cs, hl], lhsT=FT[cs, h, 3, :], rhs=FT[cs, h, 0:2, :],
                                                           start=True, stop=True), reads=[t_FT], writes=[t_bank[b2]])
                            cx.pe(lambda: nc.tensor.matmul(P3[cs, hl], lhsT=FT[cs, h, 0, :], rhs=FT[cs, h, 2, :],
                                                           start=True, stop=True), reads=[t_FT], writes=[t_bank[b3]])
                    hsl = slice(grp * 4, grp * 4 + 4)
                    mb = lambda m: m[:, :].unsqueeze(1).to_broadcast([128, 4, L])
                    cx.dve(lambda: nc.vector.tensor_tensor(out=AabT[:, hsl, :], in0=P1[:, :, 0, :], in1=mb(mST), op=ALU.mult),
                           reads=[t_bank[b1], t_c], writes=[t_AabT[grp // 2]])
                    cx.dve(lambda: nc.vector.tensor_tensor(out=ArbT[:, hsl, :], in0=P1[:, :, 1, :], in1=mb(mIT), op=ALU.mult),
                           reads=[t_bank[b1], t_c], writes=[t_ArbT[grp // 2]])
                    cx.dve(lambda: nc.vector.tensor_tensor(out=AakT[:, hsl, :], in0=P2[:, :, 0, :], in1=mb(mST), op=ALU.mult),
                           reads=[t_bank[b2], t_c], writes=[t_AakT[grp // 2]])
                    cx.dve(lambda: nc.vector.tensor_tensor(out=ArkT[:, hsl, :], in0=P2[:, :, 1, :], in1=mb(mIT), op=ALU.mult),
                           reads=[t_bank[b2], t_c], writes=[t_ArkT[grp // 2]])
                    cx.dve(lambda: nc.vector.tensor_tensor(out=GX[:, hsl, 0:L], in0=P3, in1=mb(mS), op=ALU.mult),
                           reads=[t_bank[b3], t_c], writes=[t_GX[grp // 2]])

            if en('E2'):
                pass
                PX = PS[:, 0:2, :].rearrange("p a (h i) -> p (a h) i", i=HS)
                for h in range(H):
                    for b in range(2):
                        cs = slice(64 * b, 64 * b + 64)
                        cx.pe(lambda: nc.tensor.matmul(PX[cs, h, :], lhsT=FT[cs, h, 0, :], rhs=MB[cs, h, :],
                                                       start=True, stop=False), reads=[t_FT, t_MB], writes=[t_bank[h // 8]])
                        cx.pe(lambda: nc.tensor.matmul(PX[cs, h, :], lhsT=AakT[cs, h, :], rhs=Vb[cs, h * HS:(h + 1) * HS],
                                                       start=False, stop=True), reads=[t_AakT[h // 8], t_Vb], writes=[t_bank[h // 8]])
                for hf in range(2):
                    hsl = slice(8 * hf, 8 * hf + 8)
                    cx.act(lambda: nc.scalar.copy(out=GX[:, hsl, L:2 * L], in_=PX[:, hsl, :]), reads=[t_bank[hf]], writes=[t_GX[hf]])

            if en('E3'):
                pass
                PA = PS[:, 2:6, :].rearrange("p a (h t) -> p (a h) t", t=2 * L)
                PB = PS[:, 6:8, :].rearrange("p a (h t) -> p (a h) t", t=L)
                NLEV = 6
                for lev in range(NLEV):
                    last = (lev == NLEV - 1)
                    for hf in range(2):
                        hsl = slice(8 * hf, 8 * hf + 8)
                        for h in range(8 * hf, 8 * hf + 8):
                            for b in range(2):
                                cs = slice(64 * b, 64 * b + 64)
                                if not last:
                                    cx.pe(lambda: nc.tensor.matmul(PA[cs, h, :], lhsT=AabT[cs, h, :], rhs=GX[cs, h, :],
                                                                   start=True, stop=True),
                                          reads=[t_AabT[hf], t_GX[hf]], writes=[t_bank[2 + h // 4]])
                                    cx.pe(lambda: nc.tensor.matmul(PB[cs, h, :], lhsT=GX[cs, h, 0:L], rhs=AabT[cs, h, :],
                                                                   start=True, stop=True),
                                          reads=[t_AabT[hf], t_GX[hf]], writes=[t_bank[6 + hf]])
                                else:
                                    cx.pe(lambda: nc.tensor.matmul(PA[cs, h, L:2 * L], lhsT=AabT[cs, h, :], rhs=GX[cs, h, L:2 * L],
                                                                   start=True, stop=True),
                                          reads=[t_AabT[hf], t_GX[hf]], writes=[t_bank[2 + h // 4]])
                        pa_t = [t_bank[2 + 2 * hf], t_bank[3 + 2 * hf]]
                        cx.dve(lambda: nc.vector.tensor_tensor(out=GX[:, hsl, L:2 * L], in0=PA[:, hsl, L:2 * L],
                                                               in1=GX[:, hsl, L:2 * L], op=ALU.add),
                               reads=pa_t + [t_GX[hf]], writes=[t_GX[hf]])
                        if not last:
                            cx.act(lambda: nc.scalar.copy(out=GX[:, hsl, 0:L], in_=PA[:, hsl, 0:L]), reads=pa_t, writes=[t_GX[hf]])
                            cx.act(lambda: nc.scalar.copy(out=AabT[:, hsl, :], in_=PB[:, hsl, :]), reads=[t_bank[6 + hf]],
                                   writes=[t_AabT[hf]])

            if en('E4'):
                pass
                PY = PX
                for h in range(H):
                    for b in range(2):
                        cs = slice(64 * b, 64 * b + 64)
                        cx.pe(lambda: nc.tensor.matmul(PY[cs, h, :], lhsT=FT[cs, h, 1, :], rhs=MB[cs, h, :],
                                                       start=True, stop=False), reads=[t_FT, t_MB], writes=[t_bank[h // 8]])
                        cx.pe(lambda: nc.tensor.matmul(PY[cs, h, :], lhsT=ArbT[cs, h, :], rhs=GX[cs, h, L:2 * L],
                                                       start=False, stop=False), reads=[t_ArbT[h // 8], t_GX[h // 8]], writes=[t_bank[h // 8]])
                        cx.pe(lambda: nc.tensor.matmul(PY[cs, h, :], lhsT=ArkT[cs, h, :], rhs=Vb[cs, h * HS:(h + 1) * HS],
                                                       start=False, stop=True), reads=[t_ArkT[h // 8], t_Vb], writes=[t_bank[h // 8]])
                cx.act(lambda: nc.scalar.copy(out=V3(X1), in_=PY), reads=[t_bank[0], t_bank[1]], writes=[t_X1])

            if en('E5'):
                pass
                PM = PS[:, 2:4, :].rearrange("p a (h i) -> p (a h) i", i=HS)
                for h in range(H):
                    for b in range(2):
                        cs = slice(64 * b, 64 * b + 64)
                        cx.pe(lambda: nc.tensor.matmul(PM[cs, h, :], lhsT=BH[cs, h * HS:(h + 1) * HS], rhs=GX[cs, h, L:2 * L],
                                                       start=True, stop=False), reads=[t_BH, t_GX[h // 8]], writes=[t_bank[2 + h // 8]])
                        cx.pe(lambda: nc.tensor.matmul(PM[cs, h, :], lhsT=KH[cs, h * HS:(h + 1) * HS], rhs=Vb[cs, h * HS:(h + 1) * HS],
                                                       start=False, stop=True), reads=[t_KH, t_Vb], writes=[t_bank[2 + h // 8]])
                cx.dve(lambda: nc.vector.tensor_tensor(out=M[:], in0=M[:], in1=WL[:, :].unsqueeze(2).to_broadcast([128, H, HS]),
                                                       op=ALU.mult), reads=[t_M, t_WL], writes=[t_M])
                cx.dve(lambda: nc.vector.tensor_tensor(out=M[:], in0=M[:], in1=PM, op=ALU.add),
                       reads=[t_M, t_bank[2], t_bank[3]], writes=[t_M])
                cx.act(lambda: nc.scalar.copy(out=MB[:], in_=M[:]), reads=[t_M], writes=[t_MB])

            if en('F'):
                P(lambda: g.tensor_tensor(out=V3(KK), in0=V3(Vt), in1=bc_h(st[:, 1, :]), op=ALU.mult),
                  reads=[t_V, t_st, t_KK], writes=[t_KK])

        def stage_F(c):
            if en('F'):
                pass
                Y = X1
                cx.dve(lambda: nc.vector.tensor_reduce(out=st[:, 2, :], in_=V3(Y), axis=AX.X, op=ALU.add),
                       reads=[t_X1], writes=[t_st])
                cx.dve(lambda: nc.vector.scalar_tensor_tensor(out=V3(Y), in0=bc_h(st[:, 2, :]), scalar=-1.0 / HS, in1=V3(Y),
                                                              op0=ALU.mult, op1=ALU.add), reads=[t_X1, t_st], writes=[t_X1])
                cx.dve(lambda: nc.vector.tensor_tensor(out=X2[:], in0=Y[:], in1=Y[:], op=ALU.mult), reads=[t_X1], writes=[t_X2])
                cx.dve(lambda: nc.vector.tensor_reduce(out=st[:, 3, :], in_=V3(X2), axis=AX.X, op=ALU.add),
                       reads=[t_X2], writes=[t_st])
                cx.act(lambda: nc.scalar.activation(out=st[:, 3, :], in_=st[:, 3, :], func=AF.Sqrt, bias=epsg[:, 0:1],
                                                    scale=1.0 / HS), reads=[t_st, t_c], writes=[t_st])
                cx.dve(lambda: nc.vector.reciprocal(out=st[:, 3, :], in_=st[:, 3, :]), reads=[t_st], writes=[t_st])
                cx.dve(lambda: nc.vector.tensor_tensor(out=V3(Y), in0=V3(Y), in1=bc_h(st[:, 3, :]), op=ALU.mult),
                       reads=[t_X1, t_st], writes=[t_X1])
                cx.dve(lambda: nc.vector.tensor_tensor(out=Y[:], in0=Y[:], in1=VB[:, 5, :], op=ALU.mult), reads=[t_X1, t_vb], writes=[t_X1])
                cx.dve(lambda: nc.vector.tensor_tensor(out=Y[:], in0=Y[:], in1=VB[:, 6, :], op=ALU.add), reads=[t_X1, t_vb], writes=[t_X1])
                cx.dve(lambda: nc.vector.tensor_tensor(out=Y[:], in0=Y[:], in1=KK[:], op=ALU.add),
                       reads=[t_X1, t_KK], writes=[t_X1])
                sgT, sgT2, t_sgT = sgT_[c % 2], sgT2_[c % 2], t_sgT_[c % 2]
                for half in range(2):
                    bk = half
                    hs = slice(half * 512, (half + 1) * 512)
                    cx.pe(lambda: nc.tensor.matmul(bank(bk), lhsT=sgT[:], rhs=G2a[:, hs], start=True, stop=False),
                          reads=[t_sgT, t_lw], writes=[t_bank[bk]])
                    cx.pe(lambda: nc.tensor.matmul(bank(bk), lhsT=sgT2[:], rhs=G2b[:, hs], start=False, stop=True),
                          reads=[t_sgT, t_lw], writes=[t_bank[bk]])
                    cx.dve(lambda: nc.vector.tensor_tensor(out=YG[:, hs], in0=Y[:, hs], in1=bank(bk), op=ALU.mult),
                           reads=[t_X1, t_bank[bk]], writes=[t_YG])
                PTb = PS[:, 7, :].bitcast(BF16)[:, 0:NKC * 128].rearrange("p (k t) -> p k t", t=128)
                for kc in range(NKC):
                    cx.pe(lambda kc=kc: nc.tensor.transpose(out=PTb[:, kc, :], in_=YG[:, kc * 128:(kc + 1) * 128],
                                                            identity=identb[:]), reads=[t_YG, t_c], writes=[t_bank[7]])
                cx.act(lambda: nc.scalar.copy(out=ygT[:], in_=PTb), reads=[t_bank[7]], writes=[t_ygT])
            for b in range(2):
                cx.dma_sp(XT[64 * b:64 * b + 64, :], src[b * T + c * L:b * T + (c + 1) * L, :], writes=[t_XT])
            for half in range(2 if en('F') else 0):
                bk = half
                hs = slice(half * 512, (half + 1) * 512)
                proj512(lambda k: ygT[:, k, :], lambda k: Wrkvo[:, 3, k, hs], NKC, bk, [t_ygT, t_W[3]])
                cx.dve(lambda: nc.vector.tensor_tensor(out=XT[:, hs], in0=XT[:, hs], in1=bank(bk), op=ALU.add),
                       reads=[t_XT, t_bank[bk]], writes=[t_XT])
            for b in range(2):
                cx.dma_sp(dst[b * T + c * L:b * T + (c + 1) * L, :], XT[64 * b:64 * b + 64, :], reads=[t_XT])

        stage_AB(0)
        for c in range(nchunks):
            stage_CDE(c)
            if c + 1 < nchunks:
                stage_AB(c + 1)
            stage_F(c)
        cx.phase_end()


def lay_gu(w):
    return np.ascontiguousarray(w.reshape(NKC, 128, NFC, 128).transpose(1, 2, 0, 3))


def lay_rows(w):
    r = w.shape[0] // 128
    return np.ascontiguousarray(w.reshape(r, 128, w.shape[1]).transpose(1, 0, 2))


def lay_col(v):
    return np.ascontiguousarray(v.reshape(-1, 128).T)


def build_program(ntok, phases, T=2048):
    nc = bass.Bass("TRN2", target_bir_lowering=False)
    dram_in = lambda n, s: nc.dram_tensor(n, s, F32, kind="ExternalInput").ap()
    x = dram_in("x", [ntok, D])
    out = nc.dram_tensor("out", [ntok, D], F32, kind="ExternalOutput").ap()
    scr = nc.dram_tensor("scr", [ntok, D], F32, kind="Internal").ap()
    wg = dram_in("ffn_wg", [4, 128, NFC, NKC, 128])
    wu = dram_in("ffn_wu", [4, 128, NFC, NKC, 128])
    wd = dram_in("ffn_wd", [4, 128, NFC, D])
    gcols = dram_in("gcols", [6, 128, NKC])
    fnorm = dram_in("final_norm", [D])
    cw_in = dram_in("conv_win", [128, 16, NKC, 128])
    cw_out = dram_in("conv_wout", [128, NKC, D])
    c_small = dram_in("conv_small", [128, 288])
    c_bout = dram_in("conv_bout", [D])
    rw_rkvo = dram_in("rw_rkvo", [4, 128, NKC, D])
    rw_w1 = dram_in("rw_w1", [128, NKC, 64])
    rw_a1 = dram_in("rw_a1", [128, NKC, 64])
    rw_g1 = dram_in("rw_g1", [128, NKC, 160])
    rw_w2 = dram_in("rw_w2", [64, D])
    rw_a2 = dram_in("rw_a2", [64, D])
    rw_g2 = dram_in("rw_g2", [160, D])
    rw_vecs = dram_in("rw_vecs", [7, D])
    rw_mu = dram_in("rw_mu", [128, 6, NKC])
    with contextlib.ExitStack() as es:
        cx = Ctx(nc, es)
        n = len(phases)
        for pi, ph in enumerate(phases):
            src = x if pi == 0 else scr
            dst = out if pi == n - 1 else scr
            if ph.startswith("ffn"):
                k = int(ph[3])
                layer, which = divmod(k, 2)
                gidx = layer * 3 + (0 if which == 0 else 2)
                ffn_phase(cx, src, dst, ntok, wg[k], wu[k], wd[k], gcols[gidx],
                          final_gain=(fnorm if ph.endswith("f") else None))
            elif ph == "rwkv":
                rwkv_phase(cx, src, dst, ntok, T, rw_rkvo, rw_w1, rw_a1, rw_g1, rw_w2, rw_a2, rw_g2, rw_vecs, rw_mu,
                           gcols[1])
            elif ph == "conv":
                conv_phase(cx, src, dst, ntok, T, cw_in, cw_out, c_small, c_bout, gcols[4])
            else:
                raise ValueError(ph)
    return nc


def prep_weights(inp):
    w = {}
    g = np.asarray(inp["ffn_w_gate"], np.float32)
    u = np.asarray(inp["ffn_w_up"], np.float32)
    dn = np.asarray(inp["ffn_w_down"], np.float32)
    w["ffn_wg"] = np.stack([lay_gu(g[l, j]) for l in range(2) for j in range(2)])
    w["ffn_wu"] = np.stack([lay_gu(u[l, j]) for l in range(2) for j in range(2)])
    w["ffn_wd"] = np.stack([lay_rows(dn[l, j]) for l in range(2) for j in range(2)])
    ng = np.asarray(inp["norm_gains"], np.float32)
    w["gcols"] = np.stack([lay_col(ng[l, j]) for l in range(2) for j in range(3)])
    w["final_norm"] = np.ascontiguousarray(np.asarray(inp["final_norm"], np.float32))
    cwi = np.asarray(inp["conv_w_in"], np.float32)[0]
    w["conv_win"] = np.ascontiguousarray(cwi.reshape(NKC, 128, 16, 128).transpose(1, 2, 0, 3))
    w["conv_wout"] = lay_rows(np.asarray(inp["conv_w_out"], np.float32)[0])
    dw = np.asarray(inp["conv_dw"], np.float32)[0]
    dwcol = dw.T.reshape(NKC, 128, CONVW).transpose(1, 0, 2).reshape(128, NKC * CONVW)
    w["conv_small"] = np.ascontiguousarray(np.concatenate([
        lay_col(np.asarray(inp["conv_b_in"], np.float32)[0]),
        lay_col(np.asarray(inp["conv_dw_b"], np.float32)[0]),
        lay_col(np.asarray(inp["conv_ln_gain"], np.float32)[0]),
        lay_col(np.asarray(inp["conv_ln_bias"], np.float32)[0]),
        dwcol], axis=1))
    w["conv_bout"] = np.ascontiguousarray(np.asarray(inp["conv_b_out"], np.float32)[0])
    f = lambda k: np.asarray(inp[k], np.float32)[0]
    rkv = f("rwkv_w_rkv")
    w["rw_rkvo"] = np.stack([lay_rows(rkv[0]), lay_rows(rkv[1]), lay_rows(rkv[2]), lay_rows(f("rwkv_w_out"))])
    w["rw_w1"] = lay_rows(f("rwkv_w1"))
    w["rw_a1"] = lay_rows(f("rwkv_a1"))
    w["rw_g1"] = lay_rows(f("rwkv_g1"))
    w["rw_w2"] = np.ascontiguousarray(f("rwkv_w2"))
    w["rw_a2"] = np.ascontiguousarray(f("rwkv_a2"))
    w["rw_g2"] = np.ascontiguousarray(f("rwkv_g2"))
    w["rw_vecs"] = np.ascontiguousarray(np.stack([f("rwkv_w0"), f("rwkv_a0"), f("rwkv_k_k"), f("rwkv_k_a"),
                                                  f("rwkv_r_k").reshape(D), f("rwkv_ln_gain"), f("rwkv_ln_bias")]))
    muv = f("rwkv_mu")
    w["rw_mu"] = np.ascontiguousarray(muv.reshape(6, NKC, 128).transpose(2, 0, 1))
    return w


ALL_PHASES = ["ffn0", "rwkv", "ffn1", "ffn2", "conv", "ffn3f"]


def kernel(**inputs):
    x = np.asarray(inputs["x"], np.float32)
    B, T, _ = x.shape
    nb = B // NCORES
    ntok = nb * T
    w = prep_weights(inputs)
    nc = build_program(ntok, ALL_PHASES, T=T)
    in_maps = []
    for c in range(NCORES):
        m = dict(w)
        m["x"] = np.ascontiguousarray(x[c * nb:(c + 1) * nb].reshape(ntok, D))
        in_maps.append(m)
    res = run_bass_kernel_spmd(nc, in_maps, core_ids=list(range(NCORES)))
    outs = [np.asarray(r["out"]).reshape(nb, T, D) for r in res.results]
    return np.concatenate(outs, axis=0).astype(np.float32)
```

```python
import bisect
import contextlib
import math

import numpy as np
import concourse.bass as bass
import concourse.mybir as mybir
from concourse.bass_utils import run_bass_kernel_spmd

F32 = mybir.dt.float32
BF16 = mybir.dt.bfloat16
AF = mybir.ActivationFunctionType
ALU = mybir.AluOpType
AX = mybir.AxisListType

D = 1024
DFF = 2816
NFC = DFF // 128
NKC = D // 128
H = 16
HS = 64
CONVW = 31
RMS_EPS = 1e-6
LN_EPS = 1e-5
GN_EPS = 64e-5
NCORES = 8

SAME_ENGINE_SYNC = True
_PH = [0]


def _pfx():
    _PH[0] += 1
    return f"p{_PH[0]}_"


class Trk:
    __slots__ = ("name", "writer", "readers")

    def __init__(self, name=""):
        self.name = name
        self.writer = None
        self.readers = {}


class Lane:
    def __init__(self, name, eng, sem, inc):
        self.name = name
        self.eng = eng
        self.sem = sem
        self.inc = inc
        self.n = 0
        self.count = 0
        self.sig_idx = []
        self.sig_val = []
        self.last = None
        self.last_sig = True
        self.seen = {}

    def ticket(self, idx):
        p = bisect.bisect_left(self.sig_idx, idx)
        if p < len(self.sig_idx):
            return self.sig_val[p]
        assert self.last is not None and not self.last_sig and self.n - 1 >= idx, (self.name, idx, self.n)
        self.last.then_inc(self.sem, self.inc)
        self.count += self.inc
        self.sig_idx.append(self.n - 1)
        self.sig_val.append(self.count)
        self.last_sig = True
        return self.count


class Sched:
    def __init__(self, nc):
        self.nc = nc

    def _wait(self, issuer, lane, idx):
        if lane is issuer and (not SAME_ENGINE_SYNC or lane.name == "pe"):
            return
        t = lane.ticket(idx)
        if issuer.seen.get(lane, 0) >= t:
            return
        issuer.seen[lane] = t
        issuer.eng.wait_ge(lane.sem, t)

    def _deps(self, issuer, reads, writes):
        for b in reads:
            if b.writer is not None:
                self._wait(issuer, *b.writer)
        for b in writes:
            relax = False
            if b.writer is not None and not (relax and b.writer[0] is issuer):
                self._wait(issuer, *b.writer)
            for ln, idx in b.readers.items():
                if not (relax and ln is issuer):
                    self._wait(issuer, ln, idx)

    def emit(self, lane, fn, reads=(), writes=()):
        self._deps(lane, reads, writes)
        ins = fn()
        idx = lane.n
        lane.n += 1
        lane.last = ins
        lane.last_sig = False
        for b in reads:
            b.readers[lane] = idx
        for b in writes:
            b.writer = (lane, idx)
            b.readers = {}
        return ins

    def dma(self, issuer, slot, fn, reads=(), writes=()):
        if slot.n > 0:
            t = slot.sig_val[-1]
            if issuer.seen.get(slot, 0) < t:
                issuer.seen[slot] = t
                issuer.eng.wait_ge(slot.sem, t)
        self._deps(issuer, reads, writes)
        if issuer.last is not None and not issuer.last_sig:
            issuer.ticket(issuer.n - 1)
        ins = fn()
        ins.then_inc(slot.sem, 16)
        idx = slot.n
        slot.n += 1
        slot.count += 16
        slot.sig_idx.append(idx)
        slot.sig_val.append(slot.count)
        issuer.n += 1
        issuer.last = None
        issuer.last_sig = True
        for b in reads:
            b.readers[slot] = idx
        for b in writes:
            b.writer = (slot, idx)
            b.readers = {}
        return ins

    def drain(self, issuer, slots):
        for slot in slots:
            if slot.n > 0:
                t = slot.sig_val[-1]
                if issuer.seen.get(slot, 0) < t:
                    issuer.seen[slot] = t
                    issuer.eng.wait_ge(slot.sem, t)


class Ctx:
    def __init__(self, nc, es):
        self.nc = nc
        self.S = Sched(nc)
        sem = lambda n: es.enter_context(nc.semaphore(n))
        self.PE = Lane("pe", nc.tensor, sem("s_pe"), 1)
        self.ACT = Lane("act", nc.scalar, sem("s_act"), 1)
        self.DVE = Lane("dve", nc.vector, sem("s_dve"), 1)
        self.POOL = Lane("pool", nc.gpsimd, sem("s_pool"), 1)
        self.SP = Lane("sp", nc.sync, sem("s_sp"), 1)
        self.slots_sp = [Lane(f"qs{i}", None, sem(f"s_qs{i}"), 16) for i in range(8)]
        self.slots_pl = [Lane(f"qp{i}", None, sem(f"s_qp{i}"), 16) for i in range(8)]
        self.rr_sp = 0
        self.rr_pl = 0

    def dma_sp(self, out, in_, reads=(), writes=()):
        slot = self.slots_sp[self.rr_sp % len(self.slots_sp)]
        self.rr_sp += 1
        nc = self.nc
        return self.S.dma(self.SP, slot, lambda: nc.sync.dma_start(out=out, in_=in_), reads, writes)

    def dma_pl(self, out, in_, reads=(), writes=()):
        slot = self.slots_pl[self.rr_pl % len(self.slots_pl)]
        self.rr_pl += 1
        nc = self.nc
        return self.S.dma(self.POOL, slot, lambda: nc.gpsimd.dma_start(out=out, in_=in_), reads, writes)

    def pe(self, fn, reads=(), writes=()):
        return self.S.emit(self.PE, fn, reads, writes)

    def act(self, fn, reads=(), writes=()):
        return self.S.emit(self.ACT, fn, reads, writes)

    def dve(self, fn, reads=(), writes=()):
        return self.S.emit(self.DVE, fn, reads, writes)

    def pool(self, fn, reads=(), writes=()):
        return self.S.emit(self.POOL, fn, reads, writes)

    def phase_end(self):
        self.S.drain(self.SP, self.slots_sp + self.slots_pl)
        self.nc.all_engine_barrier()


def tok_view(ap, r0, nrows):
    return ap[r0:r0 + nrows, :].rearrange("(s p) d -> p s d", p=128)


def ffn_phase(cx, src, dst, ntok, wg, wu, wd, gcol_ap, final_gain=None):
    nc = cx.nc
    TT = 256
    NS = TT // 128
    ntiles = ntok // TT
    with contextlib.ExitStack() as es:
        pf = _pfx()
        sb = lambda n, s, d: es.enter_context(nc.sbuf_tensor(pf + n, s, d))
        ps = lambda n, s, d: es.enter_context(nc.psum_tensor(pf + n, s, d))
        Wg = sb("Wg", [128, NFC, NKC, 128], BF16)
        Wu = sb("Wu", [128, NFC, NKC, 128], BF16)
        Wd = sb("Wd", [128, NFC, D], BF16)
        t_wg = [Trk() for _ in range(NFC)]
        t_wu = [Trk() for _ in range(NFC)]
        t_wd = [Trk() for _ in range(NFC)]
        xb = [sb(f"xb{i}", [128, NS, D], F32) for i in range(2)]
        t_xb = [Trk(), Trk()]
        xn = sb("xn", [128, NS, D], BF16)
        t_xn = Trk()
        junk = sb("junk", [128, D], BF16)
        t_junk = Trk()
        xnT = [sb(f"xnT{i}", [128, NKC, TT], BF16) for i in range(2)]
        t_xnT = [Trk(), Trk()]
        hT = [sb(f"hT{i}", [128, NFC, TT], BF16) for i in range(2)]
        t_hT = [Trk(), Trk()]
        sg = [sb(f"sg{i}", [128, TT], F32) for i in range(2)]
        t_sg = [Trk(), Trk()]
        ss = sb("ss", [128, 4], F32)
        t_ss = Trk()
        rstd = sb("rstd", [128, 4], F32)
        t_rstd = Trk()
        gcol = sb("gcol", [128, NKC], F32)
        t_gcol = Trk()
        ident = sb("ident", [128, 128], BF16)
        t_ident = Trk()
        epsb = sb("epsb", [128, 1], F32)
        t_eps = Trk()
        if final_gain is not None:
            fg = sb("fg", [128, D], F32)
            t_fg = Trk()
        pT = ps("pT", [128, NKC, TT], BF16)
        t_pT = Trk()
        pgu = [ps(f"pgu{i}", [128, 2, TT], F32) for i in range(2)]
        t_pgu = [Trk(), Trk()]
        py = [ps(f"py{i}", [128, 512], F32) for i in range(2)]
        t_py = [Trk(), Trk()]

        cx.pool(lambda: nc.gpsimd.memset(ident[:], 0.0), writes=[t_ident])
        cx.pool(lambda: nc.gpsimd.affine_select(
            out=ident[:], in_=ident[:], pattern=[[-1, 128]], compare_op=ALU.not_equal,
            fill=1.0, base=0, channel_multiplier=1), reads=[t_ident], writes=[t_ident])
        cx.pool(lambda: nc.gpsimd.memset(epsb[:], RMS_EPS), writes=[t_eps])
        cx.dma_sp(gcol[:], gcol_ap, writes=[t_gcol])
        if final_gain is not None:
            cx.dma_sp(fg[:], final_gain.partition_broadcast(128), writes=[t_fg])

        def load_x(i):
            cx.dma_sp(xb[i % 2][:], tok_view(src, i * TT, TT), writes=[t_xb[i % 2]])

        def load_w():
            for fc in range(NFC):
                cx.dma_pl(Wg[:, fc], wg[:, fc], writes=[t_wg[fc]])
                cx.dma_pl(Wu[:, fc], wu[:, fc], writes=[t_wu[fc]])
            for fc in range(NFC):
                cx.dma_pl(Wd[:, fc], wd[:, fc], writes=[t_wd[fc]])

        def norm(i):
            xt = xb[i % 2]
            tx = t_xb[i % 2]
            for s in range(NS):
                cx.act(lambda s=s: nc.scalar.activation(out=junk[:], in_=xt[:, s, :], func=AF.Square,
                                                        accum_out=ss[:, s:s + 1]),
                       reads=[tx], writes=[t_junk, t_ss])
            cx.act(lambda: nc.scalar.activation(out=rstd[:, 0:NS], in_=ss[:, 0:NS], func=AF.Sqrt,
                                                bias=epsb[:, 0:1], scale=1.0 / D),
                   reads=[t_ss, t_eps], writes=[t_rstd])
            cx.dve(lambda: nc.vector.reciprocal(out=rstd[:, 0:NS], in_=rstd[:, 0:NS]),
                   reads=[t_rstd], writes=[t_rstd])
            for s in range(NS):
                cx.dve(lambda s=s: nc.vector.tensor_scalar(out=xn[:, s, :], in0=xt[:, s, :],
                                                           scalar1=rstd[:, s:s + 1], scalar2=None,
                                                           op0=ALU.mult),
                       reads=[tx, t_rstd], writes=[t_xn])

        def transp(i):
            for s in range(NS):
                for kc in range(NKC):
                    cx.pe(lambda s=s, kc=kc: nc.tensor.transpose(
                        out=pT[:, kc, s * 128:(s + 1) * 128], in_=xn[:, s, kc * 128:(kc + 1) * 128],
                        identity=ident[:]), reads=[t_xn, t_ident], writes=[t_pT])
            cx.dve(lambda: nc.vector.tensor_tensor(
                out=xnT[i % 2][:], in0=pT[:], in1=gcol[:, :].unsqueeze(2).to_broadcast([128, NKC, TT]),
                op=ALU.mult), reads=[t_pT, t_gcol], writes=[t_xnT[i % 2]])

        def gate_up(i, fc):
            b = fc % 2
            xT = xnT[i % 2]
            for which, (W, tw) in enumerate(((Wg, t_wg), (Wu, t_wu))):
                for kc in range(NKC):
                    cx.pe(lambda which=which, W=W, kc=kc: nc.tensor.matmul(
                        pgu[b][:, which, :], lhsT=W[:, fc, kc, :], rhs=xT[:, kc, :],
                        start=(kc == 0), stop=(kc == NKC - 1)),
                        reads=[tw[fc], t_xnT[i % 2]], writes=[t_pgu[b]])
            cx.act(lambda: nc.scalar.activation(out=sg[b][:], in_=pgu[b][:, 0, :], func=AF.Silu),
                   reads=[t_pgu[b]], writes=[t_sg[b]])
            cx.dve(lambda: nc.vector.tensor_tensor(out=hT[i % 2][:, fc, :], in0=sg[b][:], in1=pgu[b][:, 1, :],
                                                   op=ALU.mult),
                   reads=[t_sg[b], t_pgu[b]], writes=[t_hT[i % 2]])

        def down(i):
            xt = xb[i % 2]
            tx = t_xb[i % 2]
            g = 0
            for s in range(NS):
                for half in range(2):
                    b = g % 2
                    g += 1
                    for fc in range(NFC):
                        cx.pe(lambda fc=fc: nc.tensor.matmul(
                            py[b][:], lhsT=hT[i % 2][:, fc, s * 128:(s + 1) * 128],
                            rhs=Wd[:, fc, half * 512:(half + 1) * 512],
                            start=(fc == 0), stop=(fc == NFC - 1)),
                            reads=[t_hT[i % 2], t_wd[fc]], writes=[t_py[b]])
                    cx.dve(lambda: nc.vector.scalar_tensor_tensor(
                        out=xt[:, s, half * 512:(half + 1) * 512], in0=py[b][:], scalar=0.5,
                        in1=xt[:, s, half * 512:(half + 1) * 512], op0=ALU.mult, op1=ALU.add),
                        reads=[t_py[b], tx], writes=[tx])

        def finish(i):
            xt = xb[i % 2]
            tx = t_xb[i % 2]
            if final_gain is not None:
                for s in range(NS):
                    cx.act(lambda s=s: nc.scalar.activation(out=junk[:], in_=xt[:, s, :], func=AF.Square,
                                                            accum_out=ss[:, 2 + s:3 + s]),
                           reads=[tx], writes=[t_junk, t_ss])
                cx.act(lambda: nc.scalar.activation(out=rstd[:, 2:2 + NS], in_=ss[:, 2:2 + NS], func=AF.Sqrt,
                                                    bias=epsb[:, 0:1], scale=1.0 / D),
                       reads=[t_ss, t_eps], writes=[t_rstd])
                cx.dve(lambda: nc.vector.reciprocal(out=rstd[:, 2:2 + NS], in_=rstd[:, 2:2 + NS]),
                       reads=[t_rstd], writes=[t_rstd])
                for s in range(NS):
                    cx.dve(lambda s=s: nc.vector.scalar_tensor_tensor(
                        out=xt[:, s, :], in0=xt[:, s, :], scalar=rstd[:, 2 + s:3 + s], in1=fg[:],
                        op0=ALU.mult, op1=ALU.mult), reads=[tx, t_rstd, t_fg], writes=[tx])
            cx.dma_sp(tok_view(dst, i * TT, TT), xt[:], reads=[tx])

        load_x(0)
        load_w()
        norm(0)
        transp(0)
        for i in range(ntiles):
            if i + 1 < ntiles:
                load_x(i + 1)
            for fc in range(NFC):
                gate_up(i, fc)
                if fc == 12 and i + 1 < ntiles:
                    norm(i + 1)
            if i + 1 < ntiles:
                transp(i + 1)
            down(i)
            finish(i)
        cx.phase_end()


class NormT:
    def __init__(self, cx, es, TT, nbuf_xnT=2):
        nc = cx.nc
        self.cx = cx
        self.TT = TT
        self.NS = TT // 128
        pf = _pfx()
        sb = lambda n, s, d: es.enter_context(nc.sbuf_tensor(pf + n, s, d))
        ps = lambda n, s, d: es.enter_context(nc.psum_tensor(pf + n, s, d))
        self.xn = sb("xn", [128, self.NS, D], BF16)
        self.t_xn = Trk()
        self.junk = sb("junk", [128, D], BF16)
        self.t_junk = Trk()
        self.xnT = [sb(f"xnT{i}", [128, NKC, TT], BF16) for i in range(nbuf_xnT)]
        self.t_xnT = [Trk() for _ in range(nbuf_xnT)]
        self.ss = sb("ss", [128, 4], F32)
        self.t_ss = Trk()
        self.rstd = sb("rstd", [128, 4], F32)
        self.t_rstd = Trk()
        self.gcol = sb("gcol", [128, NKC], F32)
        self.t_gcol = Trk()
        self.ident = sb("ident", [128, 128], BF16)
        self.t_ident = Trk()
        self.epsb = sb("epsb", [128, 1], F32)
        self.t_eps = Trk()
        self.pT = ps("pT", [128, NKC, TT], BF16)
        self.t_pT = Trk()

    def init(self, gcol_ap):
        cx, nc = self.cx, self.cx.nc
        ident, epsb = self.ident, self.epsb
        cx.pool(lambda: nc.gpsimd.memset(ident[:], 0.0), writes=[self.t_ident])
        cx.pool(lambda: nc.gpsimd.affine_select(
            out=ident[:], in_=ident[:], pattern=[[-1, 128]], compare_op=ALU.not_equal,
            fill=1.0, base=0, channel_multiplier=1), reads=[self.t_ident], writes=[self.t_ident])
        cx.pool(lambda: nc.gpsimd.memset(epsb[:], RMS_EPS), writes=[self.t_eps])
        cx.dma_sp(self.gcol[:], gcol_ap, writes=[self.t_gcol])

    def norm(self, xt, tx):
        cx, nc = self.cx, self.cx.nc
        NS = self.NS
        ss, rstd, junk, xn, epsb = self.ss, self.rstd, self.junk, self.xn, self.epsb
        for s in range(NS):
            cx.act(lambda s=s: nc.scalar.activation(out=junk[:], in_=xt[:, s, :], func=AF.Square,
                                                    accum_out=ss[:, s:s + 1]),
                   reads=[tx], writes=[self.t_junk, self.t_ss])
        cx.act(lambda: nc.scalar.activation(out=rstd[:, 0:NS], in_=ss[:, 0:NS], func=AF.Sqrt,
                                            bias=epsb[:, 0:1], scale=1.0 / D),
               reads=[self.t_ss, self.t_eps], writes=[self.t_rstd])
        cx.dve(lambda: nc.vector.reciprocal(out=rstd[:, 0:NS], in_=rstd[:, 0:NS]),
               reads=[self.t_rstd], writes=[self.t_rstd])
        for s in range(NS):
            cx.dve(lambda s=s: nc.vector.tensor_scalar(out=xn[:, s, :], in0=xt[:, s, :],
                                                       scalar1=rstd[:, s:s + 1], scalar2=None,
                                                       op0=ALU.mult),
                   reads=[tx, self.t_rstd], writes=[self.t_xn])

    def transp(self, j):
        cx, nc = self.cx, self.cx.nc
        pT, xn, ident, gcol, TT = self.pT, self.xn, self.ident, self.gcol, self.TT
        for s in range(self.NS):
            for kc in range(NKC):
                cx.pe(lambda s=s, kc=kc: nc.tensor.transpose(
                    out=pT[:, kc, s * 128:(s + 1) * 128], in_=xn[:, s, kc * 128:(kc + 1) * 128],
                    identity=ident[:]), reads=[self.t_xn, self.t_ident], writes=[self.t_pT])
        cx.dve(lambda: nc.vector.tensor_tensor(
            out=self.xnT[j][:], in0=pT[:], in1=gcol[:, :].unsqueeze(2).to_broadcast([128, NKC, TT]),
            op=ALU.mult), reads=[self.t_pT, self.t_gcol], writes=[self.t_xnT[j]])


def conv_phase(cx, src, dst, ntok, T, w_in, w_out, small_ap, b_out_ap, gcol_ap):
    nc = cx.nc
    TT = 256
    NS = TT // 128
    ntiles = ntok // TT
    tps = T // TT
    HALO = CONVW - 1
    with contextlib.ExitStack() as es:
        pf = _pfx()
        sb = lambda n, s, d: es.enter_context(nc.sbuf_tensor(pf + n, s, d))
        ps = lambda n, s, d: es.enter_context(nc.psum_tensor(pf + n, s, d))
        nt = NormT(cx, es, TT)
        Win = sb("Win", [128, 16, NKC, 128], BF16)
        t_win = [Trk() for _ in range(16)]
        Wout = sb("Wout", [128, NKC, D], BF16)
        t_wout = Trk()
        Dg = sb("Dg", [128, NKC, CONVW, 128], BF16)
        t_dg = Trk()
        small = sb("small", [128, 288], F32)
        t_small = Trk()
        bout = sb("bout", [128, D], F32)
        t_bout = Trk()
        identf = sb("identf", [128, 128], F32)
        t_identf = Trk()
        ones = sb("ones", [128, 128], F32)
        t_ones = Trk()
        lneps = sb("lneps", [128, 1], F32)
        t_lneps = Trk()
        xb = [sb(f"xb{i}", [128, NS, D], F32) for i in range(2)]
        t_xb = [Trk(), Trk()]
        gT = [sb(f"gT{i}", [128, NKC, HALO + TT], BF16) for i in range(2)]
        t_gT = [Trk(), Trk()]
        sg = [sb(f"sg{i}", [128, TT], F32) for i in range(2)]
        t_sg = [Trk(), Trk()]
        cT = sb("cT", [128, NKC, TT], F32)
        t_cT = Trk()
        sq = sb("sq", [128, NKC, TT], F32)
        t_sq = Trk()
        mean = sb("mean", [128, TT], F32)
        t_mean = Trk()
        var = sb("var", [128, TT], F32)
        t_var = Trk()
        aT = sb("aT", [128, NKC, TT], BF16)
        t_aT = Trk()
        pab = [ps(f"pab{i}", [128, 2, TT], F32) for i in range(2)]
        t_pab = [Trk(), Trk()]
        pc = ps("pc", [128, 2, 512], F32)
        t_pc = [Trk(), Trk()]
        pst = ps("pst", [128, 2, TT], F32)
        t_pst = Trk()
        py = [ps("py0", [128, 512], F32)] * 2
        t_py = [Trk()] * 2

        nt.init(gcol_ap)
        cx.dma_sp(small[:], small_ap, writes=[t_small])
        cx.dma_sp(bout[:], b_out_ap.partition_broadcast(128), writes=[t_bout])
        for oc in range(16):
            cx.dma_pl(Win[:, oc], w_in[:, oc], writes=[t_win[oc]])
        for kc in range(0, NKC, 2):
            cx.dma_pl(Wout[:, kc:kc + 2], w_out[:, kc:kc + 2], writes=[t_wout])
        cx.pool(lambda: nc.gpsimd.memset(identf[:], 0.0), writes=[t_identf])
        cx.pool(lambda: nc.gpsimd.affine_select(
            out=identf[:], in_=identf[:], pattern=[[-1, 128]], compare_op=ALU.not_equal,
            fill=1.0, base=0, channel_multiplier=1), reads=[t_identf], writes=[t_identf])
        cx.pool(lambda: nc.gpsimd.memset(ones[:], 1.0), writes=[t_ones])
        cx.pool(lambda: nc.gpsimd.memset(lneps[:], LN_EPS), writes=[t_lneps])
        for cc in range(NKC):
            cx.dve(lambda cc=cc: nc.vector.tensor_tensor(
                out=Dg[:, cc], in0=identf[:, :].unsqueeze(1).to_broadcast([128, CONVW, 128]),
                in1=small[:, 40 + cc * CONVW:40 + (cc + 1) * CONVW].unsqueeze(2).to_broadcast([128, CONVW, 128]),
                op=ALU.mult), reads=[t_identf, t_small], writes=[t_dg])

        def load_x(i):
            cx.dma_sp(xb[i % 2][:], tok_view(src, i * TT, TT), writes=[t_xb[i % 2]])

        def glu(i):
            j = i % 2
            xT = nt.xnT[j]
            g = gT[j]
            if i % tps == 0:
                cx.pool(lambda: nc.gpsimd.memset(g[:, :, 0:HALO], 0.0), writes=[t_gT[j]])
            else:
                cx.pool(lambda: nc.gpsimd.tensor_copy(out=g[:, :, 0:HALO], in_=gT[1 - j][:, :, TT:TT + HALO]),
                        reads=[t_gT[1 - j]], writes=[t_gT[j]])
            for cc in range(NKC):
                b = cc % 2
                for which in range(2):
                    oc = which * 8 + cc
                    for kc in range(NKC):
                        cx.pe(lambda which=which, oc=oc, kc=kc: nc.tensor.matmul(
                            pab[b][:, which, :], lhsT=Win[:, oc, kc, :], rhs=xT[:, kc, :],
                            start=(kc == 0), stop=(kc == NKC - 1)),
                            reads=[t_win[oc], nt.t_xnT[j]], writes=[t_pab[b]])
                cx.act(lambda cc=cc: nc.scalar.activation(out=sg[b][:], in_=pab[b][:, 1, :], func=AF.Sigmoid,
                                                          bias=small[:, 8 + cc:9 + cc]),
                       reads=[t_pab[b], t_small], writes=[t_sg[b]])
                cx.dve(lambda cc=cc: nc.vector.scalar_tensor_tensor(
                    out=g[:, cc, HALO:HALO + TT], in0=pab[b][:, 0, :], scalar=small[:, cc:cc + 1],
                    in1=sg[b][:], op0=ALU.add, op1=ALU.mult),
                    reads=[t_pab[b], t_sg[b], t_small], writes=[t_gT[j]])

        def dwconv(i):
            j = i % 2
            g = gT[j]
            for cc in range(NKC):
                b = cc % 2
                for k in range(CONVW):
                    cx.pe(lambda cc=cc, k=k: nc.tensor.matmul(
                        pc[:, b, 0:TT], lhsT=Dg[:, cc, k, :], rhs=g[:, cc, k:k + TT],
                        start=(k == 0), stop=(k == CONVW - 1)),
                        reads=[t_dg, t_gT[j]], writes=[t_pc[b]])
                cx.act(lambda cc=cc: nc.scalar.activation(out=cT[:, cc, :], in_=pc[:, b, 0:TT], func=AF.Identity,
                                                          bias=small[:, 16 + cc:17 + cc]),
                       reads=[t_pc[b], t_small], writes=[t_cT])
                cx.pool(lambda cc=cc: nc.gpsimd.tensor_tensor(out=sq[:, cc, :], in0=cT[:, cc, :], in1=cT[:, cc, :],
                                                              op=ALU.mult),
                        reads=[t_cT], writes=[t_sq])

        def lnorm_stats(i):
            for which, (buf, tb) in enumerate(((cT, t_cT), (sq, t_sq))):
                for cc in range(NKC):
                    cx.pe(lambda which=which, buf=buf, cc=cc: nc.tensor.matmul(
                        pst[:, which, :], lhsT=ones[:], rhs=buf[:, cc, :],
                        start=(cc == 0), stop=(cc == NKC - 1)),
                        reads=[t_ones, tb], writes=[t_pst])

        def lnorm_chain(i):
            cx.dve(lambda: nc.vector.tensor_scalar(out=mean[:], in0=pst[:, 0, :], scalar1=1.0 / D, scalar2=None,
                                                   op0=ALU.mult), reads=[t_pst], writes=[t_mean])
            cx.dve(lambda: nc.vector.tensor_tensor(out=var[:], in0=mean[:], in1=mean[:], op=ALU.mult),
                   reads=[t_mean], writes=[t_var])
            cx.dve(lambda: nc.vector.scalar_tensor_tensor(out=var[:], in0=pst[:, 1, :], scalar=1.0 / D, in1=var[:],
                                                          op0=ALU.mult, op1=ALU.subtract),
                   reads=[t_pst, t_var], writes=[t_var])
            cx.act(lambda: nc.scalar.activation(out=var[:], in_=var[:], func=AF.Sqrt, bias=lneps[:, 0:1]),
                   reads=[t_var, t_lneps], writes=[t_var])
            cx.dve(lambda: nc.vector.reciprocal(out=var[:], in_=var[:]), reads=[t_var], writes=[t_var])
            cx.dve(lambda: nc.vector.tensor_tensor(
                out=cT[:], in0=cT[:], in1=mean[:, :].unsqueeze(1).to_broadcast([128, NKC, TT]), op=ALU.subtract),
                reads=[t_cT, t_mean], writes=[t_cT])
            cx.dve(lambda: nc.vector.tensor_tensor(
                out=cT[:], in0=cT[:], in1=var[:, :].unsqueeze(1).to_broadcast([128, NKC, TT]), op=ALU.mult),
                reads=[t_cT, t_var], writes=[t_cT])
            for cc in range(NKC):
                cx.act(lambda cc=cc: nc.scalar.activation(out=aT[:, cc, :], in_=cT[:, cc, :], func=AF.Silu,
                                                          scale=small[:, 24 + cc:25 + cc],
                                                          bias=small[:, 32 + cc:33 + cc]),
                       reads=[t_cT, t_small], writes=[t_aT])

        def outproj(i):
            xt = xb[i % 2]
            tx = t_xb[i % 2]
            for s in range(NS):
                cx.pool(lambda s=s: nc.gpsimd.tensor_tensor(out=xt[:, s, :], in0=xt[:, s, :], in1=bout[:], op=ALU.add),
                        reads=[tx, t_bout], writes=[tx])
            g = 0
            for s in range(NS):
                for half in range(2):
                    b = g % 2
                    g += 1
                    for kc in range(NKC):
                        cx.pe(lambda kc=kc: nc.tensor.matmul(
                            py[b][:], lhsT=aT[:, kc, s * 128:(s + 1) * 128],
                            rhs=Wout[:, kc, half * 512:(half + 1) * 512],
                            start=(kc == 0), stop=(kc == NKC - 1)),
                            reads=[t_aT, t_wout], writes=[t_py[b]])
                    cx.dve(lambda: nc.vector.tensor_tensor(
                        out=xt[:, s, half * 512:(half + 1) * 512], in0=py[b][:],
                        in1=xt[:, s, half * 512:(half + 1) * 512], op=ALU.add),
                        reads=[t_py[b], tx], writes=[tx])
            cx.dma_sp(tok_view(dst, i * TT, TT), xt[:], reads=[tx])

        load_x(0)
        nt.norm(xb[0], t_xb[0])
        nt.transp(0)
        glu(0)
        for i in range(ntiles):
            nxt = i + 1 < ntiles
            if nxt:
                load_x(i + 1)
            dwconv(i)
            if nxt:
                nt.norm(xb[(i + 1) % 2], t_xb[(i + 1) % 2])
                nt.transp((i + 1) % 2)
            lnorm_stats(i)
            if nxt:
                glu(i + 1)
            lnorm_chain(i)
            outproj(i)
        cx.phase_end()


C0 = math.exp(-0.5)
RDT = BF16


RW_STOP = [None]


def rwkv_phase(cx, src, dst, ntok, T, w_rkvo, w1, a1, g1, w2, a2, g2, vecs, mu_ap, gcol_ap, dbg=None):
    nc = cx.nc
    L = 64
    nchunks = T // L
    assert ntok == 2 * T
    with contextlib.ExitStack() as es:
        pf = _pfx()
        sb = lambda n, s, d: es.enter_context(nc.sbuf_tensor(pf + n, s, d))
        ps = lambda n, s, d: es.enter_context(nc.psum_tensor(pf + n, s, d))

        Wrkvo = sb("Wrkvo", [128, 4, NKC, D], BF16)
        t_W = [Trk() for _ in range(4)]
        W1 = sb("W1", [128, NKC, 64], BF16)
        A1 = sb("A1", [128, NKC, 64], BF16)
        G1 = sb("G1", [128, NKC, 160], BF16)
        W2 = sb("W2", [64, D], BF16)
        A2 = sb("A2", [64, D], BF16)
        G2a = sb("G2a", [128, D], BF16)
        G2b = sb("G2b", [32, D], BF16)
        t_lw = Trk()
        VB = sb("VB", [128, 7, D], F32)
        t_vb = Trk()
        mu = sb("mu", [128, 6, NKC], F32)
        gcol = sb("gcol", [128, NKC], F32)
        t_small = Trk()
        identf = sb("identf", [128, 128], F32)
        identb = sb("identb", [128, 128], BF16)
        triI = sb("triI", [128, 128], F32)
        triLt = sb("triLt", [128, 128], F32)
        triGt = sb("triGt", [128, 128], F32)
        bsel = sb("bsel", [128, 2], F32)
        ones1 = sb("ones1", [128, 1], F32)
        mST = sb("mST", [128, 64], F32)
        mIT = sb("mIT", [128, 64], F32)
        mS = sb("mS", [128, 64], F32)
        epsr = sb("epsr", [128, 1], F32)
        epsg = sb("epsg", [128, 1], F32)
        t_c = Trk()

        XT = sb("XT", [128, D], F32); t_XT = Trk()
        Rt = sb("Rt", [128, D], F32); t_R = Trk()
        Kt = sb("Kt", [128, D], F32); t_K = Trk()
        Vt = sb("Vt", [128, D], F32); t_V = Trk()
        SW = sb("SW", [128, D], F32); t_SW = Trk()
        AS = sb("AS", [128, D], F32); t_AS = Trk()
        KK = sb("KK", [128, D], F32); t_KK = Trk()
        X1 = sb("X1", [128, D], F32); t_X1 = Trk()
        X2 = sb("X2", [128, D], F32); t_X2 = Trk()
        RB = sb("RB", [128, D], RDT); t_RB = Trk()
        AB = sb("AB", [128, D], RDT); t_AB = Trk()
        KB = sb("KB", [128, D], RDT); t_KB = Trk()
        BB = sb("BB", [128, D], RDT); t_BB = Trk()
        KH = sb("KH", [128, D], RDT); t_KH = Trk()
        BH = sb("BH", [128, D], RDT); t_BH = Trk()
        Vb = sb("Vb", [128, D], RDT); t_Vb = Trk()
        MB = sb("MB", [128, H, HS], RDT); t_MB = Trk()
        xnT = sb("xnT", [128, NKC, 2, L + 1], F32); t_xnT = Trk()
        mix = [sb(f"mix{i}", [128, NKC, 128], BF16) for i in range(2)]
        t_mix = [[Trk() for _ in range(NKC)] for _ in range(2)]
        thw = sb("thw", [64, 128], BF16); t_thw = Trk()
        haT = sb("haT", [64, 128], BF16); t_haT = Trk()
        sgT_ = [sb(f"sgT{i}", [128, 128], BF16) for i in range(2)]
        sgT2_ = [sb(f"sgT2{i}", [32, 128], BF16) for i in range(2)]
        t_sgT_ = [Trk(), Trk()]
        FT = sb("FT", [128, H, 4, L], RDT); t_FT = Trk()
        AabT = sb("AabT", [128, H, L], RDT); t_AabT = [Trk(), Trk()]
        ArbT = sb("ArbT", [128, H, L], RDT); t_ArbT = [Trk(), Trk()]
        AakT = sb("AakT", [128, H, L], RDT); t_AakT = [Trk(), Trk()]
        ArkT = sb("ArkT", [128, H, L], RDT); t_ArkT = [Trk(), Trk()]
        GX = sb("GX", [128, H, 2 * L], RDT); t_GX = [Trk(), Trk()]
        M = sb("M", [128, H, HS], F32); t_M = Trk()
        WL = sb("WL", [128, H], F32); t_WL = Trk()
        st = sb("st", [128, 8, H], F32); t_st = Trk()
        sc = sb("sc", [128, 8], F32); t_sc = Trk()
        ygT = sb("ygT", [128, NKC, 128], BF16); t_ygT = Trk()
        YG = sb("YG", [128, D], BF16); t_YG = Trk()

        PS = ps("PS", [128, 8, 512], F32)
        t_bank = [Trk() for _ in range(8)]

        def bank(i):
            return PS[:, i, :]

        for q in range(4):
            for kc in range(0, NKC, 2):
                cx.dma_pl(Wrkvo[:, q, kc:kc + 2], w_rkvo[q, :, kc:kc + 2], writes=[t_W[q]])
        cx.dma_pl(W1[:], w1, writes=[t_lw])
        cx.dma_pl(A1[:], a1, writes=[t_lw])
        cx.dma_pl(G1[:], g1, writes=[t_lw])
        cx.dma_pl(W2[:], w2, writes=[t_lw])
        cx.dma_pl(A2[:], a2, writes=[t_lw])
        cx.dma_pl(G2a[:], g2[0:128, :], writes=[t_lw])
        cx.dma_pl(G2b[:], g2[128:160, :], writes=[t_lw])
        for i in range(7):
            cx.dma_sp(VB[:, i, :], vecs[i].partition_broadcast(128), writes=[t_vb])
        cx.dma_sp(mu[:], mu_ap, writes=[t_small])
        cx.dma_sp(gcol[:], gcol_ap, writes=[t_small])

        P = cx.pool
        g = nc.gpsimd

        def ident_like(t):
            P(lambda: g.memset(t[:], 0.0), writes=[t_c])
            P(lambda: g.affine_select(out=t[:], in_=t[:], pattern=[[-1, 128]], compare_op=ALU.not_equal,
                                      fill=1.0, base=0, channel_multiplier=1), reads=[t_c], writes=[t_c])
        ident_like(identf)
        ident_like(identb)
        for t in (triI, triLt, triGt):
            P(lambda t=t: g.memset(t[:], 1.0), writes=[t_c])
        P(lambda: g.affine_select(out=triI[:], in_=triI[:], pattern=[[1, 128]], compare_op=ALU.is_ge,
                                  fill=0.0, base=0, channel_multiplier=-1), reads=[t_c], writes=[t_c])
        P(lambda: g.affine_select(out=triLt[:], in_=triLt[:], pattern=[[1, 128]], compare_op=ALU.is_gt,
                                  fill=0.0, base=0, channel_multiplier=-1), reads=[t_c], writes=[t_c])
        P(lambda: g.affine_select(out=triGt[:], in_=triGt[:], pattern=[[-1, 128]], compare_op=ALU.is_gt,
                                  fill=0.0, base=0, channel_multiplier=1), reads=[t_c], writes=[t_c])
        P(lambda: g.memset(triI[0:64, 64:128], 0.0), writes=[t_c])
        P(lambda: g.memset(triLt[0:64, 64:128], 0.0), writes=[t_c])
        P(lambda: g.memset(triGt[64:128, 0:64], 0.0), writes=[t_c])
        P(lambda: g.memset(ones1[:], 1.0), writes=[t_c])
        P(lambda: g.memset(bsel[:], 0.0), writes=[t_c])
        P(lambda: g.memset(bsel[0:64, 0:1], 1.0), writes=[t_c])
        P(lambda: g.memset(bsel[64:128, 1:2], 1.0), writes=[t_c])
        for t in (mST, mIT, mS):
            P(lambda t=t: g.memset(t[:], 1.0), writes=[t_c])
        for hb in range(2):
            sl = slice(64 * hb, 64 * hb + 64)
            P(lambda sl=sl: g.affine_select(out=mST[sl, :], in_=mST[sl, :], pattern=[[1, 64]], compare_op=ALU.is_gt,
                                            fill=0.0, base=0, channel_multiplier=-1), reads=[t_c], writes=[t_c])
            P(lambda sl=sl: g.affine_select(out=mIT[sl, :], in_=mIT[sl, :], pattern=[[1, 64]], compare_op=ALU.is_ge,
                                            fill=0.0, base=0, channel_multiplier=-1), reads=[t_c], writes=[t_c])
            P(lambda sl=sl: g.affine_select(out=mS[sl, :], in_=mS[sl, :], pattern=[[-1, 64]], compare_op=ALU.is_gt,
                                            fill=0.0, base=0, channel_multiplier=1), reads=[t_c], writes=[t_c])
        P(lambda: g.memset(epsr[:], RMS_EPS), writes=[t_c])
        P(lambda: g.memset(epsg[:], GN_EPS), writes=[t_c])
        P(lambda: g.memset(M[:], 0.0), writes=[t_M])
        P(lambda: g.memset(MB[:], 0.0), writes=[t_MB])

        V3 = lambda t: t[:, :].rearrange("p (h i) -> p h i", i=HS)
        bc_h = lambda col: col.unsqueeze(2).to_broadcast([128, H, HS])
        evac_flip = [0]

        def evac_copy(out, in_, reads, writes):
            evac_flip[0] ^= 1
            if evac_flip[0]:
                cx.act(lambda: nc.scalar.copy(out=out, in_=in_), reads=reads, writes=writes)
            else:
                cx.dve(lambda: nc.vector.tensor_copy(out=out, in_=in_), reads=reads, writes=writes)

        def proj512(lhs_fn, rhs_fn, nk, bk, reads):
            for k in range(nk):
                cx.pe(lambda k=k: nc.tensor.matmul(bank(bk), lhsT=lhs_fn(k), rhs=rhs_fn(k),
                                                   start=(k == 0), stop=(k == nk - 1)),
                      reads=(reads(k) if callable(reads) else reads), writes=[t_bank[bk]])

        def make_mix(m, j):
            XX = XT[:, :].rearrange("p (k b s) -> p k b s", k=NKC, b=2)
            if False:
                T4 = X2[:, :].rearrange("p (k b s) -> p k b s", k=NKC, b=2)
                P(lambda: g.tensor_tensor(out=T4, in0=XX,
                                          in1=mu[:, m, :].unsqueeze(2).unsqueeze(3).to_broadcast([128, NKC, 2, L]),
                                          op=ALU.mult), reads=[t_XT, t_small], writes=[t_X2])
                P(lambda: g.tensor_tensor(out=mix[j][:, :, :].rearrange("p k (b s) -> p k b s", b=2), in0=T4,
                                          in1=xnT[:, :, :, 1:L + 1], op=ALU.add),
                  reads=[t_X2, t_xnT], writes=t_mix[j])
                return
            for kc in range(NKC):
                cx.dve(lambda kc=kc: nc.vector.scalar_tensor_tensor(
                    out=mix[j][:, kc, :].rearrange("p (b s) -> p b s", b=2), in0=XX[:, kc], scalar=mu[:, m, kc:kc + 1],
                    in1=xnT[:, kc, :, 1:L + 1], op0=ALU.mult, op1=ALU.add),
                    reads=[t_XT, t_xnT, t_small], writes=[t_mix[j][kc]])

        _order = ['A', 'B', 'C', 'D', 'E1', 'E2', 'E3', 'E4', 'E5', 'F']
        en = lambda s: RW_STOP[0] is None or _order.index(s) <= _order.index(RW_STOP[0])

        def stage_AB(c):
            for b in range(2):
                cx.dma_sp(XT[64 * b:64 * b + 64, :], src[b * T + c * L:b * T + (c + 1) * L, :], writes=[t_XT])
            cx.act(lambda: nc.scalar.activation(out=YG[:], in_=XT[:], func=AF.Square, accum_out=sc[:, 0:1]),
                   reads=[t_XT], writes=[t_YG, t_sc])
            cx.act(lambda: nc.scalar.activation(out=sc[:, 1:2], in_=sc[:, 0:1], func=AF.Sqrt, bias=epsr[:, 0:1],
                                                scale=1.0 / D), reads=[t_sc, t_c], writes=[t_sc])
            cx.dve(lambda: nc.vector.reciprocal(out=sc[:, 1:2], in_=sc[:, 1:2]), reads=[t_sc], writes=[t_sc])
            cx.dve(lambda: nc.vector.tensor_scalar(out=XT[:], in0=XT[:], scalar1=sc[:, 1:2], scalar2=None,
                                                   op0=ALU.mult), reads=[t_XT, t_sc], writes=[t_XT])
            if c == 0:
                P(lambda: g.memset(xnT[:, :, :, 0:1], 0.0), writes=[t_xnT])
            else:
                P(lambda: g.tensor_copy(out=xnT[:, :, :, 0:1], in_=xnT[:, :, :, L:L + 1]), reads=[t_xnT], writes=[t_xnT])
            PTv = PS[:, 0:2, :].rearrange("p a (k t) -> p (a k) t", t=128)
            for kc in range(NKC):
                cx.pe(lambda kc=kc: nc.tensor.transpose(out=PTv[:, kc, :], in_=XT[:, kc * 128:(kc + 1) * 128],
                                                        identity=identf[:]),
                      reads=[t_XT, t_c], writes=[t_bank[kc // 4]])
            cx.dve(lambda: nc.vector.tensor_tensor(
                out=xnT[:, :, :, 1:L + 1], in0=PTv.rearrange("p k (b s) -> p k b s", b=2),
                in1=gcol[:, :].unsqueeze(2).unsqueeze(3).to_broadcast([128, NKC, 2, L]), op=ALU.mult),
                reads=[t_bank[0], t_bank[1], t_small], writes=[t_xnT])
            cx.dve(lambda: nc.vector.tensor_tensor(
                out=XT[:, :].rearrange("p (k b s) -> p k b s", k=NKC, b=2), in0=xnT[:, :, :, 0:L],
                in1=xnT[:, :, :, 1:L + 1], op=ALU.subtract), reads=[t_xnT], writes=[t_XT])

            if en('B'):
                pass
                def rkv(m, q, dstt, tdst, j):
                    make_mix(m, j)
                    for half in range(2):
                        bk = 2 + half
                        proj512(lambda k: mix[j][:, k, :], lambda k: Wrkvo[:, q, k, half * 512:(half + 1) * 512], NKC, bk,
                                lambda k: [t_mix[j][k], t_W[q]])
                        evac_copy(dstt[:, half * 512:(half + 1) * 512], bank(bk), [t_bank[bk]], [tdst])
                rkv(0, 0, Rt, t_R, 0)
                rkv(2, 1, Kt, t_K, 1)
                rkv(3, 2, Vt, t_V, 0)
                P(lambda: g.tensor_copy(out=Vb[:], in_=Vt[:]), reads=[t_V], writes=[t_Vb])
                make_mix(1, 1)
                for k in range(NKC):
                    cx.pe(lambda k=k: nc.tensor.matmul(PS[0:64, 4, 0:128], lhsT=W1[:, k, :], rhs=mix[1][:, k, :],
                                                       start=(k == 0), stop=(k == NKC - 1)),
                          reads=[t_mix[1][k], t_lw], writes=[t_bank[4]])
                cx.act(lambda: nc.scalar.activation(out=thw[:], in_=PS[0:64, 4, 0:128], func=AF.Tanh),
                       reads=[t_bank[4]], writes=[t_thw])
                for half in range(2):
                    bk = 2 + half
                    cx.pe(lambda: nc.tensor.matmul(bank(bk), lhsT=thw[:], rhs=W2[:, half * 512:(half + 1) * 512],
                                                   start=True, stop=True), reads=[t_thw, t_lw], writes=[t_bank[bk]])
                    cx.dve(lambda: nc.vector.tensor_tensor(out=SW[:, half * 512:(half + 1) * 512], in0=bank(bk),
                                                           in1=VB[:, 0, half * 512:(half + 1) * 512], op=ALU.add),
                           reads=[t_bank[bk], t_vb], writes=[t_SW])
                cx.act(lambda: nc.scalar.activation(out=SW[:], in_=SW[:], func=AF.Sigmoid), reads=[t_SW], writes=[t_SW])
                make_mix(4, 0)
                for k in range(NKC):
                    cx.pe(lambda k=k: nc.tensor.matmul(PS[0:64, 5, 0:128], lhsT=A1[:, k, :], rhs=mix[0][:, k, :],
                                                       start=(k == 0), stop=(k == NKC - 1)),
                          reads=[t_mix[0][k], t_lw], writes=[t_bank[5]])
                cx.act(lambda: nc.scalar.copy(out=haT[:], in_=PS[0:64, 5, 0:128]), reads=[t_bank[5]], writes=[t_haT])
                for half in range(2):
                    bk = 2 + half
                    cx.pe(lambda: nc.tensor.matmul(bank(bk), lhsT=haT[:], rhs=A2[:, half * 512:(half + 1) * 512],
                                                   start=True, stop=True), reads=[t_haT, t_lw], writes=[t_bank[bk]])
                    cx.dve(lambda: nc.vector.tensor_tensor(out=AS[:, half * 512:(half + 1) * 512], in0=bank(bk),
                                                           in1=VB[:, 1, half * 512:(half + 1) * 512], op=ALU.add),
                           reads=[t_bank[bk], t_vb], writes=[t_AS])
                cx.act(lambda: nc.scalar.activation(out=AS[:], in_=AS[:], func=AF.Sigmoid), reads=[t_AS], writes=[t_AS])
                make_mix(5, 1)
                for k in range(NKC):
                    cx.pe(lambda k=k: nc.tensor.matmul(PS[:, 4, 0:128], lhsT=G1[:, k, 0:128], rhs=mix[1][:, k, :],
                                                       start=(k == 0), stop=(k == NKC - 1)),
                          reads=[t_mix[1][k], t_lw], writes=[t_bank[4]])
                for k in range(NKC):
                    cx.pe(lambda k=k: nc.tensor.matmul(PS[0:32, 5, 0:128], lhsT=G1[:, k, 128:160], rhs=mix[1][:, k, :],
                                                       start=(k == 0), stop=(k == NKC - 1)),
                          reads=[t_mix[1][k], t_lw], writes=[t_bank[5]])
                sgT, sgT2, t_sgT = sgT_[c % 2], sgT2_[c % 2], t_sgT_[c % 2]
                cx.act(lambda: nc.scalar.activation(out=sgT[:], in_=PS[:, 4, 0:128], func=AF.Sigmoid),
                       reads=[t_bank[4]], writes=[t_sgT])
                cx.act(lambda: nc.scalar.activation(out=sgT2[:], in_=PS[0:32, 5, 0:128], func=AF.Sigmoid),
                       reads=[t_bank[5]], writes=[t_sgT])

        def stage_CDE(c):
            if en('C'):
                pass
                cx.dve(lambda: nc.vector.tensor_tensor(out=KK[:], in0=Kt[:], in1=VB[:, 2, :], op=ALU.mult),
                       reads=[t_K, t_vb], writes=[t_KK])
                cx.dve(lambda: nc.vector.tensor_tensor(out=XT[:], in0=KK[:], in1=KK[:], op=ALU.mult),
                       reads=[t_KK], writes=[t_XT])
                cx.dve(lambda: nc.vector.tensor_reduce(out=st[:, 0, :], in_=V3(XT), axis=AX.X, op=ALU.add),
                       reads=[t_XT], writes=[t_st])
                cx.act(lambda: nc.scalar.activation(out=st[:, 0, :], in_=st[:, 0, :], func=AF.Sqrt),
                       reads=[t_st], writes=[t_st])
                cx.dve(lambda: nc.vector.tensor_scalar_max(out=st[:, 0, :], in0=st[:, 0, :], scalar1=1e-12),
                       reads=[t_st], writes=[t_st])
                cx.dve(lambda: nc.vector.reciprocal(out=st[:, 0, :], in_=st[:, 0, :]), reads=[t_st], writes=[t_st])
                cx.dve(lambda: nc.vector.tensor_tensor(out=V3(KK), in0=V3(KK), in1=bc_h(st[:, 0, :]), op=ALU.mult),
                       reads=[t_KK, t_st], writes=[t_KK])
                cx.dve(lambda: nc.vector.scalar_tensor_tensor(out=XT[:], in0=AS[:], scalar=-1.0, in1=VB[:, 3, :],
                                                              op0=ALU.add, op1=ALU.mult),
                       reads=[t_AS, t_vb], writes=[t_XT])
                cx.dve(lambda: nc.vector.scalar_tensor_tensor(out=Kt[:], in0=XT[:], scalar=1.0, in1=Kt[:],
                                                              op0=ALU.add, op1=ALU.mult),
                       reads=[t_XT, t_K], writes=[t_K])
                cx.dve(lambda: nc.vector.tensor_tensor(out=AS[:], in0=AS[:], in1=KK[:], op=ALU.mult), reads=[t_AS, t_KK], writes=[t_AS])
                P(lambda: g.tensor_tensor(out=XT[:], in0=Rt[:], in1=Kt[:], op=ALU.mult), reads=[t_R, t_K], writes=[t_XT])
                P(lambda: g.tensor_tensor(out=XT[:], in0=XT[:], in1=VB[:, 4, :], op=ALU.mult), reads=[t_XT, t_vb], writes=[t_XT])
                cx.dve(lambda: nc.vector.tensor_reduce(out=st[:, 1, :], in_=V3(XT), axis=AX.X, op=ALU.add),
                       reads=[t_XT], writes=[t_st])
                WLp = PS[:, 6, 0:H]
                for h in range(H):
                    for b in range(2):
                        cs = slice(64 * b, 64 * b + 64)
                        cx.pe(lambda: nc.tensor.matmul(WLp[cs, h:h + 1], lhsT=SW[cs, h * HS:(h + 1) * HS], rhs=ones1[cs, 0:1],
                                                       start=True, stop=True), reads=[t_SW, t_c], writes=[t_bank[6]])
                cx.act(lambda: nc.scalar.activation(out=WL[:], in_=WLp, func=AF.Exp, scale=-C0),
                       reads=[t_bank[6]], writes=[t_WL])
                for half in range(2):
                    hs = slice(half * 512, (half + 1) * 512)
                    bkI, bkE, bkT = (2, 3, 4) if half == 0 else (5, 6, 2)
                    cx.pe(lambda: nc.tensor.matmul(bank(bkI), lhsT=triI[:], rhs=SW[:, hs], start=True, stop=True),
                          reads=[t_SW, t_c], writes=[t_bank[bkI]])
                    cx.pe(lambda: nc.tensor.matmul(bank(bkE), lhsT=triLt[:], rhs=SW[:, hs], start=True, stop=True),
                          reads=[t_SW, t_c], writes=[t_bank[bkE]])
                    cx.pe(lambda: nc.tensor.matmul(bank(bkT), lhsT=triGt[:], rhs=SW[:, hs], start=True, stop=True),
                          reads=[t_SW, t_c], writes=[t_bank[bkT]])
                    tmps = ((XT, t_XT), (X1, t_X1), (X2, t_X2))
                    E, tE = tmps[(4 * half + 0) % 3]
                    cx.act(lambda: nc.scalar.activation(out=E[:, hs], in_=bank(bkI), func=AF.Exp, scale=-C0),
                           reads=[t_bank[bkI]], writes=[tE])
                    cx.dve(lambda: nc.vector.tensor_tensor(out=RB[:, hs], in0=Rt[:, hs], in1=E[:, hs], op=ALU.mult),
                           reads=[t_R, tE], writes=[t_RB])
                    E, tE = tmps[(4 * half + 1) % 3]
                    cx.act(lambda: nc.scalar.activation(out=E[:, hs], in_=bank(bkI), func=AF.Exp, scale=C0),
                           reads=[t_bank[bkI]], writes=[tE])
                    cx.dve(lambda: nc.vector.tensor_tensor(out=KB[:, hs], in0=Kt[:, hs], in1=E[:, hs], op=ALU.mult),
                           reads=[t_K, tE], writes=[t_KB])
                    P(lambda: g.tensor_tensor(out=BB[:, hs], in0=AS[:, hs], in1=E[:, hs], op=ALU.mult),
                      reads=[t_AS, tE], writes=[t_BB])
                    E, tE = tmps[(4 * half + 2) % 3]
                    cx.act(lambda: nc.scalar.activation(out=E[:, hs], in_=bank(bkE), func=AF.Exp, scale=-C0),
                           reads=[t_bank[bkE]], writes=[tE])
                    cx.dve(lambda: nc.vector.scalar_tensor_tensor(out=AB[:, hs], in0=KK[:, hs], scalar=-1.0, in1=E[:, hs],
                                                                  op0=ALU.mult, op1=ALU.mult),
                           reads=[t_KK, tE], writes=[t_AB])
                    E, tE = tmps[(4 * half + 3) % 3]
                    cx.act(lambda: nc.scalar.activation(out=E[:, hs], in_=bank(bkT), func=AF.Exp, scale=-C0),
                           reads=[t_bank[bkT]], writes=[tE])
                    cx.dve(lambda: nc.vector.tensor_tensor(out=KH[:, hs], in0=Kt[:, hs], in1=E[:, hs], op=ALU.mult),
                           reads=[t_K, tE], writes=[t_KH])
                    P(lambda: g.tensor_tensor(out=BH[:, hs], in0=AS[:, hs], in1=E[:, hs], op=ALU.mult),
                      reads=[t_AS, tE], writes=[t_BH])

            if en('D'):
                pass
                srcs = ((AB, t_AB), (RB, t_RB), (BB, t_BB), (KB, t_KB))
                identq = identb if RDT == BF16 else identf
                for hp in range(NKC):
                    bk = hp % 2
                    PTq = bank(bk).rearrange("p (e q t) -> p e q t", e=2, q=4)
                    for hh in range(2):
                        h = 2 * hp + hh
                        for q, (tl, tt) in enumerate(srcs):
                            for b in range(2):
                                cs = slice(64 * b, 64 * b + 64)
                                cx.pe(lambda q=q, tl=tl: nc.tensor.matmul(
                                    PTq[cs, hh, q, :], lhsT=tl[cs, h * HS:(h + 1) * HS], rhs=identq[cs, cs],
                                    start=True, stop=True), reads=[tt, t_c], writes=[t_bank[bk]])
                    evac_copy(FT[:, 2 * hp:2 * hp + 2], PTq, [t_bank[bk]], [t_FT])

            if en('E1'):
                pass
                for grp in range(4):
                    b1, b2, b3 = (2, 3, 4) if grp % 2 == 0 else (5, 6, 7)
                    P1 = bank(b1).rearrange("p (h q t) -> p h q t", h=4, q=2)
                    P2 = bank(b2).rearrange("p (h q t) -> p h q t", h=4, q=2)
                    P3 = bank(b3)[:, 0:256].rearrange("p (h t) -> p h t", h=4)
                    for hl in range(4):
                        h = grp * 4 + hl
                        for b in range(2):
                            cs = slice(64 * b, 64 * b + 64)
                            cx.pe(lambda: nc.tensor.matmul(P1[cs, hl], lhsT=FT[cs, h, 2, :], rhs=FT[cs, h, 0:2, :],
                                                           start=True, stop=True), reads=[t_FT], writes=[t_bank[b1]])
                            cx.pe(lambda: nc.tensor.matmul(P2[cs, hl], lhsT=FT[cs, h, 3, :], rhs=FT[cs, h, 0:2, :],
                                                           start=True, stop=True), reads=[t_FT], writes=[t_bank[b2]])
                            cx.pe(lambda: nc.tensor.matmul(P3[cs, hl], lhsT=FT[cs, h, 0, :], rhs=FT[cs, h, 2, :],
                                                           start=True, stop=True), reads=[t_FT], writes=[t_bank[b3]])
                    hsl = slice(grp * 4, grp * 4 + 4)
                    mb = lambda m: m[:, :].unsqueeze(1).to_broadcast([128, 4, L])
                    cx.dve(lambda: nc.vector.tensor_tensor(out=AabT[:, hsl, :], in0=P1[:, :, 0, :], in1=mb(mST), op=ALU.mult),
                           reads=[t_bank[b1], t_c], writes=[t_AabT[grp // 2]])
                    cx.dve(lambda: nc.vector.tensor_tensor(out=ArbT[:, hsl, :], in0=P1[:, :, 1, :], in1=mb(mIT), op=ALU.mult),
                           reads=[t_bank[b1], t_c], writes=[t_ArbT[grp // 2]])
                    cx.dve(lambda: nc.vector.tensor_tensor(out=AakT[:, hsl, :], in0=P2[:, :, 0, :], in1=mb(mST), op=ALU.mult),
                           reads=[t_bank[b2], t_c], writes=[t_AakT[grp // 2]])
                    cx.dve(lambda: nc.vector.tensor_tensor(out=ArkT[:, hsl, :], in0=P2[:, :, 1, :], in1=mb(mIT), op=ALU.mult),
                           reads=[t_bank[b2], t_c], writes=[t_ArkT[grp // 2]])
                    cx.dve(lambda: nc.vector.tensor_tensor(out=GX[:, hsl, 0:L], in0=P3, in1=mb(mS), op=ALU.mult),
                           reads=[t_bank[b3], t_c], writes=[t_GX[grp // 2]])

            if en('E2'):
                pass
                PX = PS[:, 0:2, :].rearrange("p a (h i) -> p (a h) i", i=HS)
                for h in range(H):
                    for b in range(2):
                        cs = slice(64 * b, 64 * b + 64)
                        cx.pe(lambda: nc.tensor.matmul(PX[cs, h, :], lhsT=FT[cs, h, 0, :], rhs=MB[cs, h, :],
                                                       start=True, stop=False), reads=[t_FT, t_MB], writes=[t_bank[h // 8]])
                        cx.pe(lambda: nc.tensor.matmul(PX[cs, h, :], lhsT=AakT[cs, h, :], rhs=Vb[cs, h * HS:(h + 1) * HS],
                                                       start=False, stop=True), reads=[t_AakT[h // 8], t_Vb], writes=[t_bank[h // 8]])
                for hf in range(2):
                    hsl = slice(8 * hf, 8 * hf + 8)
                    cx.act(lambda: nc.scalar.copy(out=GX[:, hsl, L:2 * L], in_=PX[:, hsl, :]), reads=[t_bank[hf]], writes=[t_GX[hf]])

            if en('E3'):
                pass
                PA = PS[:, 2:6, :].rearrange("p a (h t) -> p (a h) t", t=2 * L)
                PB = PS[:, 6:8, :].rearrange("p a (h t) -> p (a h) t", t=L)
                NLEV = 6
                for lev in range(NLEV):
                    last = (lev == NLEV - 1)
                    for hf in range(2):
                        hsl = slice(8 * hf, 8 * hf + 8)
                        for h in range(8 * hf, 8 * hf + 8):
                            for b in range(2):
                                cs = slice(64 * b, 64 * b + 64)
                                if not last:
                                    cx.pe(lambda: nc.tensor.matmul(PA[cs, h, :], lhsT=AabT[cs, h, :], rhs=GX[cs, h, :],
                                                                   start=True, stop=True),
                                          reads=[t_AabT[hf], t_GX[hf]], writes=[t_bank[2 + h // 4]])
                                    cx.pe(lambda: nc.tensor.matmul(PB[cs, h, :], lhsT=GX[cs, h, 0:L], rhs=AabT[cs, h, :],
                                                                   start=True, stop=True),
                                          reads=[t_AabT[hf], t_GX[hf]], writes=[t_bank[6 + hf]])
                                else:
                                    cx.pe(lambda: nc.tensor.matmul(PA[cs, h, L:2 * L], lhsT=AabT[cs, h, :], rhs=GX[cs, h, L:2 * L],
                                                                   start=True, stop=True),
                                          reads=[t_AabT[hf], t_GX[hf]], writes=[t_bank[2 + h // 4]])
                        pa_t = [t_bank[2 + 2 * hf], t_bank[3 + 2 * hf]]
                        cx.dve(lambda: nc.vector.tensor_tensor(out=GX[:, hsl, L:2 * L], in0=PA[:, hsl, L:2 * L],
                                                               in1=GX[:, hsl, L:2 * L], op=ALU.add),
                               reads=pa_t + [t_GX[hf]], writes=[t_GX[hf]])
                        if not last:
                            cx.act(lambda: nc.scalar.copy(out=GX[:, hsl, 0:L], in_=PA[:, hsl, 0:L]), reads=pa_t, writes=[t_GX[hf]])
                            cx.act(lambda: nc.scalar.copy(out=AabT[:, hsl, :], in_=PB[:, hsl, :]), reads=[t_bank[6 + hf]],
                                   writes=[t_AabT[hf]])

            if en('E4'):
                pass
                PY = PX
                for h in range(H):
                    for b in range(2):
                        cs = slice(64 * b, 64 * b + 64)
                        cx.pe(lambda: nc.tensor.matmul(PY[cs, h, :], lhsT=FT[cs, h, 1, :], rhs=MB[cs, h, :],
                                                       start=True, stop=False), reads=[t_FT, t_MB], writes=[t_bank[h // 8]])
                        cx.pe(lambda: nc.tensor.matmul(PY[cs, h, :], lhsT=ArbT[cs, h, :], rhs=GX[cs, h, L:2 * L],
                                                       start=False, stop=False), reads=[t_ArbT[h // 8], t_GX[h // 8]], writes=[t_bank[h // 8]])
                        cx.pe(lambda: nc.tensor.matmul(PY[cs, h, :], lhsT=ArkT[cs, h, :], rhs=Vb[cs, h * HS:(h + 1) * HS],
                                                       start=False, stop=True), reads=[t_ArkT[h // 8], t_Vb], writes=[t_bank[h // 8]])
                cx.act(lambda: nc.scalar.copy(out=V3(X1), in_=PY), reads=[t_bank[0], t_bank[1]], writes=[t_X1])

            if en('E5'):
                pass
                PM = PS[:, 2:4, :].rearrange("p a (h i) -> p (a h) i", i=HS)
                for h in range(H):
                    for b in range(2):
                        cs = slice(64 * b, 64 * b + 64)
                        cx.pe(lambda: nc.tensor.matmul(PM[cs, h, :], lhsT=BH[cs, h * HS:(h + 1) * HS], rhs=GX[cs, h, L:2 * L],
                                                       start=True, stop=False), reads=[t_BH, t_GX[h // 8]], writes=[t_bank[2 + h // 8]])
                        cx.pe(lambda: nc.tensor.matmul(PM[cs, h, :], lhsT=KH[cs, h * HS:(h + 1) * HS], rhs=Vb[cs, h * HS:(h + 1) * HS],
                                                       start=False, stop=True), reads=[t_KH, t_Vb], writes=[t_bank[2 + h // 8]])
                cx.dve(lambda: nc.vector.tensor_tensor(out=M[:], in0=M[:], in1=WL[:, :].unsqueeze(2).to_broadcast([128, H, HS]),
                                                       op=ALU.mult), reads=[t_M, t_WL], writes=[t_M])
                cx.dve(lambda: nc.vector.tensor_tensor(out=M[:], in0=M[:], in1=PM, op=ALU.add),
                       reads=[t_M, t_bank[2], t_bank[3]], writes=[t_M])
                cx.act(lambda: nc.scalar.copy(out=MB[:], in_=M[:]), reads=[t_M], writes=[t_MB])

            if en('F'):
                P(lambda: g.tensor_tensor(out=V3(KK), in0=V3(Vt), in1=bc_h(st[:, 1, :]), op=ALU.mult),
                  reads=[t_V, t_st, t_KK], writes=[t_KK])

        def stage_F(c):
            if en('F'):
                pass
                Y = X1
                cx.dve(lambda: nc.vector.tensor_reduce(out=st[:, 2, :], in_=V3(Y), axis=AX.X, op=ALU.add),
                       reads=[t_X1], writes=[t_st])
                cx.dve(lambda: nc.vector.scalar_tensor_tensor(out=V3(Y), in0=bc_h(st[:, 2, :]), scalar=-1.0 / HS, in1=V3(Y),
                                                              op0=ALU.mult, op1=ALU.add), reads=[t_X1, t_st], writes=[t_X1])
                cx.dve(lambda: nc.vector.tensor_tensor(out=X2[:], in0=Y[:], in1=Y[:], op=ALU.mult), reads=[t_X1], writes=[t_X2])
                cx.dve(lambda: nc.vector.tensor_reduce(out=st[:, 3, :], in_=V3(X2), axis=AX.X, op=ALU.add),
                       reads=[t_X2], writes=[t_st])
                cx.act(lambda: nc.scalar.activation(out=st[:, 3, :], in_=st[:, 3, :], func=AF.Sqrt, bias=epsg[:, 0:1],
                                                    scale=1.0 / HS), reads=[t_st, t_c], writes=[t_st])
                cx.dve(lambda: nc.vector.reciprocal(out=st[:, 3, :], in_=st[:, 3, :]), reads=[t_st], writes=[t_st])
                cx.dve(lambda: nc.vector.tensor_tensor(out=V3(Y), in0=V3(Y), in1=bc_h(st[:, 3, :]), op=ALU.mult),
                       reads=[t_X1, t_st], writes=[t_X1])
                cx.dve(lambda: nc.vector.tensor_tensor(out=Y[:], in0=Y[:], in1=VB[:, 5, :], op=ALU.mult), reads=[t_X1, t_vb], writes=[t_X1])
                cx.dve(lambda: nc.vector.tensor_tensor(out=Y[:], in0=Y[:], in1=VB[:, 6, :], op=ALU.add), reads=[t_X1, t_vb], writes=[t_X1])
                cx.dve(lambda: nc.vector.tensor_tensor(out=Y[:], in0=Y[:], in1=KK[:], op=ALU.add),
                       reads=[t_X1, t_KK], writes=[t_X1])
                sgT, sgT2, t_sgT = sgT_[c % 2], sgT2_[c % 2], t_sgT_[c % 2]
                for half in range(2):
                    bk = half
                    hs = slice(half * 512, (half + 1) * 512)
                    cx.pe(lambda: nc.tensor.matmul(bank(bk), lhsT=sgT[:], rhs=G2a[:, hs], start=True, stop=False),
                          reads=[t_sgT, t_lw], writes=[t_bank[bk]])
                    cx.pe(lambda: nc.tensor.matmul(bank(bk), lhsT=sgT2[:], rhs=G2b[:, hs], start=False, stop=True),
                          reads=[t_sgT, t_lw], writes=[t_bank[bk]])
                    cx.dve(lambda: nc.vector.tensor_tensor(out=YG[:, hs], in0=Y[:, hs], in1=bank(bk), op=ALU.mult),
                           reads=[t_X1, t_bank[bk]], writes=[t_YG])
                PTb = PS[:, 7, :].bitcast(BF16)[:, 0:NKC * 128].rearrange("p (k t) -> p k t", t=128)
                for kc in range(NKC):
                    cx.pe(lambda kc=kc: nc.tensor.transpose(out=PTb[:, kc, :], in_=YG[:, kc * 128:(kc + 1) * 128],
                                                            identity=identb[:]), reads=[t_YG, t_c], writes=[t_bank[7]])
                cx.act(lambda: nc.scalar.copy(out=ygT[:], in_=PTb), reads=[t_bank[7]], writes=[t_ygT])
            for b in range(2):
                cx.dma_sp(XT[64 * b:64 * b + 64, :], src[b * T + c * L:b * T + (c + 1) * L, :], writes=[t_XT])
            for half in range(2 if en('F') else 0):
                bk = half
                hs = slice(half * 512, (half + 1) * 512)
                proj512(lambda k: ygT[:, k, :], lambda k: Wrkvo[:, 3, k, hs], NKC, bk, [t_ygT, t_W[3]])
                cx.dve(lambda: nc.vector.tensor_tensor(out=XT[:, hs], in0=XT[:, hs], in1=bank(bk), op=ALU.add),
                       reads=[t_XT, t_bank[bk]], writes=[t_XT])
            for b in range(2):
                cx.dma_sp(dst[b * T + c * L:b * T + (c + 1) * L, :], XT[64 * b:64 * b + 64, :], reads=[t_XT])

        stage_AB(0)
        for c in range(nchunks):
            stage_CDE(c)
            if c + 1 < nchunks:
                stage_AB(c + 1)
            stage_F(c)
        cx.phase_end()


def lay_gu(w):
    return np.ascontiguousarray(w.reshape(NKC, 128, NFC, 128).transpose(1, 2, 0, 3))


def lay_rows(w):
    r = w.shape[0] // 128
    return np.ascontiguousarray(w.reshape(r, 128, w.shape[1]).transpose(1, 0, 2))


def lay_col(v):
    return np.ascontiguousarray(v.reshape(-1, 128).T)


def build_program(ntok, phases, T=2048):
    nc = bass.Bass("TRN2", target_bir_lowering=False)
    dram_in = lambda n, s: nc.dram_tensor(n, s, F32, kind="ExternalInput").ap()
    x = dram_in("x", [ntok, D])
    out = nc.dram_tensor("out", [ntok, D], F32, kind="ExternalOutput").ap()
    scr = nc.dram_tensor("scr", [ntok, D], F32, kind="Internal").ap()
    wg = dram_in("ffn_wg", [4, 128, NFC, NKC, 128])
    wu = dram_in("ffn_wu", [4, 128, NFC, NKC, 128])
    wd = dram_in("ffn_wd", [4, 128, NFC, D])
    gcols = dram_in("gcols", [6, 128, NKC])
    fnorm = dram_in("final_norm", [D])
    cw_in = dram_in("conv_win", [128, 16, NKC, 128])
    cw_out = dram_in("conv_wout", [128, NKC, D])
    c_small = dram_in("conv_small", [128, 288])
    c_bout = dram_in("conv_bout", [D])
    rw_rkvo = dram_in("rw_rkvo", [4, 128, NKC, D])
    rw_w1 = dram_in("rw_w1", [128, NKC, 64])
    rw_a1 = dram_in("rw_a1", [128, NKC, 64])
    rw_g1 = dram_in("rw_g1", [128, NKC, 160])
    rw_w2 = dram_in("rw_w2", [64, D])
    rw_a2 = dram_in("rw_a2", [64, D])
    rw_g2 = dram_in("rw_g2", [160, D])
    rw_vecs = dram_in("rw_vecs", [7, D])
    rw_mu = dram_in("rw_mu", [128, 6, NKC])
    with contextlib.ExitStack() as es:
        cx = Ctx(nc, es)
        n = len(phases)
        for pi, ph in enumerate(phases):
            src = x if pi == 0 else scr
            dst = out if pi == n - 1 else scr
            if ph.startswith("ffn"):
                k = int(ph[3])
                layer, which = divmod(k, 2)
                gidx = layer * 3 + (0 if which == 0 else 2)
                ffn_phase(cx, src, dst, ntok, wg[k], wu[k], wd[k], gcols[gidx],
                          final_gain=(fnorm if ph.endswith("f") else None))
            elif ph == "rwkv":
                rwkv_phase(cx, src, dst, ntok, T, rw_rkvo, rw_w1, rw_a1, rw_g1, rw_w2, rw_a2, rw_g2, rw_vecs, rw_mu,
                           gcols[1])
            elif ph == "conv":
                conv_phase(cx, src, dst, ntok, T, cw_in, cw_out, c_small, c_bout, gcols[4])
            else:
                raise ValueError(ph)
    return nc


def prep_weights(inp):
    w = {}
    g = np.asarray(inp["ffn_w_gate"], np.float32)
    u = np.asarray(inp["ffn_w_up"], np.float32)
    dn = np.asarray(inp["ffn_w_down"], np.float32)
    w["ffn_wg"] = np.stack([lay_gu(g[l, j]) for l in range(2) for j in range(2)])
    w["ffn_wu"] = np.stack([lay_gu(u[l, j]) for l in range(2) for j in range(2)])
    w["ffn_wd"] = np.stack([lay_rows(dn[l, j]) for l in range(2) for j in range(2)])
    ng = np.asarray(inp["norm_gains"], np.float32)
    w["gcols"] = np.stack([lay_col(ng[l, j]) for l in range(2) for j in range(3)])
    w["final_norm"] = np.ascontiguousarray(np.asarray(inp["final_norm"], np.float32))
    cwi = np.asarray(inp["conv_w_in"], np.float32)[0]
    w["conv_win"] = np.ascontiguousarray(cwi.reshape(NKC, 128, 16, 128).transpose(1, 2, 0, 3))
    w["conv_wout"] = lay_rows(np.asarray(inp["conv_w_out"], np.float32)[0])
    dw = np.asarray(inp["conv_dw"], np.float32)[0]
    dwcol = dw.T.reshape(NKC, 128, CONVW).transpose(1, 0, 2).reshape(128, NKC * CONVW)
    w["conv_small"] = np.ascontiguousarray(np.concatenate([
        lay_col(np.asarray(inp["conv_b_in"], np.float32)[0]),
        lay_col(np.asarray(inp["conv_dw_b"], np.float32)[0]),
        lay_col(np.asarray(inp["conv_ln_gain"], np.float32)[0]),
        lay_col(np.asarray(inp["conv_ln_bias"], np.float32)[0]),
        dwcol], axis=1))
    w["conv_bout"] = np.ascontiguousarray(np.asarray(inp["conv_b_out"], np.float32)[0])
    f = lambda k: np.asarray(inp[k], np.float32)[0]
    rkv = f("rwkv_w_rkv")
    w["rw_rkvo"] = np.stack([lay_rows(rkv[0]), lay_rows(rkv[1]), lay_rows(rkv[2]), lay_rows(f("rwkv_w_out"))])
    w["rw_w1"] = lay_rows(f("rwkv_w1"))
    w["rw_a1"] = lay_rows(f("rwkv_a1"))
    w["rw_g1"] = lay_rows(f("rwkv_g1"))
    w["rw_w2"] = np.ascontiguousarray(f("rwkv_w2"))
    w["rw_a2"] = np.ascontiguousarray(f("rwkv_a2"))
    w["rw_g2"] = np.ascontiguousarray(f("rwkv_g2"))
    w["rw_vecs"] = np.ascontiguousarray(np.stack([f("rwkv_w0"), f("rwkv_a0"), f("rwkv_k_k"), f("rwkv_k_a"),
                                                  f("rwkv_r_k").reshape(D), f("rwkv_ln_gain"), f("rwkv_ln_bias")]))
    muv = f("rwkv_mu")
    w["rw_mu"] = np.ascontiguousarray(muv.reshape(6, NKC, 128).transpose(2, 0, 1))
    return w


ALL_PHASES = ["ffn0", "rwkv", "ffn1", "ffn2", "conv", "ffn3f"]


def kernel(**inputs):
    x = np.asarray(inputs["x"], np.float32)
    B, T, _ = x.shape
    nb = B // NCORES
    ntok = nb * T
    w = prep_weights(inputs)
    nc = build_program(ntok, ALL_PHASES, T=T)
    in_maps = []
    for c in range(NCORES):
        m = dict(w)
        m["x"] = np.ascontiguousarray(x[c * nb:(c + 1) * nb].reshape(ntok, D))
        in_maps.append(m)
    res = run_bass_kernel_spmd(nc, in_maps, core_ids=list(range(NCORES)))
    outs = [np.asarray(r["out"]).reshape(nb, T, D) for r in res.results]
    return np.concatenate(outs, axis=0).astype(np.float32)
```
